# Optimizing a Trainium2 kernel written in Bass

```python
import math
import jax, jax.numpy as jnp
from jax import lax
import numpy as np

D_MODEL = 1024
BATCH = 8
SEQ = 2048
DEPTH = 4

CHUNK = 64
Q_BLOCK = 128
N_MIXERS = 3

SSD_EXPAND = 2
SSD_D_INNER = SSD_EXPAND * D_MODEL
SSD_HEAD_DIM = 64
SSD_HEADS = SSD_D_INNER // SSD_HEAD_DIM
SSD_GROUPS = 4
SSD_HEADS_PER_GROUP = SSD_HEADS // SSD_GROUPS
SSD_STATE = 128
SSD_CONV = 4
SSD_CONV_DIM = SSD_D_INNER + 2 * SSD_GROUPS * SSD_STATE
SSD_IN_DIM = SSD_D_INNER + SSD_CONV_DIM + SSD_HEADS

ATT_HEAD_DIM = 64
ATT_HEADS = D_MODEL // ATT_HEAD_DIM
ATT_DIM = ATT_HEADS * ATT_HEAD_DIM

FFN_DIM = 2816
N_EXPERTS = 8
TOP_K = 2
EXPERT_DIM = 3584

DN_ALPHA = (2.0 * DEPTH) ** 0.25
DN_BETA = (8.0 * DEPTH) ** -0.25
LN_EPS = 1e-5
RMS_EPS = 1e-5

kernel_name = "hybrid_ssd_stickbreak_fox_moe_deepnorm"

F32 = jnp.float32


def layer_norm(x, g, b):
    xf = x.astype(F32)
    mu = jnp.mean(xf, -1, keepdims=True)
    var = jnp.mean(jnp.square(xf - mu), -1, keepdims=True)
    return ((xf - mu) * lax.rsqrt(var + LN_EPS) * g + b).astype(x.dtype)


def causal_dwconv(u, w, b):
    c = u.shape[-1]
    y = lax.conv_general_dilated(u, w[:, None, :], window_strides=(1,),
                                 padding=[(SSD_CONV - 1, 0)],
                                 dimension_numbers=("NWC", "WIO", "NWC"),
                                 feature_group_count=c)
    return y + b


def segsum_exp(a_cs):
    n = a_cs.shape[-1]
    mask = jnp.tril(jnp.ones((n, n), bool))
    diff = a_cs[..., :, None] - a_cs[..., None, :]
    return jnp.where(mask, jnp.exp(jnp.where(mask, diff, 0.0)), 0.0)


def ssd_mixer(u, w_in, conv_w, conv_b, dt_bias, a_log, d_skip, norm_w, w_out):
    bsz, seq, _ = u.shape
    n_chunks = seq // CHUNK
    G, R, P, N = SSD_GROUPS, SSD_HEADS_PER_GROUP, SSD_HEAD_DIM, SSD_STATE
    zxbcdt = u @ w_in
    z, xbc, dt_raw = jnp.split(zxbcdt, [SSD_D_INNER, SSD_D_INNER + SSD_CONV_DIM], axis=-1)
    xbc = jax.nn.silu(causal_dwconv(xbc, conv_w, conv_b))
    xs, bm, cm = jnp.split(xbc, [SSD_D_INNER, SSD_D_INNER + G * N], axis=-1)
    dt = jax.nn.softplus((dt_raw + dt_bias).astype(F32))
    da = dt * (-jnp.exp(a_log.astype(F32)))
    xh = xs.reshape(bsz, seq, SSD_HEADS, P).astype(F32)
    xdt = xh * dt[..., None]

    xc = xdt.reshape(bsz, n_chunks, CHUNK, G, R, P)
    bc = bm.astype(F32).reshape(bsz, n_chunks, CHUNK, G, N)
    cc = cm.astype(F32).reshape(bsz, n_chunks, CHUNK, G, N)
    ac = da.reshape(bsz, n_chunks, CHUNK, G, R).transpose(0, 1, 3, 4, 2)
    a_cs = jnp.cumsum(ac, axis=-1)

    decay_in = segsum_exp(a_cs)
    cb = jnp.einsum("bclgn,bcsgn->bcgls", cc, bc)
    y_diag = jnp.einsum("bcgls,bcgrls,bcsgrp->bclgrp", cb, decay_in, xc)

    decay_to_end = jnp.exp(a_cs[..., -1:] - a_cs)
    chunk_states = jnp.einsum("bclgn,bcgrl,bclgrp->bcgrpn", bc, decay_to_end, xc)
    chunk_decay = jnp.exp(a_cs[..., -1])

    def step(h, inp):
        dec, st = inp
        return dec[..., None, None] * h + st, h

    h0 = jnp.zeros((bsz, G, R, P, N), F32)
    _, h_in = lax.scan(step, h0, (jnp.moveaxis(chunk_decay, 1, 0), jnp.moveaxis(chunk_states, 1, 0)))
    h_in = jnp.moveaxis(h_in, 0, 1)
    y_off = jnp.einsum("bclgn,bcgrpn,bcgrl->bclgrp", cc, h_in, jnp.exp(a_cs))

    y = (y_diag + y_off).reshape(bsz, seq, SSD_HEADS, P) + d_skip.astype(F32)[:, None] * xh
    y = y.reshape(bsz, seq, SSD_D_INNER) * jax.nn.silu(z.astype(F32))
    yg = y.reshape(bsz, seq, G, SSD_D_INNER // G)
    yg = yg * lax.rsqrt(jnp.mean(jnp.square(yg), -1, keepdims=True) + RMS_EPS)
    y = (yg.reshape(bsz, seq, SSD_D_INNER) * norm_w).astype(u.dtype)
    return y @ w_out


def stick_breaking_mixer(u, w_qkv, w_out):
    bsz, seq, _ = u.shape
    qkv = (u @ w_qkv).reshape(bsz, seq, 3, ATT_HEADS, ATT_HEAD_DIM)
    q, k, v = qkv[:, :, 0], qkv[:, :, 1], qkv[:, :, 2]
    scale = ATT_HEAD_DIM ** -0.5
    outs = []
    for blk in range(seq // Q_BLOCK):
        q0 = blk * Q_BLOCK
        kv_len = q0 + Q_BLOCK
        z = jnp.einsum("bqhd,bkhd->bhqk", q[:, q0:kv_len], k[:, :kv_len]).astype(F32) * scale
        t_idx = q0 + jnp.arange(Q_BLOCK)[:, None]
        s_idx = jnp.arange(kv_len)[None, :]
        strict = s_idx < t_idx
        log_keep = jnp.where(strict, jax.nn.log_sigmoid(-z), 0.0)
        after = lax.cumsum(log_keep, axis=3, reverse=True) - log_keep
        w = jnp.where(strict, jnp.exp(jax.nn.log_sigmoid(z) + after), 0.0)
        outs.append(jnp.einsum("bhqk,bkhd->bqhd", w.astype(v.dtype), v[:, :kv_len]))
    o = jnp.concatenate(outs, axis=1).reshape(bsz, seq, ATT_DIM)
    return o @ w_out


def forgetting_mixer(u, w_qkvf, b_f, w_out):
    bsz, seq, _ = u.shape
    proj = u @ w_qkvf
    qkv = proj[..., :3 * ATT_DIM].reshape(bsz, seq, 3, ATT_HEADS, ATT_HEAD_DIM)
    q, k, v = qkv[:, :, 0], qkv[:, :, 1], qkv[:, :, 2]
    log_f = jax.nn.log_sigmoid((proj[..., 3 * ATT_DIM:] + b_f).astype(F32))
    c = jnp.cumsum(log_f, axis=1).transpose(0, 2, 1)
    scale = ATT_HEAD_DIM ** -0.5
    outs = []
    for blk in range(seq // Q_BLOCK):
        q0 = blk * Q_BLOCK
        kv_len = q0 + Q_BLOCK
        s = jnp.einsum("bqhd,bkhd->bhqk", q[:, q0:kv_len], k[:, :kv_len]).astype(F32) * scale
        s = s + c[:, :, q0:kv_len, None] - c[:, :, None, :kv_len]
        t_idx = q0 + jnp.arange(Q_BLOCK)[:, None]
        s_idx = jnp.arange(kv_len)[None, :]
        s = jnp.where(s_idx <= t_idx, s, -jnp.inf)
        p = jax.nn.softmax(s, axis=-1)
        outs.append(jnp.einsum("bhqk,bkhd->bqhd", p.astype(v.dtype), v[:, :kv_len]))
    o = jnp.concatenate(outs, axis=1).reshape(bsz, seq, ATT_DIM)
    return o @ w_out


def dense_swiglu(u, w_gate, w_up, w_down):
    return (jax.nn.silu(u @ w_gate) * (u @ w_up)) @ w_down


def moe_swiglu(u, w_router, we_gate, we_up, we_down):
    bsz, seq, d = u.shape
    xt = u.reshape(-1, d)
    logits = (xt @ w_router).astype(F32)
    top_logits, top_idx = lax.top_k(logits, TOP_K)
    gates = jax.nn.softmax(top_logits, axis=-1)
    flat_e = top_idx.reshape(-1)
    order = jnp.argsort(flat_e)
    tok = order // TOP_K
    xs = xt[tok]
    sizes = jnp.bincount(flat_e, length=N_EXPERTS).astype(jnp.int32)
    h = jax.nn.silu(lax.ragged_dot(xs, we_gate, sizes)) * lax.ragged_dot(xs, we_up, sizes)
    ys = lax.ragged_dot(h, we_down, sizes)
    ys = ys * gates.reshape(-1)[order][:, None].astype(ys.dtype)
    out = jnp.zeros_like(xt).at[tok].add(ys)
    return out.reshape(bsz, seq, d)


def _normal(key, shape, std):
    return jax.random.normal(key, shape, F32) * std


def _init_ssd(key, p):
    ks = jax.random.split(key, 8)
    dt = jnp.exp(jax.random.uniform(ks[3], (SSD_HEADS,), F32, math.log(1e-3), math.log(1e-1)))
    return {
        f"{p}_ssd_w_in": _normal(ks[0], (D_MODEL, SSD_IN_DIM), D_MODEL ** -0.5),
        f"{p}_ssd_conv_w": _normal(ks[1], (SSD_CONV, SSD_CONV_DIM), SSD_CONV ** -0.5),
        f"{p}_ssd_conv_b": _normal(ks[2], (SSD_CONV_DIM,), 0.02),
        f"{p}_ssd_dt_bias": dt + jnp.log(-jnp.expm1(-dt)),
        f"{p}_ssd_a_log": jnp.log(jax.random.uniform(ks[4], (SSD_HEADS,), F32, 1.0, 16.0)),
        f"{p}_ssd_d_skip": 1.0 + _normal(ks[5], (SSD_HEADS,), 0.1),
        f"{p}_ssd_norm_w": 1.0 + _normal(ks[6], (SSD_D_INNER,), 0.02),
        f"{p}_ssd_w_out": _normal(ks[7], (SSD_D_INNER, D_MODEL), DN_BETA * SSD_D_INNER ** -0.5),
    }


def _init_stick(key, p):
    ks = jax.random.split(key, 2)
    return {
        f"{p}_sb_w_qkv": _normal(ks[0], (D_MODEL, 3 * ATT_DIM), D_MODEL ** -0.5),
        f"{p}_sb_w_out": _normal(ks[1], (ATT_DIM, D_MODEL), DN_BETA * ATT_DIM ** -0.5),
    }


def _init_fox(key, p):
    ks = jax.random.split(key, 3)
    return {
        f"{p}_fox_w_qkvf": _normal(ks[0], (D_MODEL, 3 * ATT_DIM + ATT_HEADS), D_MODEL ** -0.5),
        f"{p}_fox_b_f": 1.0 + _normal(ks[1], (ATT_HEADS,), 0.5),
        f"{p}_fox_w_out": _normal(ks[2], (ATT_DIM, D_MODEL), DN_BETA * ATT_DIM ** -0.5),
    }


def _init_ln(key, p):
    ks = jax.random.split(key, 2)
    return {
        f"{p}_g": 1.0 + _normal(ks[0], (D_MODEL,), 0.02),
        f"{p}_b": _normal(ks[1], (D_MODEL,), 0.02),
    }


def _init_dense(key, p):
    ks = jax.random.split(key, 3)
    return {
        f"{p}_ffn_w_gate": _normal(ks[0], (D_MODEL, FFN_DIM), D_MODEL ** -0.5),
        f"{p}_ffn_w_up": _normal(ks[1], (D_MODEL, FFN_DIM), D_MODEL ** -0.5),
        f"{p}_ffn_w_down": _normal(ks[2], (FFN_DIM, D_MODEL), DN_BETA * FFN_DIM ** -0.5),
    }


def _init_moe(key, p):
    ks = jax.random.split(key, 4)
    return {
        f"{p}_moe_w_router": _normal(ks[0], (D_MODEL, N_EXPERTS), D_MODEL ** -0.5),
        f"{p}_moe_w_gate": _normal(ks[1], (N_EXPERTS, D_MODEL, EXPERT_DIM), D_MODEL ** -0.5),
        f"{p}_moe_w_up": _normal(ks[2], (N_EXPERTS, D_MODEL, EXPERT_DIM), D_MODEL ** -0.5),
        f"{p}_moe_w_down": _normal(ks[3], (N_EXPERTS, EXPERT_DIM, D_MODEL), DN_BETA * EXPERT_DIM ** -0.5),
    }


def setup_inputs(seed: int = 0) -> dict:
    key = jax.random.key(seed)
    key, k_x = jax.random.split(key)
    params = {"x": jax.random.normal(k_x, (BATCH, SEQ, D_MODEL), F32)}
    mixer_inits = (_init_ssd, _init_stick, _init_fox)
    for i in range(DEPTH):
        key, k_mix, k_ln1, k_ffn, k_ln2 = jax.random.split(key, 5)
        params.update(mixer_inits[i % N_MIXERS](k_mix, f"l{i}"))
        params.update(_init_ln(k_ln1, f"l{i}_ln_mix"))
        params.update((_init_dense if i % 2 == 0 else _init_moe)(k_ffn, f"l{i}"))
        params.update(_init_ln(k_ln2, f"l{i}_ln_ffn"))
    return params


def reference(x,
              l0_ssd_w_in, l0_ssd_conv_w, l0_ssd_conv_b, l0_ssd_dt_bias, l0_ssd_a_log,
              l0_ssd_d_skip, l0_ssd_norm_w, l0_ssd_w_out, l0_ln_mix_g, l0_ln_mix_b,
              l0_ffn_w_gate, l0_ffn_w_up, l0_ffn_w_down, l0_ln_ffn_g, l0_ln_ffn_b,
              l1_sb_w_qkv, l1_sb_w_out, l1_ln_mix_g, l1_ln_mix_b,
              l1_moe_w_router, l1_moe_w_gate, l1_moe_w_up, l1_moe_w_down, l1_ln_ffn_g, l1_ln_ffn_b,
              l2_fox_w_qkvf, l2_fox_b_f, l2_fox_w_out, l2_ln_mix_g, l2_ln_mix_b,
              l2_ffn_w_gate, l2_ffn_w_up, l2_ffn_w_down, l2_ln_ffn_g, l2_ln_ffn_b,
              l3_ssd_w_in, l3_ssd_conv_w, l3_ssd_conv_b, l3_ssd_dt_bias, l3_ssd_a_log,
              l3_ssd_d_skip, l3_ssd_norm_w, l3_ssd_w_out, l3_ln_mix_g, l3_ln_mix_b,
              l3_moe_w_router, l3_moe_w_gate, l3_moe_w_up, l3_moe_w_down, l3_ln_ffn_g, l3_ln_ffn_b):
    mixers = [
        (ssd_mixer, (l0_ssd_w_in, l0_ssd_conv_w, l0_ssd_conv_b, l0_ssd_dt_bias, l0_ssd_a_log,
                     l0_ssd_d_skip, l0_ssd_norm_w, l0_ssd_w_out), l0_ln_mix_g, l0_ln_mix_b),
        (stick_breaking_mixer, (l1_sb_w_qkv, l1_sb_w_out), l1_ln_mix_g, l1_ln_mix_b),
        (forgetting_mixer, (l2_fox_w_qkvf, l2_fox_b_f, l2_fox_w_out), l2_ln_mix_g, l2_ln_mix_b),
        (ssd_mixer, (l3_ssd_w_in, l3_ssd_conv_w, l3_ssd_conv_b, l3_ssd_dt_bias, l3_ssd_a_log,
                     l3_ssd_d_skip, l3_ssd_norm_w, l3_ssd_w_out), l3_ln_mix_g, l3_ln_mix_b),
    ]
    ffns = [
        (dense_swiglu, (l0_ffn_w_gate, l0_ffn_w_up, l0_ffn_w_down), l0_ln_ffn_g, l0_ln_ffn_b),
        (moe_swiglu, (l1_moe_w_router, l1_moe_w_gate, l1_moe_w_up, l1_moe_w_down), l1_ln_ffn_g, l1_ln_ffn_b),
        (dense_swiglu, (l2_ffn_w_gate, l2_ffn_w_up, l2_ffn_w_down), l2_ln_ffn_g, l2_ln_ffn_b),
        (moe_swiglu, (l3_moe_w_router, l3_moe_w_gate, l3_moe_w_up, l3_moe_w_down), l3_ln_ffn_g, l3_ln_ffn_b),
    ]
    for i in range(DEPTH):
        mix_fn, mix_p, g1, b1 = mixers[i]
        x = layer_norm(DN_ALPHA * x + mix_fn(x, *mix_p), g1, b1)
        ffn_fn, ffn_p, g2, b2 = ffns[i]
        x = layer_norm(DN_ALPHA * x + ffn_fn(x, *ffn_p), g2, b2)
    return x
```

```python
import numpy as np
from contextlib import ExitStack
import concourse.bass as bass
import concourse.mybir as mybir
from concourse.bass_utils import run_bass_kernel_spmd

F32 = mybir.dt.float32
BF16 = mybir.dt.bfloat16
AF = mybir.ActivationFunctionType
ALU = mybir.AluOpType

T = 2048
D = 1024
NB = 16
ALPHA = (2.0 * 4) ** 0.25
LN_EPS = 1e-5
NEG = -30000.0


class Op:
    __slots__ = ("eng", "fn", "reads", "writes", "dma", "signal", "count", "deps", "semkey", "kind", "kw")

    def __init__(self, eng, fn, reads, writes, dma, semkey):
        self.eng, self.fn, self.reads, self.writes = eng, fn, reads, writes
        self.dma, self.semkey = dma, semkey
        self.signal = False
        self.count = 0
        self.deps = []


class Sched:
    def __init__(self, nc, ctx):
        self.nc = nc
        self.ctx = ctx
        self.ops = []
        self.engobj = {"pe": nc.tensor, "act": nc.scalar, "dve": nc.vector,
                       "pool": nc.gpsimd, "sp": nc.sync}
        self.sems = {}
        self.cnt = {}
        self.last_w = {}
        self.readers = {}
        self.seen = {e: {} for e in self.engobj}
        self.last_op = {}
        self.pending_barrier = None
        self.dma_last = {}
        self.nops = 0
        self.if_state = None
        self.cregs = None

    def add(self, eng, fn, reads=(), writes=(), dma=False, semkey=None):
        op = Op(eng, fn, tuple(reads), tuple(writes), dma, semkey)
        self.ops.append(op)
        return op

    def pe(self, fn, reads=(), writes=()):
        return self.add("pe", fn, reads, writes)

    def act(self, fn, reads=(), writes=()):
        return self.add("act", fn, reads, writes)

    def dve(self, fn, reads=(), writes=()):
        return self.add("dve", fn, reads, writes)

    def pool(self, fn, reads=(), writes=()):
        return self.add("pool", fn, reads, writes)

    def dma(self, q, fn, reads=(), writes=(), semkey=None):
        return self.add(q, fn, reads, writes, dma=True, semkey=(writes[0] if semkey is None else semkey))

    def ctl(self, kind, **kw):
        op = Op("ctl", None, (), (), False, None)
        op.kind, op.kw = kind, kw
        self.ops.append(op)
        return op

    def _sem(self, key):
        s = self.sems.get(key)
        if s is None:
            s = self.ctx.enter_context(self.nc.semaphore("s%d" % len(self.sems)))
            self.sems[key] = s
        return s

    def flush(self, barrier=True):
        ops = self.ops
        self.ops = []
        last_w, readers = self.last_w, self.readers
        first_after = {}
        bar = self.pending_barrier
        for op in ops:
            if op.eng == "ctl":
                continue
            deps = set()
            for k in op.reads:
                w = last_w.get(k)
                if w is not None:
                    deps.add(w)
            for k in op.writes:
                w = last_w.get(k)
                if w is not None:
                    deps.add(w)
                for r in readers.get(k, ()):
                    deps.add(r)
            deps.discard(op)
            fin = []
            for d in deps:
                if d.dma:
                    fin.append(d)
                    continue
                if d.eng == op.eng and not op.dma:
                    if not any(k in d.writes for k in op.reads):
                        continue
                fin.append(d)
            if bar is not None and op.eng not in first_after:
                first_after[op.eng] = True
                fin.extend(bar)
            op.deps = fin
            for d in fin:
                d.signal = True
            for k in op.writes:
                last_w[k] = op
                readers[k] = []
            for k in op.reads:
                readers.setdefault(k, []).append(op)
            self.last_op[op.eng] = op
            if op.dma:
                self.dma_last[op.semkey] = op
        if bar is not None:
            pass
        if barrier:
            blist = [o for o in self.last_op.values() if not o.dma]
            blist += list(self.dma_last.values())
            if bar is not None and len(first_after) < len(self.engobj):
                blist += [o for o in bar if o not in blist]
            for o in blist:
                o.signal = True
            self.pending_barrier = blist
            self.last_w = {}
            self.readers = {}
            self.dma_last = {}
        else:
            self.pending_barrier = None
        cnt = self.cnt
        for op in ops:
            if op.eng == "ctl":
                continue
            if op.dma:
                key = ("dma", op.semkey)
                cnt[key] = cnt.get(key, 0) + 16
                op.count = cnt[key]
            elif op.signal:
                key = ("eng", op.eng)
                cnt[key] = cnt.get(key, 0) + 1
                op.count = cnt[key]
        for idx, op in enumerate(ops):
            if op.eng == "ctl":
                self._emit_ctl(op, ops, idx)
                continue
            eng = self.engobj[op.eng]
            need = {}
            for d in op.deps:
                key = ("dma", d.semkey) if d.dma else ("eng", d.eng)
                if d.count > need.get(key, 0):
                    need[key] = d.count
            sv = self.seen[op.eng]
            for key, c in need.items():
                if sv.get(key, 0) >= c:
                    continue
                eng.wait_ge(self._sem(key), c)
                sv[key] = c
            inst = op.fn(eng)
            if op.dma:
                inst.then_inc(self._sem(("dma", op.semkey)), 16)
            elif op.signal:
                inst.then_inc(self._sem(("eng", op.eng)), 1)
        self.nops += len(ops)

    def _emit_ctl(self, op, ops, idx):
        nc = self.nc
        if op.kind == "if":
            inc = {}
            base = {}
            j = idx + 1
            while not (ops[j].eng == "ctl" and ops[j].kind == "endif"):
                o = ops[j]
                if o.eng != "ctl":
                    if o.dma:
                        k2 = (("dma", o.semkey), o.eng)
                        inc[k2] = inc.get(k2, 0) + 16
                        if k2 not in base:
                            base[k2] = o.count - 16
                    elif o.signal:
                        k2 = (("eng", o.eng), o.eng)
                        inc[k2] = inc.get(k2, 0) + 1
                j += 1
            if self.cregs is None:
                self.cregs = nc.alloc_registers("cnd")
            nc.regs_load(self.cregs, op.kw["ap"])
            cm = nc.If_lt(self.cregs, op.kw["thresh"] + 1)
            cm.__enter__()
            for (key, engname), amt in inc.items():
                if (key, engname) in base and base[(key, engname)] > 0:
                    self.engobj[engname].wait_ge(self._sem(key), base[(key, engname)])
                self.engobj[engname].drain().then_inc(self._sem(key), amt)
            cm.__exit__(None, None, None)
            cm2 = nc.Else()
            cm2.__enter__()
            self.if_state = dict(cm=cm2, seen={e: dict(v) for e, v in self.seen.items()})
        elif op.kind == "endif":
            st = self.if_state
            st["cm"].__exit__(None, None, None)
            self.seen = st["seen"]
            self.if_state = None

    def final_wait(self, eng_name="sp"):
        eng = self.engobj[eng_name]
        for key, s in self.sems.items():
            eng.wait_ge(s, self.cnt[key])


class Builder:
    def __init__(self, stages):
        self.stages = stages
        self.nc = bass.Bass("TRN2", target_bir_lowering=False)
        self.din = {}
        self.used_inputs = []

    def inp(self, name, shape):
        if name not in self.din:
            self.din[name] = self.nc.dram_tensor(name, list(shape), F32, kind="ExternalInput").ap()
            self.used_inputs.append(name)
        return self.din[name]

    def sb(self, ctx, name, shape, dt):
        self.uid += 1
        return ctx.enter_context(self.nc.sbuf_tensor("%s_%d" % (name, self.uid), list(shape), dt))

    def build(self):
        nc = self.nc
        self.uid = 0
        self.x_in = self.inp("x", [T, D])
        self.y = nc.dram_tensor("y", [T, D], F32, kind="ExternalOutput").ap()
        self.scr = nc.dram_tensor("scr", [4, 3, 16 * T], BF16, kind="Internal").ap()
        with ExitStack() as ctx:
            self.ctx = ctx
            S = self.S = Sched(nc, ctx)
            self.P = [ctx.enter_context(nc.psum_tensor("ps%d" % i, [128, 512], F32)) for i in range(8)]
            self.xT = self.sb(ctx, "xT", [128, 8, T], BF16)
            self.gates = self.sb(ctx, "gates", [128, NB, 8], F32)
            self.identf = self.sb(ctx, "identf", [128, 128], F32)
            self.identb = self.sb(ctx, "identb", [128, 128], BF16)
            self.onesb = self.sb(ctx, "onesb", [128, 128], BF16)
            self.negm = self.sb(ctx, "negm", [128, 128], BF16)
            self.negs = self.sb(ctx, "negs", [128, 128], BF16)
            self.trige = self.sb(ctx, "trige", [128, 128], BF16)
            self.mstrict = self.sb(ctx, "mstrict", [128, 128], BF16)
            self.oneb = self.sb(ctx, "oneb", [128, 1], F32)
            self.consts()
            self.prep()
            S.flush(barrier=True)
            for st in self.stages:
                with ExitStack() as sctx:
                    layer, kind = st // 2, st % 2
                    if kind == 0:
                        if layer in (0, 3):
                            self.ssd_stage(sctx, layer)
                        elif layer == 1:
                            self.attn_stage(sctx, layer, "sb")
                        else:
                            self.attn_stage(sctx, layer, "fox")
                    else:
                        if layer % 2 == 1:
                            self.moe_stage(sctx, layer)
                        else:
                            self.ffn_stage(sctx, layer, moe=False)
                    S.flush(barrier=True)
            S.flush(barrier=True)
            S.final_wait("sp")
        return nc

    def consts(self):
        nc, S = self.nc, self.S
        S.pool(lambda e: e.memset(self.identf[:], 1.0), writes=["identf"])
        S.pool(lambda e: e.affine_select(out=self.identf[:], in_=self.identf[:], pattern=[[-1, 128]],
                                         compare_op=ALU.is_equal, fill=0.0, base=0, channel_multiplier=1),
               reads=["identf"], writes=["identf"])
        S.dve(lambda e: e.tensor_copy(out=self.identb[:], in_=self.identf[:]), reads=["identf"], writes=["identb"])
        S.pool(lambda e: e.memset(self.onesb[:], 1.0), writes=["onesb"])
        S.pool(lambda e: e.memset(self.oneb[:], 1.0), writes=["oneb"])
        S.pool(lambda e: e.memset(self.negm[:], 0.0), writes=["negm"])
        S.pool(lambda e: e.affine_select(out=self.negm[:], in_=self.negm[:], pattern=[[1, 128]],
                                         compare_op=ALU.is_ge, fill=NEG, base=0, channel_multiplier=-1),
               reads=["negm"], writes=["negm"])
        S.pool(lambda e: e.memset(self.negs[:], 0.0), writes=["negs"])
        S.pool(lambda e: e.affine_select(out=self.negs[:], in_=self.negs[:], pattern=[[1, 128]],
                                         compare_op=ALU.is_gt, fill=NEG, base=0, channel_multiplier=-1),
               reads=["negs"], writes=["negs"])
        S.pool(lambda e: e.memset(self.trige[:], 1.0), writes=["trige"])
        S.pool(lambda e: e.affine_select(out=self.trige[:], in_=self.trige[:], pattern=[[-1, 128]],
                                         compare_op=ALU.is_ge, fill=0.0, base=0, channel_multiplier=1),
               reads=["trige"], writes=["trige"])
        S.pool(lambda e: e.memset(self.mstrict[:], 1.0), writes=["mstrict"])
        S.pool(lambda e: e.affine_select(out=self.mstrict[:], in_=self.mstrict[:], pattern=[[1, 128]],
                                         compare_op=ALU.is_gt, fill=0.0, base=0, channel_multiplier=-1),
               reads=["mstrict"], writes=["mstrict"])

    def prep(self):
        S = self.S
        with ExitStack() as c:
            xb = [self.sb(c, "px", [128, D], F32) for _ in range(2)]
            for b in range(NB):
                t = xb[b % 2]
                rk = ("px", b % 2)
                S.dma("sp", lambda e, t=t, b=b: e.dma_start(out=t[:], in_=self.x_in[b * 128:(b + 1) * 128, :]),
                      writes=[rk])
                S.dma("sp", lambda e, t=t, b=b: e.dma_start(out=self.y[b * 128:(b + 1) * 128, :], in_=t[:]),
                      reads=[rk], writes=[("y", b)], semkey=("yst", b % 2))
                self.make_xT(t, rk, b)
            S.flush(barrier=True)

    def make_xT(self, src, rk, b, xtf=None):
        S, P = self.S, self.P
        for half in range(2):
            pb = P[6 + half]
            pk = ("ps", 6 + half)
            for kk in range(4):
                k = half * 4 + kk
                S.pe(lambda e, pb=pb, kk=kk, k=k: e.transpose(out=pb[:, kk * 128:(kk + 1) * 128],
                                                              in_=src[:, k * 128:(k + 1) * 128],
                                                              identity=self.identf[:]),
                     reads=[rk, "identf"], writes=[pk])
            dst = self.xT[:, half * 4:half * 4 + 4, b * 128:(b + 1) * 128]
            srcp = pb[:].rearrange("p (a c) -> p a c", a=4)
            if xtf is not None:
                S.dve(lambda e, half=half, srcp=srcp: e.tensor_copy(out=xtf[:, half * 4:half * 4 + 4, :], in_=srcp),
                      reads=[pk], writes=[("xtf", half)])
            S.act(lambda e, dst=dst, srcp=srcp: e.activation(out=dst, in_=srcp, func=AF.Copy),
                  reads=[pk] + ([("xtf", half)] if xtf is not None else []), writes=[("xT", b)])

    def ln_setup(self, sctx, layer, which):
        S = self.S
        g = self.inp("l%d_ln_%s_g" % (layer, which), [D])
        bta = self.inp("l%d_ln_%s_b" % (layer, which), [D])
        L = {}
        L["g"] = self.sb(sctx, "lng", [128, D], F32)
        L["b"] = self.sb(sctx, "lnb", [128, D], F32)
        S.dma("sp", lambda e: e.dma_start(out=L["g"][:], in_=g.partition_broadcast(128)), writes=["lng"])
        S.dma("sp", lambda e: e.dma_start(out=L["b"][:], in_=bta.partition_broadcast(128)), writes=["lnb"])
        L["xr"] = [self.sb(sctx, "lnxr", [128, D], F32) for _ in range(2)]
        L["s"] = [self.sb(sctx, "lns", [128, D], F32) for _ in range(2)]
        L["st"] = self.sb(sctx, "lnst", [128, 2, 2, 6], F32)
        L["mv"] = self.sb(sctx, "lnmv", [128, 2, 2], F32)
        L["sm"] = self.sb(sctx, "lnsm", [128, 2, 4], F32)
        L["n"] = 0
        return L

    def ln_load_x(self, L, b, i=None):
        S = self.S
        if i is None:
            i = L["n"] % 2
        t = L["xr"][i]
        S.dma("sp", lambda e: e.dma_start(out=t[:], in_=self.y[b * 128:(b + 1) * 128, :]),
              reads=[("y", b)], writes=[("lnxr", i)])
        return t, ("lnxr", i)

    def ln_finish(self, L, b, s, sk, router=None, i=None, part="all"):
        S = self.S
        if i is None:
            i = L["n"] % 2
            L["n"] += 1
        st, mv, sm = L["st"], L["mv"], L["sm"]
        if part in ("all", "stats"):
            self._ln_stats(L, s, sk, i)
        if part in ("all", "apply"):
            self._ln_apply(L, b, s, sk, i, router)

    def _ln_stats(self, L, s, sk, i):
        S = self.S
        st, mv, sm = L["st"], L["mv"], L["sm"]
        for h in range(2):
            S.dve(lambda e, h=h: e.bn_stats(out=st[:, i, h, :], in_=s[:, h * 512:(h + 1) * 512]),
                  reads=[sk], writes=[("lnst", i)])
        S.dve(lambda e: e.bn_aggr(out=mv[:, i, :], in_=st[:, i, :, :].rearrange("p a b -> p (a b)")),
              reads=[("lnst", i)], writes=[("lnmv", i)])
        S.act(lambda e: e.activation(out=sm[:, i, 0:1], in_=mv[:, i, 1:2], func=AF.Sqrt, bias=self.epsb[:, 0:1], scale=1.0),
              reads=[("lnmv", i), "epsb"], writes=[("lnsm0", i)])
        S.dve(lambda e: e.reciprocal(out=sm[:, i, 1:2], in_=sm[:, i, 0:1]), reads=[("lnsm0", i)], writes=[("lnsm1", i)])
        S.dve(lambda e: e.tensor_scalar(out=sm[:, i, 2:3], in0=mv[:, i, 0:1], scalar1=sm[:, i, 1:2], scalar2=-1.0,
                                        op0=ALU.mult, op1=ALU.mult),
              reads=[("lnmv", i), ("lnsm1", i)], writes=[("lnsm2", i)])

    def _ln_apply(self, L, b, s, sk, i, router):
        S = self.S
        sm = L["sm"]
        S.act(lambda e: e.activation(out=s[:], in_=s[:], func=AF.Identity, bias=sm[:, i, 2:3], scale=sm[:, i, 1:2]),
              reads=[sk, ("lnsm1", i), ("lnsm2", i)], writes=[sk])
        S.dve(lambda e: e.tensor_tensor(out=s[:], in0=s[:], in1=L["g"][:], op=ALU.mult), reads=[sk, "lng"], writes=[sk])
        S.dve(lambda e: e.tensor_tensor(out=s[:], in0=s[:], in1=L["b"][:], op=ALU.add), reads=[sk, "lnb"], writes=[sk])
        S.dma("sp", lambda e: e.dma_start(out=self.y[b * 128:(b + 1) * 128, :], in_=s[:]), reads=[sk], writes=[("y", b)], semkey=("yst", i))
        self.make_xT(s, sk, b, xtf=(router["xtf"] if router else None))
        if router:
            self.route(router, b)

    def ln_block(self, L, b, pouts, pkeys, scale_ap=None, acc=None, acck=None, i=None):
        S = self.S
        if i is None:
            i = L["n"] % 2
        s = L["s"][i]
        sk = ("lns", i)
        if acc is None:
            xr, xk = self.ln_load_x(L, b, i=i)
            for h in range(2):
                S.dve(lambda e, h=h, xr=xr: e.scalar_tensor_tensor(out=s[:, h * 512:(h + 1) * 512], in0=xr[:, h * 512:(h + 1) * 512],
                                                                   scalar=ALPHA, in1=pouts[h][:], op0=ALU.mult, op1=ALU.add),
                      reads=[xk, pkeys[h]], writes=[sk])
        else:
            for h in range(2):
                sc = 1.0 if scale_ap is None else scale_ap
                S.dve(lambda e, h=h, sc=sc: e.scalar_tensor_tensor(out=s[:, h * 512:(h + 1) * 512], in0=pouts[h][:],
                                                                   scalar=sc, in1=acc[:, h * 512:(h + 1) * 512],
                                                                   op0=ALU.mult, op1=ALU.add),
                      reads=[acck, pkeys[h], "gates"], writes=[sk])
        return s, sk

    def run_pipeline(self, its, stages, lags):
        offs = [0]
        for l in lags:
            offs.append(offs[-1] + l)
        n = len(its)
        for step in range(n + offs[-1]):
            for s in range(len(stages)):
                i = step - offs[s]
                if 0 <= i < n:
                    stages[s](its[i], i)

    def wload(self, dst, src_ap, key, reads=(), semkey=None):
        self.S.dma("pool", lambda e: e.dma_start(out=dst, in_=src_ap), reads=reads, writes=[key], semkey=semkey)

    def ffn_stage(self, sctx, layer, moe):
        S, P = self.S, self.P
        L = self.ln_setup(sctx, layer, "ffn")
        self.epsb = self.sb(sctx, "epsb", [128, 1], F32)
        S.dve(lambda e: e.memset(self.epsb[:], LN_EPS), writes=["epsb"])
        if moe:
            F = 3584
            experts = list(range(8))
            Wg = self.inp("l%d_moe_w_gate" % layer, [8, D, F])
            Wu = self.inp("l%d_moe_w_up" % layer, [8, D, F])
            Wd = self.inp("l%d_moe_w_down" % layer, [8, F, D])
            passes = [(e, j0, 7) for e in experts for j0 in (0, 7, 14, 21)]
        else:
            F = 2816
            Wg = self.inp("l%d_ffn_w_gate" % layer, [D, F])
            Wu = self.inp("l%d_ffn_w_up" % layer, [D, F])
            Wd = self.inp("l%d_ffn_w_down" % layer, [F, D])
            passes = [(None, 0, 8), (None, 8, 7), (None, 15, 7)]
        NH = 8
        acc = self.sb(sctx, "acc", [128, NB, D], F32)
        hT = self.sb(sctx, "hT", [128, NH, T], BF16)
        wd = self.sb(sctx, "wd", [128, NH, D], BF16)
        GW = 2
        wg = [self.sb(sctx, "wg", [128, 8, GW * 128], BF16) for _ in range(2)]
        wu = [self.sb(sctx, "wu", [128, 8, GW * 128], BF16) for _ in range(2)]
        sg = [self.sb(sctx, "sg", [128, 512], F32) for _ in range(2)]
        gi = 0
        si = 0
        for pi, (ex, j0, nch) in enumerate(passes):
            first, last = pi == 0, pi == len(passes) - 1
            wgs = Wg if ex is None else Wg[ex]
            wus = Wu if ex is None else Wu[ex]
            wds = Wd if ex is None else Wd[ex]
            for g0 in range(0, nch, GW):
                gn = min(GW, nch - g0)
                slot = gi % 2
                gi += 1
                c0 = (j0 + g0) * 128
                self.wload(wg[slot][:, :, 0:gn * 128], wgs[:, c0:c0 + gn * 128].rearrange("(k p) c -> p k c", p=128), ("wg", slot))
                self.wload(wu[slot][:, :, 0:gn * 128], wus[:, c0:c0 + gn * 128].rearrange("(k p) c -> p k c", p=128), ("wu", slot))
                for jj in range(g0, g0 + gn):
                    jl = jj - g0
                    for tt in range(4):
                        pg, pu = P[(si % 2)], P[2 + (si % 2)]
                        kg, ku = ("ps", si % 2), ("ps", 2 + si % 2)
                        sgt, sgk = sg[si % 2], ("sg", si % 2)
                        si += 1
                        xr = [("xT", tt * 4 + q) for q in range(4)]
                        for k in range(8):
                            S.pe(lambda e, pg=pg, k=k, slot=slot, jl=jl, tt=tt: e.matmul(
                                pg[:], lhsT=wg[slot][:, k, jl * 128:(jl + 1) * 128], rhs=self.xT[:, k, tt * 512:(tt + 1) * 512],
                                start=(k == 0), stop=(k == 7)), reads=[("wg", slot)] + xr, writes=[kg])
                        for k in range(8):
                            S.pe(lambda e, pu=pu, k=k, slot=slot, jl=jl, tt=tt: e.matmul(
                                pu[:], lhsT=wu[slot][:, k, jl * 128:(jl + 1) * 128], rhs=self.xT[:, k, tt * 512:(tt + 1) * 512],
                                start=(k == 0), stop=(k == 7)), reads=[("wu", slot)] + xr, writes=[ku])
                        S.act(lambda e, pg=pg, sgt=sgt: e.activation(out=sgt[:], in_=pg[:], func=AF.Silu),
                              reads=[kg], writes=[sgk])
                        S.dve(lambda e, pu=pu, sgt=sgt, jj=jj, tt=tt: e.tensor_tensor(
                            out=hT[:, jj, tt * 512:(tt + 1) * 512], in0=sgt[:], in1=pu[:], op=ALU.mult),
                            reads=[sgk, ku], writes=[("hT", jj, tt)])
            for jj in range(nch):
                r0 = (j0 + jj) * 128
                self.wload(wd[:, jj, :], wds[r0:r0 + 128, :], ("wd", jj))
            for b in range(NB):
                po = [P[4 + 2 * (b % 2)], P[5 + 2 * (b % 2)]]
                pk = [("ps", 4 + 2 * (b % 2)), ("ps", 5 + 2 * (b % 2))]
                for jj in range(nch):
                    for h in range(2):
                        S.pe(lambda e, jj=jj, h=h, b=b, po=po: e.matmul(
                            po[h][:], lhsT=hT[:, jj, b * 128:(b + 1) * 128], rhs=wd[:, jj, h * 512:(h + 1) * 512],
                            start=(jj == 0), stop=(jj == nch - 1)),
                            reads=[("hT", jj, b // 4), ("wd", jj)], writes=[pk[h]])
                gate = None if ex is None else self.gates[:, b, ex:ex + 1]
                acck = ("acc", b)
                if first:
                    xr, xk = self.ln_load_x(L, b)
                    L["n"] += 1
                    for h in range(2):
                        if gate is None:
                            S.dve(lambda e, h=h, xr=xr, b=b, po=po: e.scalar_tensor_tensor(
                                out=acc[:, b, h * 512:(h + 1) * 512], in0=xr[:, h * 512:(h + 1) * 512], scalar=ALPHA,
                                in1=po[h][:], op0=ALU.mult, op1=ALU.add), reads=[xk, pk[h]], writes=[acck])
                        else:
                            S.act(lambda e, h=h, xr=xr, b=b: e.activation(out=acc[:, b, h * 512:(h + 1) * 512],
                                                                          in_=xr[:, h * 512:(h + 1) * 512], func=AF.Copy, scale=ALPHA),
                                  reads=[xk], writes=[acck])
                            S.dve(lambda e, h=h, b=b, po=po, gate=gate: e.scalar_tensor_tensor(
                                out=acc[:, b, h * 512:(h + 1) * 512], in0=po[h][:], scalar=gate,
                                in1=acc[:, b, h * 512:(h + 1) * 512], op0=ALU.mult, op1=ALU.add),
                                reads=[acck, pk[h], "gates"], writes=[acck])
                elif not last:
                    for h in range(2):
                        S.dve(lambda e, h=h, b=b, po=po, gate=gate: e.scalar_tensor_tensor(
                            out=acc[:, b, h * 512:(h + 1) * 512], in0=po[h][:], scalar=(1.0 if gate is None else gate),
                            in1=acc[:, b, h * 512:(h + 1) * 512], op0=ALU.mult, op1=ALU.add),
                            reads=[acck, pk[h], "gates"], writes=[acck])
                else:
                    s, sk = self.ln_block(L, b, po, pk, scale_ap=gate, acc=acc[:, b, :], acck=acck)
                    self.ln_finish(L, b, s, sk, router=self.next_router(layer, "ffn"))

    def moe_stage(self, sctx, layer):
        S, P, nc = self.S, self.P, self.nc
        import os
        NSB = int(os.environ.get("KDBG_NSB", "5"))
        CS = NSB * 128
        tiles = [(0, min(512, CS))] + ([(512, CS - 512)] if CS > 512 else [])
        F = 3584
        Wg = self.inp("l%d_moe_w_gate" % layer, [8, D, F])
        Wu = self.inp("l%d_moe_w_up" % layer, [8, D, F])
        Wd = self.inp("l%d_moe_w_down" % layer, [8, F, D])
        self.epsb = self.sb(sctx, "epsb", [128, 1], F32)
        S.dve(lambda e: e.memset(self.epsb[:], LN_EPS), writes=["epsb"])
        acc = self.sb(sctx, "acc", [128, NB, D], F32)
        with ExitStack() as mx:
            xtok = self.sb(mx, "xtok", [128, NB, D], BF16)
            flat = self.xT[:].rearrange("p k t -> p (k t)")
            xgT = flat[:, 0:8 * CS].rearrange("p (k c) -> p k c", k=8)
            hT2 = flat[:, 8 * CS:15 * CS].rearrange("p (k c) -> p k c", k=7)
            yb = flat[:, 15 * CS:15 * CS + NSB * D].rearrange("p (s d) -> p s d", s=NSB)
            Sx = self.sb(mx, "Sx", [128, NB, CS], BF16)
            STr = [self.sb(mx, "STr", [128, NSB, 512], BF16) for _ in range(1)]
            wd = self.sb(mx, "wd", [128, 7, D], BF16)
            wg = [self.sb(mx, "wg", [128, 8, 128], BF16) for _ in range(2)]
            wu = [self.sb(mx, "wu", [128, 8, 128], BF16) for _ in range(2)]
            sg = [self.sb(mx, "sg", [128, 512], F32) for _ in range(1)]
            yacc = self.sb(mx, "yacc", [128, NSB, D], F32)
            maskf = self.sb(mx, "maskf", [128, NB, 8], F32)
            maskb = self.sb(mx, "maskb", [128, NB, 32], BF16)
            nmb = self.sb(mx, "nmb", [128, NB, 8], BF16)
            rk = self.sb(mx, "rk", [128, NB, 8], F32)
            ioi = self.sb(mx, "ioi", [128, CS], mybir.dt.int32)
            cii = self.sb(mx, "cii", [128, NSB], mybir.dt.int32)
            cidx = self.sb(mx, "cidx", [128, NSB], F32)
            io = ioi
            rk2 = self.sb(mx, "rk2", [128, NB, 8], F32)
            cid2 = self.sb(mx, "cid2", [128, NSB], F32)
            cnti = self.sb(mx, "cnti", [128, 32], mybir.dt.int32)
            cntD = nc.dram_tensor("cntD%d" % layer, [1, 32], mybir.dt.int32, kind="Internal").ap()
            UW = self.sb(mx, "UW", [128, 896], BF16)
            ones5 = self.onesb[:, 0:1].to_broadcast([128, 512])
            for b in range(NB):
                self.wload(xtok[:, b, :], self.y[b * 128:(b + 1) * 128, :], ("xtok", b), semkey="xtokall")
                S.dma("sp", lambda e, b=b: e.dma_start(out=acc[:, b, :], in_=self.y[b * 128:(b + 1) * 128, :]), writes=[("accld", b)],
                      semkey="accld")
            for b in range(NB):
                S.act(lambda e, b=b: e.activation(out=acc[:, b, :], in_=acc[:, b, :], func=AF.Copy, scale=ALPHA),
                      reads=[("accld", bb) for bb in range(NB)], writes=[("acc", b)])
            S.dve(lambda e: e.tensor_scalar(out=maskf[:], in0=self.gates[:], scalar1=0.0, scalar2=None, op0=ALU.is_gt),
                  reads=["gates"], writes=["maskf"])
            S.dve(lambda e: e.memset(maskb[:], 0.0), writes=["maskb"])
            S.dve(lambda e: e.tensor_copy(out=maskb[:, :, 0:8], in_=maskf[:]), reads=["maskf", "maskb"], writes=["maskb"])
            S.dve(lambda e: e.tensor_scalar(out=nmb[:], in0=maskf[:], scalar1=-4096.0, scalar2=4096.0, op0=ALU.mult, op1=ALU.add),
                  reads=["maskf"], writes=["nmb"])
            S.pool(lambda e: e.iota(out=ioi[:], pattern=[[1, CS]], base=0, channel_multiplier=0), writes=["ioi"])
            S.dve(lambda e: e.memset(cidx[:], 0.0), reads=["ioi"], writes=["io"])
            S.pool(lambda e: e.iota(out=cii[:], pattern=[[128, NSB]], base=0, channel_multiplier=1), writes=["cii"])
            S.dve(lambda e: e.tensor_copy(out=cidx[:], in_=cii[:]), reads=["cii"], writes=["cidx"])
            S.dve(lambda e: e.memset(UW[:, 0:384], 0.0), writes=["U4"])
            S.dve(lambda e: e.tensor_copy(out=UW[:, 384:512], in_=self.mstrict[:]), reads=["mstrict", "U4"], writes=["U4"])
            S.dve(lambda e: e.memset(UW[:, 512:896], 1.0), reads=["U4"], writes=["U4"])
            for b in range(NB):
                pr, prk = P[b % 2], ("ps", b % 2)
                for b2 in range(b + 1):
                    lhs = self.onesb[:] if b2 < b else self.mstrict[:]
                    S.pe(lambda e, b2=b2, lhs=lhs, pr=pr, b=b: e.matmul(pr[:, 0:32], lhsT=lhs, rhs=maskb[:, b2, :], start=(b2 == 0), stop=(b2 == b)),
                         reads=["maskb", "onesb", "mstrict"], writes=[prk])
                S.dve(lambda e, b=b, pr=pr: e.tensor_copy(out=rk[:, b, :], in_=pr[:, 0:8]), reads=[prk], writes=["rk"])
            for b in range(NB):
                S.pe(lambda e, b=b: e.matmul(P[2][:, 0:32], lhsT=self.onesb[:], rhs=maskb[:, b, :], start=(b == 0), stop=(b == NB - 1)),
                     reads=["maskb", "onesb"], writes=[("ps", 2)])
            S.dve(lambda e: e.tensor_copy(out=cnti[:], in_=P[2][:, 0:32]), reads=[("ps", 2)], writes=["cnti"])
            S.dma("sp", lambda e: e.dma_start(out=cntD, in_=cnti[0:1, :]), reads=["cnti"], writes=["cntD"])
            gi = 0
            si = 0
            gn = 0
            NROUND = (T + CS - 1) // CS
            for ex in range(8):
                for rnd in range(NROUND):
                    R0 = rnd * CS
                    if rnd == 0:
                        rkR, cidR = rk, cidx
                    else:
                        if rnd == 1:
                            for en in ("pe", "act", "dve", "pool", "sp"):
                                S.add(en, lambda e: e.nop(), reads=["cntD"])
                            S.ctl("if", ap=cntD[0:1, ex:ex + 1], thresh=CS)
                        rkR, cidR = rk2, cid2
                        S.dve(lambda e, R0=R0: e.tensor_scalar(out=rk2[:], in0=rk[:], scalar1=float(-R0), scalar2=None, op0=ALU.add),
                              reads=["rk"], writes=["rk2"])
                        S.dve(lambda e, R0=R0: e.tensor_scalar(out=cid2[:], in0=cidx[:], scalar1=float(R0), scalar2=None, op0=ALU.add),
                              reads=["cidx"], writes=["cid2"])
                    for b in range(NB):
                        S.dve(lambda e, b=b, ex=ex, rkR=rkR: e.tensor_scalar(out=Sx[:, b, :], in0=io[:], scalar1=rkR[:, b, ex:ex + 1],
                                                                    scalar2=maskf[:, b, ex:ex + 1], op0=ALU.is_equal, op1=ALU.mult),
                              reads=["io", "rk", "rk2", "maskf"], writes=[("Sx", b)])
                    for k in range(8):
                        for (c0, cw) in tiles:
                            pgk = gn % 2
                            gn += 1
                            pg, pgkk = P[pgk], ("ps", pgk)
                            for b in range(NB):
                                S.pe(lambda e, b=b, k=k, c0=c0, cw=cw, pg=pg: e.matmul(pg[:, 0:cw], lhsT=xtok[:, b, k * 128:(k + 1) * 128],
                                                                                      rhs=Sx[:, b, c0:c0 + cw], start=(b == 0), stop=(b == NB - 1)),
                                     reads=[("xtok", bb) for bb in range(NB)] + [("Sx", b)], writes=[pgkk])
                            S.act(lambda e, k=k, c0=c0, cw=cw, pg=pg: e.activation(out=xgT[:, k, c0:c0 + cw], in_=pg[:, 0:cw], func=AF.Copy),
                                  reads=[pgkk], writes=[("xgT", k)])
                    xgk = [("xgT", k) for k in range(8)]
                    for pi, j0 in enumerate((0, 7, 14, 21)):
                        nch = 7
                        firstp, lastp = pi == 0, pi == 3
                        for jj in range(nch):
                            slot = gi % 2
                            gi += 1
                            c0 = (j0 + jj) * 128
                            self.wload(wg[slot][:], Wg[ex][:, c0:c0 + 128].rearrange("(k p) c -> p k c", p=128), ("wg", slot))
                            self.wload(wu[slot][:], Wu[ex][:, c0:c0 + 128].rearrange("(k p) c -> p k c", p=128), ("wu", slot))
                            for (t0, tw) in tiles:
                                pg, pu = P[(si % 2)], P[2 + (si % 2)]
                                kg, ku = ("ps", si % 2), ("ps", 2 + si % 2)
                                sgt, sgk = sg[0], ("sg", 0)
                                si += 1
                                for k in range(8):
                                    S.pe(lambda e, pg=pg, k=k, slot=slot, t0=t0, tw=tw: e.matmul(
                                        pg[:, 0:tw], lhsT=wg[slot][:, k, :], rhs=xgT[:, k, t0:t0 + tw], start=(k == 0), stop=(k == 7)),
                                        reads=[("wg", slot)] + xgk, writes=[kg])
                                for k in range(8):
                                    S.pe(lambda e, pu=pu, k=k, slot=slot, t0=t0, tw=tw: e.matmul(
                                        pu[:, 0:tw], lhsT=wu[slot][:, k, :], rhs=xgT[:, k, t0:t0 + tw], start=(k == 0), stop=(k == 7)),
                                        reads=[("wu", slot)] + xgk, writes=[ku])
                                S.act(lambda e, pg=pg, sgt=sgt, tw=tw: e.activation(out=sgt[:, 0:tw], in_=pg[:, 0:tw], func=AF.Silu),
                                      reads=[kg], writes=[sgk])
                                S.dve(lambda e, pu=pu, sgt=sgt, jj=jj, t0=t0, tw=tw: e.tensor_tensor(
                                    out=hT2[:, jj, t0:t0 + tw], in0=sgt[:, 0:tw], in1=pu[:, 0:tw], op=ALU.mult),
                                    reads=[sgk, ku], writes=[("hT2", jj)])
                        for jj in range(nch):
                            r0 = (j0 + jj) * 128
                            self.wload(wd[:, jj, :], Wd[ex][r0:r0 + 128, :], ("wd", jj))
                        for sbk in range(NSB):
                            po = [P[4 + 2 * (sbk % 2)], P[5 + 2 * (sbk % 2)]]
                            pk = [("ps", 4 + 2 * (sbk % 2)), ("ps", 5 + 2 * (sbk % 2))]
                            for jj in range(nch):
                                for h in range(2):
                                    S.pe(lambda e, jj=jj, h=h, sbk=sbk, po=po: e.matmul(
                                        po[h][:], lhsT=hT2[:, jj, sbk * 128:(sbk + 1) * 128], rhs=wd[:, jj, h * 512:(h + 1) * 512],
                                        start=(jj == 0), stop=(jj == nch - 1)), reads=[("hT2", jj), ("wd", jj)], writes=[pk[h]])
                            for h in range(2):
                                if firstp:
                                    S.act(lambda e, h=h, sbk=sbk, po=po: e.activation(out=yacc[:, sbk, h * 512:(h + 1) * 512], in_=po[h][:], func=AF.Copy),
                                          reads=[pk[h]], writes=[("yacc", sbk)])
                                elif not lastp:
                                    S.dve(lambda e, h=h, sbk=sbk, po=po: e.tensor_tensor(out=yacc[:, sbk, h * 512:(h + 1) * 512], in0=yacc[:, sbk, h * 512:(h + 1) * 512],
                                                                                       in1=po[h][:], op=ALU.add), reads=[pk[h], ("yacc", sbk)], writes=[("yacc", sbk)])
                                else:
                                    S.dve(lambda e, h=h, sbk=sbk, po=po: e.tensor_tensor(out=yb[:, sbk, h * 512:(h + 1) * 512], in0=yacc[:, sbk, h * 512:(h + 1) * 512],
                                                                                       in1=po[h][:], op=ALU.add), reads=[pk[h], ("yacc", sbk)], writes=[("yb", sbk)])
                    ybk = [("yb", s_) for s_ in range(NSB)]
                    for tq in range(4):
                        prb, prbk = P[0], ("ps", 0)
                        nb2 = 4 * tq + 4
                        for b2 in range(nb2):
                            rhs = ones5 if b2 < 4 * tq else UW[:, (3 - (b2 - 4 * tq)) * 128:(3 - (b2 - 4 * tq)) * 128 + 512]
                            S.pe(lambda e, b2=b2, rhs=rhs, ex=ex: e.matmul(prb[:], lhsT=maskb[:, b2, ex:ex + 1].to_broadcast([128, 128]), rhs=rhs,
                                                                          start=(b2 == 0), stop=False),
                                 reads=["maskb", "onesb", "U4"], writes=[prbk])
                        for q in range(4):
                            b2 = 4 * tq + q
                            S.pe(lambda e, b2=b2, q=q, ex=ex: e.matmul(prb[:, q * 128:(q + 1) * 128], lhsT=nmb[:, b2, ex:ex + 1].to_broadcast([128, 128]),
                                                                      rhs=self.identb[:], start=False, stop=(q == 3)),
                                 reads=["nmb", "identb"], writes=[prbk])
                        st_ = STr[0]
                        for sbk in range(NSB):
                            S.dve(lambda e, sbk=sbk, cidR=cidR: e.tensor_scalar(out=st_[:, sbk, :], in0=prb[:], scalar1=cidR[:, sbk:sbk + 1], scalar2=None,
                                                                     op0=ALU.is_equal), reads=[prbk, "cidx", "cid2"], writes=[("STr", sbk)])
                        for q in range(4):
                            b = 4 * tq + q
                            po = [P[4 + 2 * (b % 2)], P[5 + 2 * (b % 2)]]
                            pk = [("ps", 4 + 2 * (b % 2)), ("ps", 5 + 2 * (b % 2))]
                            for sbk in range(NSB):
                                for h in range(2):
                                    S.pe(lambda e, sbk=sbk, h=h, q=q, po=po: e.matmul(po[h][:], lhsT=st_[:, sbk, q * 128:(q + 1) * 128],
                                                                                  rhs=yb[:, sbk, h * 512:(h + 1) * 512],
                                                                                  start=(sbk == 0), stop=(sbk == NSB - 1)),
                                         reads=[("STr", sbk), ("yb", sbk)], writes=[pk[h]])
                            gate = self.gates[:, b, ex:ex + 1]
                            for h in range(2):
                                S.dve(lambda e, h=h, b=b, po=po, gate=gate: e.scalar_tensor_tensor(
                                    out=acc[:, b, h * 512:(h + 1) * 512], in0=po[h][:], scalar=gate,
                                    in1=acc[:, b, h * 512:(h + 1) * 512], op0=ALU.mult, op1=ALU.add),
                                    reads=[("acc", b), pk[h], "gates"], writes=[("acc", b)])

                    if rnd == NROUND - 1:
                        S.ctl("endif")
            S.flush(barrier=True)
        L = self.ln_setup(sctx, layer, "ffn")
        self.run_pipeline(list(range(NB)),
                          [lambda b, n: self.ln_finish(L, b, acc[:, b, :], ("acc", b), i=b % 2, part="stats"),
                           lambda b, n: self.ln_finish(L, b, acc[:, b, :], ("acc", b), i=b % 2, part="apply")], [1])

    def next_router(self, layer, which):
        import os
        if os.environ.get("KDBG_NOROUTER"):
            return None
        return self.router if (which == "mix" and layer % 2 == 1) else None

    def router_setup(self, sctx, layer):
        S = self.S
        R = {}
        wr = self.inp("l%d_moe_w_router" % layer, [D, 8])
        R["wr"] = self.sb(sctx, "wr", [128, 8, 8], F32)
        with self.nc.allow_non_contiguous_dma(reason="tiny router weight"):
            S.dma("sp", lambda e: e.dma_start(out=R["wr"][:], in_=wr.rearrange("(k p) e -> p k e", p=128)), writes=["wr"])
        R["xtf"] = self.sb(sctx, "xtf", [128, 8, 128], F32)
        R["xh"] = self.sb(sctx, "xh", [128, 8, 128], BF16)
        R["xl"] = self.sb(sctx, "xl", [128, 8, 128], BF16)
        R["wrh"] = self.sb(sctx, "wrh", [128, 8, 32], BF16)
        R["wrl"] = self.sb(sctx, "wrl", [128, 8, 32], BF16)
        S.dve(lambda e: e.memset(R["wrh"][:], 0.0), writes=["wrh"])
        S.dve(lambda e: e.memset(R["wrl"][:], 0.0), writes=["wrl"])
        R["t"] = self.sb(sctx, "rt", [128, 8, 8], F32)
        S.dve(lambda e: e.tensor_copy(out=R["wrh"][:, :, 0:8], in_=R["wr"][:]), reads=["wr", "wrh"], writes=["wrh"])
        S.dve(lambda e: e.tensor_tensor(out=R["wrl"][:, :, 0:8], in0=R["wr"][:], in1=R["wrh"][:, :, 0:8], op=ALU.subtract),
              reads=["wr", "wrh", "wrl"], writes=["wrl"])
        self.router = R
        return R

    def route(self, R, b):
        S, P = self.S, self.P
        pr, pk = P[5], ("ps", 5)
        t = R["t"]
        xk = [("xtf", 0), ("xtf", 1)]
        S.dve(lambda e: e.tensor_copy(out=R["xh"][:], in_=R["xtf"][:]), reads=xk, writes=["xh"])
        S.dve(lambda e: e.tensor_tensor(out=R["xl"][:], in0=R["xtf"][:], in1=R["xh"][:], op=ALU.subtract),
              reads=xk + ["xh"], writes=["xl"])
        combos = [("xh", "wrh"), ("xh", "wrl"), ("xl", "wrh")]
        n = 0
        for k in range(8):
            for (xa, wa) in combos:
                S.pe(lambda e, k=k, xa=xa, wa=wa, n=n: e.matmul(pr[:, 0:32], lhsT=R[xa][:, k, :], rhs=R[wa][:, k, :],
                                                              start=(n == 0), stop=(n == 23)),
                     reads=[xa, wa], writes=[pk])
                n += 1
        lg, mask, ex = t[:, 0, :], t[:, 2, :], t[:, 4, :]
        m1, m2, nm1, den = t[:, 1, 0:1], t[:, 1, 1:2], t[:, 3, 0:1], t[:, 5, 0:1]
        w4, w2, lg2 = t[:, 6, 0:4], t[:, 6, 4:6], t[:, 7, :]
        S.dve(lambda e: e.tensor_copy(out=lg, in_=pr[:, 0:8]), reads=[pk], writes=["r_lg"])

        def max8(src_ap, dst, key_in, key_out):
            S.dve(lambda e: e.tensor_tensor(out=w4, in0=src_ap[:, 0:4], in1=src_ap[:, 4:8], op=ALU.max), reads=[key_in], writes=["r_w4"])
            S.dve(lambda e: e.tensor_tensor(out=w2, in0=t[:, 6, 0:2], in1=t[:, 6, 2:4], op=ALU.max), reads=["r_w4"], writes=["r_w2"])
            S.dve(lambda e: e.tensor_tensor(out=dst, in0=t[:, 6, 4:5], in1=t[:, 6, 5:6], op=ALU.max), reads=["r_w2"], writes=[key_out])
        max8(lg, m1, "r_lg", "r_m1")
        S.dve(lambda e: e.tensor_scalar(out=lg2, in0=lg, scalar1=m1, scalar2=-1e30, op0=ALU.is_equal, op1=ALU.mult),
              reads=["r_lg", "r_m1"], writes=["r_lg2"])
        S.dve(lambda e: e.tensor_tensor(out=lg2, in0=lg2, in1=lg, op=ALU.add), reads=["r_lg2", "r_lg"], writes=["r_lg2"])
        max8(lg2, m2, "r_lg2", "r_m2")
        S.dve(lambda e: e.tensor_scalar(out=mask, in0=lg, scalar1=m2, scalar2=None, op0=ALU.is_ge),
              reads=["r_lg", "r_m2"], writes=["r_mask"])
        S.dve(lambda e: e.tensor_scalar(out=nm1, in0=m1, scalar1=-1.0, scalar2=None, op0=ALU.mult),
              reads=["r_m1"], writes=["r_nm1"])
        S.act(lambda e: e.activation(out=ex, in_=lg, func=AF.Exp, bias=nm1, scale=1.0), reads=["r_lg", "r_nm1"], writes=["r_ex"])
        S.dve(lambda e: e.tensor_tensor(out=ex, in0=ex, in1=mask, op=ALU.mult), reads=["r_ex", "r_mask"], writes=["r_ex"])
        S.dve(lambda e: e.tensor_tensor(out=w4, in0=t[:, 4, 0:4], in1=t[:, 4, 4:8], op=ALU.add), reads=["r_ex"], writes=["r_w4"])
        S.dve(lambda e: e.tensor_tensor(out=w2, in0=t[:, 6, 0:2], in1=t[:, 6, 2:4], op=ALU.add), reads=["r_w4"], writes=["r_w2"])
        S.dve(lambda e: e.tensor_tensor(out=den, in0=t[:, 6, 4:5], in1=t[:, 6, 5:6], op=ALU.add), reads=["r_w2"], writes=["r_den"])
        S.dve(lambda e: e.reciprocal(out=den, in_=den), reads=["r_den"], writes=["r_den"])
        S.dve(lambda e: e.tensor_scalar(out=self.gates[:, b, :], in0=ex, scalar1=den, scalar2=None, op0=ALU.mult),
              reads=["r_ex", "r_den"], writes=["gates"])

    def mixer_out(self, sctx, layer, lhs_fn, lhs_keys_fn, nk, Wo_ap, L, pre_block=None):
        S, P = self.S, self.P
        wo = self.sb(sctx, "wo", [128, nk, D], BF16)
        for k in range(nk):
            self.wload(wo[:, k, :], Wo_ap[k * 128:(k + 1) * 128, :], ("wo", k), semkey=("wo", k % 4))
        rt = self.next_router(layer, "mix")
        hold = {}

        def stA(b, n):
            po = [P[2 * (b % 2)], P[1 + 2 * (b % 2)]]
            pk = [("ps", 2 * (b % 2)), ("ps", 1 + 2 * (b % 2))]
            if pre_block is not None:
                pre_block(b)
            for k in range(nk):
                for h in range(2):
                    S.pe(lambda e, k=k, h=h, b=b, po=po: e.matmul(po[h][:], lhsT=lhs_fn(k, b), rhs=wo[:, k, h * 512:(h + 1) * 512],
                                                              start=(k == 0), stop=(k == nk - 1)),
                         reads=lhs_keys_fn(k, b) + [("wo", kk) for kk in range(k % 4, nk, 4)], writes=[pk[h]])
            s, sk = self.ln_block(L, b, po, pk, i=b % 2)
            self.ln_finish(L, b, s, sk, i=b % 2, part="stats")
            hold[b] = (s, sk)

        def stB(b, n):
            s, sk = hold.pop(b)
            self.ln_finish(L, b, s, sk, router=rt, i=b % 2, part="apply")
        self.run_pipeline(list(range(NB)), [stA, stB], [1])

    def attn_stage(self, sctx, layer, kind):
        S, P = self.S, self.P
        fox = kind == "fox"
        L = self.ln_setup(sctx, layer, "mix")
        self.epsb = self.sb(sctx, "epsb", [128, 1], F32)
        S.dve(lambda e: e.memset(self.epsb[:], LN_EPS), writes=["epsb"])
        if layer % 2 == 1:
            self.router_setup(sctx, layer)
        if fox:
            Wq = self.inp("l%d_fox_w_qkvf" % layer, [D, 3088])
            Wo = self.inp("l%d_fox_w_out" % layer, [D, D])
            bf = self.inp("l%d_fox_b_f" % layer, [16])
        else:
            Wq = self.inp("l%d_sb_w_qkv" % layer, [D, 3072])
            Wo = self.inp("l%d_sb_w_out" % layer, [D, D])
        OT = self.sb(sctx, "OT", [128, 8, T], BF16)
        qT = [self.sb(sctx, "qT", [128, T], BF16) for _ in range(2)]
        kT = [None, None]
        vv = [self.sb(sctx, "vv", [128, NB, 128], BF16) for _ in range(2)]
        wq = [self.sb(sctx, "wq", [128, 8, 384], BF16) for _ in range(2)]
        E = [self.sb(sctx, "E", [128, 512], BF16) for _ in range(4)]
        if fox:
            wf = self.sb(sctx, "wf", [128, 8, 16], BF16)
            nbf = self.sb(sctx, "nbf", [16, 1], F32)
            cnT = self.sb(sctx, "cnT", [128, NB, 16], F32)
            c3 = [self.sb(sctx, "c3", [128, 2 * T], BF16) for _ in range(2)]
            rl = self.sb(sctx, "rl", [128, 512], F32)
            kTp = [[self.sb(sctx, "kTp", [128, T], BF16) for _ in range(2)] for _ in range(2)]
            ones3p = self.sb(sctx, "ones3p", [128, 128], BF16)
            fpx = ExitStack()
            spf = self.sb(fpx, "spf", [16, T], F32)
            cn = self.sb(fpx, "cn", [16, T], F32)
            tmpf = self.sb(fpx, "tmpf", [16, T], F32)
            c3b = self.sb(fpx, "c3b", [16, 3, T], BF16)
            S.dve(lambda e: e.memset(ones3p[:], 0.0), writes=["ones3p"])
            S.dve(lambda e: e.memset(ones3p[0:3, :], 1.0), reads=["ones3p"], writes=["ones3p"])
            for i_ in range(2):
                S.dve(lambda e, i_=i_: e.memset(c3[i_][:], 0.0), writes=[("c3", i_)])
                for j_ in range(2):
                    S.dve(lambda e, i_=i_, j_=j_: e.memset(kTp[i_][j_][:], 0.0), writes=[("kTpz", i_, j_)])
            with self.nc.allow_non_contiguous_dma(reason="tiny"):
                self.wload(wf[:], Wq[:, 3072:3088].rearrange("(k p) c -> p k c", p=128), "wf")
                S.dma("sp", lambda e: e.dma_start(out=nbf[:], in_=bf.rearrange("(p o) -> p o", o=1)), writes=["nbf"])
            S.dve(lambda e: e.tensor_scalar(out=nbf[:], in0=nbf[:], scalar1=-1.0, scalar2=None, op0=ALU.mult),
                  reads=["nbf"], writes=["nbf"])
            for tt in range(4):
                pf, pfk = P[6 + tt % 2], ("ps", 6 + tt % 2)
                for k in range(8):
                    S.pe(lambda e, k=k, tt=tt, pf=pf: e.matmul(pf[0:16, :], lhsT=wf[:, k, :], rhs=self.xT[:, k, tt * 512:(tt + 1) * 512],
                                                            start=(k == 0), stop=(k == 7)),
                         reads=["wf"] + [("xT", tt * 4 + q) for q in range(4)], writes=[pfk])
                S.act(lambda e, tt=tt, pf=pf: e.activation(out=tmpf[:, tt * 512:(tt + 1) * 512], in_=pf[0:16, :], func=AF.Exp,
                                                        bias=nbf[:, 0:1], scale=-1.0), reads=[pfk, "nbf"], writes=[("tmpf", tt)])
                S.act(lambda e, tt=tt: e.activation(out=spf[:, tt * 512:(tt + 1) * 512], in_=tmpf[:, tt * 512:(tt + 1) * 512],
                                                    func=AF.Ln, bias=self.oneb[0:16, 0:1], scale=1.0),
                      reads=[("tmpf", tt), "oneb"], writes=[("spf", tt)])
            allsp = [("spf", tt) for tt in range(4)]
            S.dve(lambda e: e.memset(tmpf[:], 1.0), writes=[("tmpf", tt) for tt in range(4)] + ["m8"])
            S.dve(lambda e: e.tensor_tensor_scan(out=cn[:], data0=tmpf[:], data1=spf[:], initial=0.0, op0=ALU.mult, op1=ALU.add),
                  reads=allsp + ["m8"], writes=["cn"])
            for b in range(NB):
                pt, ptk = P[6 + b % 2], ("ps", 6 + b % 2)
                S.pe(lambda e, b=b, pt=pt: e.transpose(out=pt[:, 0:16], in_=cn[:, b * 128:(b + 1) * 128], identity=self.identf[0:16, 0:16]),
                     reads=["cn", "identf"], writes=[ptk])
                S.dve(lambda e, b=b, pt=pt: e.tensor_copy(out=cnT[:, b, :], in_=pt[:, 0:16]), reads=[ptk], writes=["cnT"])
            S.dve(lambda e: e.tensor_scalar(out=tmpf[:], in0=cn[:], scalar1=-8.0, scalar2=None, op0=ALU.mult),
                  reads=["cn"] + [("tmpf", tt) for tt in range(4)], writes=["m8"])
            for i in range(3):
                S.dve(lambda e, i=i: e.tensor_copy(out=c3b[:, i, :], in_=tmpf[:]), reads=["m8"], writes=[("c3b", i)])
                if i < 2:
                    S.dve(lambda e, i=i: e.tensor_tensor(out=tmpf[:], in0=tmpf[:], in1=c3b[:, i, :], op=ALU.subtract),
                          reads=["m8", ("c3b", i)], writes=["m8"])
            S.dma("sp", lambda e: e.dma_start(out=self.scr[0, :, :].rearrange("i (h t) -> h i t", h=16), in_=c3b[:]),
                  reads=[("c3b", i) for i in range(3)], writes=["scr0"])
            S.flush(barrier=True)
            fpx.close()
        else:
            zs = [self.sb(sctx, "zs", [128, 512], F32) for _ in range(3)]
            ez = [self.sb(sctx, "ez", [128, 512], F32) for _ in range(2)]
            spb = [self.sb(sctx, "spb", [128, 512], BF16) for _ in range(3)]
            lw = [self.sb(sctx, "lw", [128, 512], F32) for _ in range(2)]
            zerob = self.sb(sctx, "zerob", [128, 128], BF16)
            S.dve(lambda e: e.memset(zerob[:], 0.0), writes=["zerob"])
            kTp = [[self.sb(sctx, "kTp", [128, T], BF16) for _ in range(2)] for _ in range(2)]
            for i_ in range(2):
                for j_ in range(2):
                    S.dve(lambda e, i_=i_, j_=j_: e.memset(kTp[i_][j_][:], 0.0), writes=[("kTpz", i_, j_)])
        self.oneb_needed = True
        ei = 0
        for c in range(8):
            sl = c % 2
            for i, off in enumerate((0, 1024, 2048)):
                self.wload(wq[sl][:, :, i * 128:(i + 1) * 128],
                           Wq[:, off + c * 128:off + (c + 1) * 128].rearrange("(k p) c -> p k c", p=128), ("wq", sl, i))
            if fox:
                S.dma("sp", lambda e, sl=sl, c=c: e.dma_start(out=c3[sl][0:3, :], in_=self.scr[0, :, 2 * c * T:(2 * c + 2) * T]),
                      reads=["scr0"], writes=[("c3", sl)])
            for i, dst in enumerate((qT[sl], kT[sl])):
                nm = ("qT", "kT")[i]
                for tt in range(4):
                    pp, ppk = P[6 + tt % 2], ("ps", 6 + tt % 2)
                    for k in range(8):
                        S.pe(lambda e, k=k, tt=tt, pp=pp, i=i, sl=sl: e.matmul(pp[:], lhsT=wq[sl][:, k, i * 128:(i + 1) * 128],
                                                                          rhs=self.xT[:, k, tt * 512:(tt + 1) * 512],
                                                                          start=(k == 0), stop=(k == 7)),
                             reads=[("wq", sl, i)] + [("xT", tt * 4 + q) for q in range(4)], writes=[ppk])
                    if i == 1:
                        S.act(lambda e, tt=tt, pp=pp, sl=sl: e.activation(out=kTp[sl][0][0:64, tt * 512:(tt + 1) * 512], in_=pp[0:64, :], func=AF.Copy),
                              reads=[ppk, ("kTpz", sl, 0)], writes=[(nm, sl, tt)])
                        S.act(lambda e, tt=tt, pp=pp, sl=sl: e.activation(out=kTp[sl][1][64:128, tt * 512:(tt + 1) * 512], in_=pp[64:128, :], func=AF.Copy),
                              reads=[ppk, ("kTpz", sl, 1)], writes=[(nm, sl, tt)])
                    else:
                        S.act(lambda e, dst=dst, tt=tt, pp=pp: e.activation(out=dst[:, tt * 512:(tt + 1) * 512], in_=pp[:], func=AF.Copy),
                              reads=[ppk], writes=[(nm, sl, tt)])
            for bq in range(4):
                pp, ppk = P[6 + bq % 2], ("ps", 6 + bq % 2)
                for q in range(4):
                    b = bq * 4 + q
                    for k in range(8):
                        S.pe(lambda e, k=k, b=b, q=q, pp=pp, sl=sl: e.matmul(pp[:, q * 128:(q + 1) * 128], lhsT=self.xT[:, k, b * 128:(b + 1) * 128],
                                                                        rhs=wq[sl][:, k, 256:384], start=(k == 0), stop=(k == 7)),
                             reads=[("wq", sl, 2), ("xT", b)], writes=[ppk])
                S.act(lambda e, bq=bq, pp=pp, sl=sl: e.activation(out=vv[sl][:, bq * 4:bq * 4 + 4, :],
                                                                 in_=pp[:].rearrange("p (a c) -> p a c", a=4), func=AF.Copy),
                      reads=[ppk], writes=[("vv", sl, bq)])
            its = []
            for qt in range(4):
                nkb = 4 * qt + 4
                if fox:
                    seq = [(hh, kb) for hh in (0, 1) for kb in range(nkb)]
                else:
                    seq = [(hh, kb) for kb in range(nkb - 1, -1, -1) for hh in (0, 1)]
                for idx, (hh, kb) in enumerate(seq):
                    first = (kb == 0) if fox else (kb == nkb - 1)
                    last = (kb == nkb - 1) if fox else (kb == 0)
                    its.append(dict(qt=qt, hh=hh, kb=kb, first=first, last=last, epi=(idx == len(seq) - 1)))

            def geom(it):
                t0 = it["qt"] * 512
                j0 = it["kb"] * 128
                off = max(0, j0 - t0)
                return t0, j0, off, j0 >= t0, 64 * it["hh"]

            if fox:
                PSB = (0, 1, 4)
                POB = ((2, 5), (3, 6))

                def f1(it, n, c=c, sl=sl):
                    t0, j0, off, diag, r0 = geom(it)
                    hh, kb, qt = it["hh"], it["kb"], it["qt"]
                    h = 2 * c + hh
                    ps, psk = P[PSB[n % 3]], ("ps", PSB[n % 3])
                    Et, Ek = E[n % 4], ("E", n % 4)
                    kTh = kTp[sl][hh]
                    S.pe(lambda e: e.matmul(ps[:, off:512], lhsT=kTh[:, j0:j0 + 128], rhs=qT[sl][:, t0 + off:t0 + 512],
                                            start=True, stop=False), reads=[("kT", sl, kb // 4), ("qT", sl, qt)], writes=[psk])
                    S.pe(lambda e: e.matmul(ps[:, off:512], lhsT=ones3p[:], rhs=c3[sl][:, hh * T + t0 + off:hh * T + t0 + 512],
                                            start=False, stop=(not diag)), reads=[("c3", sl), "ones3p"], writes=[psk])
                    if diag:
                        S.pe(lambda e: e.matmul(ps[:, off:off + 128], lhsT=self.identb[:], rhs=self.negm[:], start=False, stop=True),
                             reads=["identb", "negm"], writes=[psk])
                    S.act(lambda e: e.activation(out=Et[:, off:512], in_=ps[:, off:512], func=AF.Exp, bias=cnT[:, kb, h:h + 1], scale=0.125),
                          reads=[psk, "cnT"], writes=[Ek])

                def f2(it, n, c=c, sl=sl):
                    t0, j0, off, diag, r0 = geom(it)
                    kb, qt, hh = it["kb"], it["qt"], it["hh"]
                    Et, Ek = E[n % 4], ("E", n % 4)
                    po, pok = P[POB[hh][0]], ("ps", POB[hh][0])
                    pl, plk = P[POB[hh][1]], ("ps", POB[hh][1])
                    first, last = it["first"], it["last"]
                    S.pe(lambda e: e.matmul(po[:, off:512], lhsT=vv[sl][:, kb, :], rhs=Et[:, off:512], start=first, stop=last),
                         reads=[Ek, ("vv", sl, kb // 4)], writes=[pok])
                    S.pe(lambda e: e.matmul(pl[:, off:512], lhsT=self.onesb[:], rhs=Et[:, off:512], start=first, stop=last),
                         reads=[Ek, "onesb"], writes=[plk])
                    if last:
                        S.dve(lambda e: e.reciprocal(out=rl[r0:r0 + 64, :], in_=pl[r0:r0 + 64, :]), reads=[plk], writes=[("rl", hh)])
                        S.dve(lambda e: e.tensor_tensor(out=OT[r0:r0 + 64, c, t0:t0 + 512], in0=po[r0:r0 + 64, :], in1=rl[r0:r0 + 64, :], op=ALU.mult),
                              reads=[pok, ("rl", hh)], writes=[("OT", c, qt)])
                self.run_pipeline(its, [f1, f2], [3])
            else:
                def s1(it, n, c=c, sl=sl):
                    t0, j0, off, diag, r0 = geom(it)
                    kb, qt = it["kb"], it["qt"]
                    ps, psk = P[n % 2], ("ps", n % 2)
                    zt, zk = zs[n % 3], ("zs", n % 3)
                    et, ek = ez[n % 2], ("ez", n % 2)
                    st_, stk = spb[n % 3], ("spb", n % 3)
                    S.pe(lambda e, kTh=kTp[sl][it["hh"]]: e.matmul(ps[:, off:512], lhsT=kTh[:, j0:j0 + 128], rhs=qT[sl][:, t0 + off:t0 + 512],
                                                                 start=True, stop=(not diag)), reads=[("kT", sl, kb // 4), ("qT", sl, qt)], writes=[psk])
                    if diag:
                        S.pe(lambda e: e.matmul(ps[:, off:off + 128], lhsT=self.identb[:], rhs=self.negs[:], start=False, stop=True),
                             reads=["identb", "negs"], writes=[psk])
                    S.dve(lambda e: e.tensor_scalar(out=zt[:, off:512], in0=ps[:, off:512], scalar1=0.125, scalar2=None, op0=ALU.mult),
                          reads=[psk], writes=[zk])
                    S.act(lambda e: e.activation(out=et[:, off:512], in_=zt[:, off:512], func=AF.Exp), reads=[zk], writes=[ek])
                    S.act(lambda e: e.activation(out=st_[:, off:512], in_=et[:, off:512], func=AF.Ln, bias=self.oneb[:, 0:1], scale=1.0),
                          reads=[ek, "oneb"], writes=[stk])
                    if it["first"]:
                        S.pe(lambda e, hh=it["hh"]: e.matmul(P[6 + hh][:, 0:512], lhsT=zerob[:], rhs=self.onesb[:, 0:1].to_broadcast([128, 512]),
                                                            start=True, stop=False), reads=["zerob", "onesb"], writes=[("ps", 6 + it["hh"])])

                def s2(it, n, c=c, sl=sl):
                    t0, j0, off, diag, r0 = geom(it)
                    hh = it["hh"]
                    zt, zk = zs[n % 3], ("zs", n % 3)
                    st_, stk = spb[n % 3], ("spb", n % 3)
                    pc, pck = P[4 + n % 2], ("ps", 4 + n % 2)
                    pr, prk = P[6 + hh], ("ps", 6 + hh)
                    lt, lk = lw[n % 2], ("lw", n % 2)
                    Et, Ek = E[n % 3], ("E", n % 3)
                    first, last = it["first"], it["last"]
                    S.pe(lambda e: e.matmul(pc[:, off:512], lhsT=self.trige[:], rhs=st_[:, off:512], start=True, stop=True),
                         reads=[stk, "trige"], writes=[pck])
                    S.dve(lambda e: e.tensor_tensor(out=lt[:, off:512], in0=zt[:, off:512], in1=pc[:, off:512], op=ALU.subtract),
                          reads=[zk, pck], writes=[lk])
                    if not first:
                        S.dve(lambda e: e.tensor_tensor(out=lt[:, off:512], in0=lt[:, off:512], in1=pr[:, off:512], op=ALU.subtract),
                              reads=[lk, prk], writes=[lk])
                    if not last:
                        S.pe(lambda e: e.matmul(pr[:, off:512], lhsT=self.onesb[:], rhs=st_[:, off:512], start=False, stop=(it["kb"] == 1)),
                             reads=[stk, "onesb"], writes=[prk])
                    S.act(lambda e: e.activation(out=Et[:, off:512], in_=lt[:, off:512], func=AF.Exp), reads=[lk], writes=[Ek])

                def s3(it, n, c=c, sl=sl):
                    t0, j0, off, diag, r0 = geom(it)
                    kb, qt = it["kb"], it["qt"]
                    Et, Ek = E[n % 3], ("E", n % 3)
                    po, pok = P[2 + it["hh"]], ("ps", 2 + it["hh"])
                    S.pe(lambda e: e.matmul(po[:, off:512], lhsT=vv[sl][:, kb, :], rhs=Et[:, off:512],
                                            start=it["first"], stop=it["last"], skip_group_check=True),
                         reads=[Ek, ("vv", sl, kb // 4)], writes=[pok])
                    if it["last"]:
                        S.act(lambda e: e.activation(out=OT[r0:r0 + 64, c, t0:t0 + 512], in_=po[r0:r0 + 64, :], func=AF.Copy),
                              reads=[pok], writes=[("OT", c, qt)])
                self.run_pipeline(its, [s1, s2, s3], [1, 2])
        self.mixer_out(sctx, layer, lambda k, b: OT[:, k, b * 128:(b + 1) * 128], lambda k, b: [("OT", k, b // 4)], 8, Wo, L)

    def colvec(self, ctx2, vec_ap, n, dst, key):
        S, P = self.S, self.P
        tmp = self.sb(ctx2, "cvt", [n, 128], F32)
        tk = ("cvt", key)
        S.dma("sp", lambda e: e.dma_start(out=tmp[:], in_=vec_ap.rearrange("(c p) -> c p", p=128)), writes=[tk])
        S.pe(lambda e: e.transpose(out=P[7][:, 0:n], in_=tmp[:], identity=self.identf[0:n, 0:n]), reads=[tk, "identf"], writes=[("ps", 7)])
        S.dve(lambda e: e.tensor_copy(out=dst, in_=P[7][:, 0:n]), reads=[("ps", 7)], writes=[key])

    def ssd_stage(self, sctx, layer):
        S, P, nc = self.S, self.P, self.nc
        pre = "l%d_ssd_" % layer
        Win = self.inp(pre + "w_in", [D, 5152])
        convw = self.inp(pre + "conv_w", [4, 3072])
        convb = self.inp(pre + "conv_b", [3072])
        dtbias = self.inp(pre + "dt_bias", [32])
        alog = self.inp(pre + "a_log", [32])
        dskip = self.inp(pre + "d_skip", [32])
        normw = self.inp(pre + "norm_w", [2048])
        Wo = self.inp(pre + "w_out", [2048, D])
        L = self.ln_setup(sctx, layer, "mix")
        self.epsb = self.sb(sctx, "epsb", [128, 1], F32)
        S.dve(lambda e: e.memset(self.epsb[:], LN_EPS), writes=["epsb"])
        if layer % 2 == 1:
            self.router_setup(sctx, layer)
        ysp = nc.dram_tensor("ysp%d" % layer, [16, 128, T], BF16, kind="Internal").ap()
        cw = self.sb(sctx, "cw", [128, 24, 4], F32)
        cb = self.sb(sctx, "cb", [128, 24], F32)
        nwp = self.sb(sctx, "nwp", [128, 16], F32)
        dskp = self.sb(sctx, "dskp", [128, 16], F32)
        nAT = self.sb(sctx, "nAT", [128, NB, 32], F32)
        dtk = self.sb(sctx, "dtk", [128, NB, 32], F32)
        with ExitStack() as c2:
            self.colvec(c2, convb, 24, cb[:], "cb")
            self.colvec(c2, normw, 16, nwp[:], "nwp")
            cwr = self.sb(c2, "cwr", [4, 3072], F32)
            S.dma("sp", lambda e: e.dma_start(out=cwr[:], in_=convw), writes=["cwr"])
            for fc in range(24):
                S.pe(lambda e, fc=fc: e.transpose(out=P[6][:, fc * 4:fc * 4 + 4], in_=cwr[:, fc * 128:(fc + 1) * 128],
                                                  identity=self.identf[0:4, 0:4]), reads=["cwr", "identf"], writes=[("ps", 6)])
            S.dve(lambda e: e.tensor_copy(out=cw[:].rearrange("p a b -> p (a b)"), in_=P[6][:, 0:96]), reads=[("ps", 6)], writes=["cw"])
            with nc.allow_non_contiguous_dma(reason="tiny"):
                pass
            d2 = dskip.rearrange("(c two) -> two c", two=2)
            S.dma("sp", lambda e: e.dma_start(out=dskp[0:64, :], in_=d2[0].partition_broadcast(64), allow_slow_non_contiguous=True), writes=["dskp0"])
            S.dma("sp", lambda e: e.dma_start(out=dskp[64:128, :], in_=d2[1].partition_broadcast(64), allow_slow_non_contiguous=True), writes=["dskp1"])
            wdt = self.sb(c2, "wdt", [128, 8, 32], BF16)
            self.wload(wdt[:], Win[:, 5120:5152].rearrange("(k p) c -> p k c", p=128), "wdt")
            dtb = self.sb(c2, "dtb", [32, 1], F32)
            al = self.sb(c2, "al", [32, 1], F32)
            S.dma("sp", lambda e: e.dma_start(out=dtb[:], in_=dtbias.rearrange("(p o) -> p o", o=1)), writes=["dtb"])
            S.dma("sp", lambda e: e.dma_start(out=al[:], in_=alog.rearrange("(p o) -> p o", o=1)), writes=["al"])
            S.act(lambda e: e.activation(out=al[:], in_=al[:], func=AF.Exp), reads=["al"], writes=["al"])
            S.dve(lambda e: e.tensor_scalar(out=al[:], in0=al[:], scalar1=-1.0, scalar2=None, op0=ALU.mult), reads=["al"], writes=["al"])
            dtT = self.sb(c2, "dtT", [32, T], F32)
            An = self.sb(c2, "An", [32, T], F32)
            tm = self.sb(c2, "tm", [32, T], F32)
            Ad = self.sb(c2, "Ad", [32, T], F32)
            c3b = self.sb(c2, "c3b", [32, 3, T], BF16)
            for tt in range(4):
                pf, pfk = P[tt % 2], ("ps", tt % 2)
                for k in range(8):
                    S.pe(lambda e, k=k, tt=tt, pf=pf: e.matmul(pf[0:32, :], lhsT=wdt[:, k, :], rhs=self.xT[:, k, tt * 512:(tt + 1) * 512],
                                                            start=(k == 0), stop=(k == 7)),
                         reads=["wdt"] + [("xT", tt * 4 + q) for q in range(4)], writes=[pfk])
                S.act(lambda e, tt=tt, pf=pf: e.activation(out=tm[:, tt * 512:(tt + 1) * 512], in_=pf[0:32, :], func=AF.Exp,
                                                        bias=dtb[:, 0:1], scale=1.0), reads=[pfk, "dtb"], writes=[("tm", tt)])
                S.act(lambda e, tt=tt: e.activation(out=dtT[:, tt * 512:(tt + 1) * 512], in_=tm[:, tt * 512:(tt + 1) * 512],
                                                    func=AF.Ln, bias=self.oneb[0:32, 0:1], scale=1.0),
                      reads=[("tm", tt), "oneb"], writes=[("dtT", tt)])
            alld = [("dtT", tt) for tt in range(4)]
            allt = [("tm", tt) for tt in range(4)]
            S.dve(lambda e: e.tensor_scalar(out=Ad[:], in0=dtT[:], scalar1=al[:, 0:1], scalar2=None, op0=ALU.mult),
                  reads=alld + ["al"], writes=["da"])
            S.dve(lambda e: e.memset(tm[:], 1.0), writes=allt + ["tm1"])
            S.dve(lambda e: e.tensor_tensor_scan(out=An[:], data0=tm[:], data1=Ad[:], initial=0.0, op0=ALU.mult, op1=ALU.add),
                  reads=["da", "tm1"], writes=["An"])
            for b in range(NB):
                pt, ptk = P[2 + b % 2], ("ps", 2 + b % 2)
                S.pe(lambda e, b=b, pt=pt: e.transpose(out=pt[:, 0:32], in_=An[:, b * 128:(b + 1) * 128], identity=self.identf[0:32, 0:32]),
                     reads=["An", "identf"], writes=[ptk])
                S.pe(lambda e, b=b, pt=pt: e.transpose(out=pt[:, 32:64], in_=dtT[:, b * 128:(b + 1) * 128], identity=self.identf[0:32, 0:32]),
                     reads=alld + ["identf"], writes=[ptk])
                S.dve(lambda e, b=b, pt=pt: e.tensor_scalar(out=nAT[:, b, :], in0=pt[:, 0:32], scalar1=-1.0, scalar2=None, op0=ALU.mult),
                      reads=[ptk], writes=["nAT"])
                S.dve(lambda e, b=b, pt=pt: e.tensor_copy(out=dtk[:, b, :], in_=pt[:, 32:64]), reads=[ptk], writes=["dtk"])
            S.dve(lambda e: e.tensor_copy(out=tm[:], in_=An[:]), reads=["An", "tm1"], writes=["m8"])
            for i in range(3):
                S.dve(lambda e, i=i: e.tensor_copy(out=c3b[:, i, :], in_=tm[:]), reads=["m8"], writes=[("c3b", i)])
                if i < 2:
                    S.dve(lambda e, i=i: e.tensor_tensor(out=tm[:], in0=tm[:], in1=c3b[:, i, :], op=ALU.subtract),
                          reads=["m8", ("c3b", i)], writes=["m8"])
            for a in range(2):
                S.dma("sp", lambda e, a=a: e.dma_start(out=self.scr[1 + a, :, :].rearrange("i (h t) -> h i t", h=16), in_=c3b[a * 16:(a + 1) * 16, :, :]),
                      reads=[("c3b", i) for i in range(3)], writes=[("scrA", a)])
            S.flush(barrier=True)
        with ExitStack() as c3x:
            xsT = self.sb(c3x, "xsT", [128, 4, T], BF16)
            zT = self.sb(c3x, "zT", [128, 4, T], BF16)
            Vg = self.sb(c3x, "Vg", [128, NB, 512], BF16)
            BT = self.sb(c3x, "BT", [128, T], BF16)
            CT = self.sb(c3x, "CT", [128, T], BF16)
            raw = [self.sb(c3x, "raw", [128, 3 + T], BF16) for _ in range(2)]
            Dg = self.sb(c3x, "Dg", [128, 6, 4, 128], BF16)
            wch = [self.sb(c3x, "wch", [128, 8, 128], BF16) for _ in range(2)]
            tmpf = [self.sb(c3x, "tmpf", [128, 512], F32) for _ in range(2)]
            c3q = [self.sb(c3x, "c3q", [128, 8, 512], BF16) for _ in range(2)]
            ones3p = self.sb(c3x, "ones3p", [128, 128], BF16)
            S.dve(lambda e: e.memset(ones3p[:], 0.0), writes=["ones3p"])
            S.dve(lambda e: e.memset(ones3p[0:3, :], 1.0), reads=["ones3p"], writes=["ones3p"])
            for i_ in range(2):
                S.dve(lambda e, i_=i_: e.memset(c3q[i_][:], 0.0), writes=[("c3q", i_)])
            Ab8 = self.sb(c3x, "Ab8", [128, 8, 512], F32)
            CBs = [self.sb(c3x, "CBs", [128, 512], BF16) for _ in range(3)]
            Ef = [self.sb(c3x, "Ef", [128, 512], BF16) for _ in range(3)]
            Mt = [self.sb(c3x, "Mt", [128, 512], BF16) for _ in range(3)]
            yz = self.sb(c3x, "yz", [128, 4, 512], F32)
            sq = [self.sb(c3x, "sq", [128, 512], BF16) for _ in range(2)]
            rs = self.sb(c3x, "rs", [128, 512], F32)
            ynt = [self.sb(c3x, "ynt", [128, 512], BF16) for _ in range(2)]
            for i in range(2):
                S.dve(lambda e, i=i: e.memset(raw[i][:, 0:3], 0.0), writes=[("rawpad", i)])
            wi = 0
            ri = 0
            ti = 0
            for g in range(4):
                a, hloc = (8 * g) // 16, (8 * g) % 16
                chunks = [("xs", i, 2048 + g * 512 + i * 128, g * 4 + i) for i in range(4)]
                chunks += [("B", 0, 4096 + g * 128, 16 + g), ("C", 0, 4608 + g * 128, 20 + g)]
                chunks += [("z", i, g * 512 + i * 128, None) for i in range(4)]
                ci = 0
                for kind, idx, col0, fc in chunks:
                    ws = wi % 2
                    wi += 1
                    self.wload(wch[ws][:], Win[:, col0:col0 + 128].rearrange("(k p) c -> p k c", p=128), ("wch", ws))
                    if fc is not None:
                        for k in range(4):
                            S.dve(lambda e, ci=ci, k=k, fc=fc: e.tensor_scalar(out=Dg[:, ci, k, :], in0=self.identb[:], scalar1=cw[:, fc, k:k + 1],
                                                                              scalar2=None, op0=ALU.mult),
                                  reads=["identb", "cw"], writes=[("Dg", ci)])
                        rw = ri % 2
                        ri += 1
                    for tt in range(4):
                        pp, ppk = P[tt % 2], ("ps", tt % 2)
                        for k in range(8):
                            S.pe(lambda e, k=k, tt=tt, pp=pp, ws=ws: e.matmul(pp[:], lhsT=wch[ws][:, k, :], rhs=self.xT[:, k, tt * 512:(tt + 1) * 512],
                                                                         start=(k == 0), stop=(k == 7)),
                                 reads=[("wch", ws)] + [("xT", tt * 4 + q) for q in range(4)], writes=[ppk])
                        if fc is None:
                            S.act(lambda e, tt=tt, pp=pp, idx=idx: e.activation(out=zT[:, idx, tt * 512:(tt + 1) * 512], in_=pp[:], func=AF.Silu),
                                  reads=[ppk], writes=[("zT", idx, tt)])
                        else:
                            S.act(lambda e, tt=tt, pp=pp, rw=rw: e.activation(out=raw[rw][:, 3 + tt * 512:3 + (tt + 1) * 512], in_=pp[:], func=AF.Copy),
                                  reads=[ppk, ("rawpad", rw)], writes=[("raw", rw, tt)])
                    if fc is not None:
                        for tt in range(4):
                            pc, pck = P[2 + tt % 2], ("ps", 2 + tt % 2)
                            rr = [("raw", rw, tt), ("rawpad", rw)] + ([("raw", rw, tt - 1)] if tt > 0 else [])
                            for k in range(4):
                                S.pe(lambda e, k=k, tt=tt, pc=pc, ci=ci, rw=rw: e.matmul(pc[:], lhsT=Dg[:, ci, k, :],
                                                                                   rhs=raw[rw][:, tt * 512 + k:tt * 512 + k + 512],
                                                                                   start=(k == 0), stop=(k == 3)),
                                     reads=rr + [("Dg", ci)], writes=[pck])
                            if kind == "xs":
                                tf, tfk = tmpf[ti % 2], ("tmpf", ti % 2)
                                ti += 1
                                S.act(lambda e, pc=pc, tf=tf, fc=fc: e.activation(out=tf[:], in_=pc[:], func=AF.Silu, bias=cb[:, fc:fc + 1], scale=1.0),
                                      reads=[pck, "cb"], writes=[tfk])
                                S.dve(lambda e, tf=tf, idx=idx, tt=tt: e.tensor_copy(out=xsT[:, idx, tt * 512:(tt + 1) * 512], in_=tf[:]),
                                      reads=[tfk], writes=[("xsT", idx, tt)])
                                pt, ptk = P[4 + tt % 2], ("ps", 4 + tt % 2)
                                for q in range(4):
                                    S.pe(lambda e, q=q, pt=pt, tf=tf: e.transpose(out=pt[:, q * 128:(q + 1) * 128], in_=tf[:, q * 128:(q + 1) * 128],
                                                                                identity=self.identf[:]), reads=[tfk, "identf"], writes=[ptk])
                                h0 = 8 * g + 2 * idx
                                S.dve(lambda e, pt=pt, tt=tt, idx=idx, h0=h0: e.tensor_tensor(
                                    out=Vg[:, tt * 4:tt * 4 + 4, idx * 128:(idx + 1) * 128].rearrange("p b (h d) -> p b h d", h=2),
                                    in0=pt[:].rearrange("p (b h d) -> p b h d", b=4, h=2),
                                    in1=dtk[:, tt * 4:tt * 4 + 4, h0:h0 + 2].unsqueeze(3).to_broadcast([128, 4, 2, 64]), op=ALU.mult),
                                    reads=[ptk, "dtk"], writes=[("Vg", tt)])
                            else:
                                dstT = BT if kind == "B" else CT
                                S.act(lambda e, pc=pc, dstT=dstT, tt=tt, fc=fc: e.activation(out=dstT[:, tt * 512:(tt + 1) * 512], in_=pc[:], func=AF.Silu,
                                                                                          bias=cb[:, fc:fc + 1], scale=1.0),
                                      reads=[pck, "cb"], writes=[(kind + "T", tt)])
                        ci += 1
                its = []
                for qt in range(4):
                    nkb = 4 * qt + 4
                    for half in range(2):
                        for kb in range(nkb):
                            for hq in range(4):
                                its.append(dict(qt=qt, kb=kb, hl=4 * half + hq, hq=hq, half=half, first=(kb == 0), last=(kb == nkb - 1),
                                                cbn=len(its) // 4, epi=(kb == nkb - 1 and hq == 3)))
                PSB = (0, 1, 7)

                def epi_half(qt, half, g=g):
                    t0 = qt * 512
                    for hq in range(4):
                        i = 2 * half + hq // 2
                        r0 = 64 * (hq % 2)
                        fcg = g * 4 + i
                        po, pok = P[3 + hq], ("ps", 3 + hq)
                        S.dve(lambda e, i=i, po=po, fcg=fcg, t0=t0, r0=r0: e.scalar_tensor_tensor(
                            out=yz[r0:r0 + 64, i, :], in0=xsT[r0:r0 + 64, i, t0:t0 + 512], scalar=dskp[r0:r0 + 64, fcg:fcg + 1], in1=po[r0:r0 + 64, :],
                            op0=ALU.mult, op1=ALU.add), reads=[("xsT", i, qt), "dskp0", "dskp1", pok], writes=[("yz", i)])
                        S.dve(lambda e, i=i, t0=t0, r0=r0: e.tensor_tensor(out=yz[r0:r0 + 64, i, :], in0=yz[r0:r0 + 64, i, :],
                                                                        in1=zT[r0:r0 + 64, i, t0:t0 + 512], op=ALU.mult),
                              reads=[("yz", i), ("zT", i, qt)], writes=[("yz", i)])

                def epilogue(qt, g=g):
                    t0 = qt * 512
                    pss, pssk = P[7], ("ps", 7)
                    for i in range(4):
                        fcg = g * 4 + i
                        sqt, sqk = sq[i % 2], ("sq", i % 2)
                        S.act(lambda e, i=i, sqt=sqt: e.activation(out=sqt[:], in_=yz[:, i, :], func=AF.Square), reads=[("yz", i)], writes=[sqk])
                        S.pe(lambda e, i=i, sqt=sqt: e.matmul(pss[:], lhsT=self.onesb[:], rhs=sqt[:], start=(i == 0), stop=(i == 3)),
                             reads=[sqk, "onesb"], writes=[pssk])
                    S.act(lambda e: e.activation(out=rs[:], in_=pss[:], func=AF.Sqrt, bias=self.epsb[:, 0:1], scale=1.0 / 512.0),
                          reads=[pssk, "epsb"], writes=["rs"])
                    S.dve(lambda e: e.reciprocal(out=rs[:], in_=rs[:]), reads=["rs"], writes=["rs"])
                    for i in range(4):
                        fcg = g * 4 + i
                        yt, ytk = ynt[i % 2], ("ynt", i % 2)
                        S.dve(lambda e, i=i, yt=yt, fcg=fcg: e.scalar_tensor_tensor(out=yt[:], in0=yz[:, i, :], scalar=nwp[:, fcg:fcg + 1], in1=rs[:],
                                                                                  op0=ALU.mult, op1=ALU.mult),
                              reads=[("yz", i), "nwp", "rs"], writes=[ytk])
                        S.dma("sp", lambda e, yt=yt, fcg=fcg, t0=t0: e.dma_start(out=ysp[fcg, :, t0:t0 + 512], in_=yt[:]),
                              reads=[ytk], writes=[("ysp", fcg, qt)], semkey=("yspst", i % 2))

                def d1(it, n, g=g):
                    qt, kb, hl = it["qt"], it["kb"], it["hl"]
                    t0, j0 = qt * 512, kb * 128
                    off = max(0, j0 - t0)
                    diag = j0 >= t0
                    h = 8 * g + hl
                    cbt, cbk = CBs[it["cbn"] % 3], ("CBs", it["cbn"] % 3)
                    if it["hq"] == 0:
                        pcb, pcbk = P[2], ("ps", 2)
                        S.pe(lambda e: e.matmul(pcb[:, off:512], lhsT=BT[:, j0:j0 + 128], rhs=CT[:, t0 + off:t0 + 512], start=True, stop=True),
                             reads=[("BT", kb // 4), ("CT", qt)], writes=[pcbk])
                        S.dve(lambda e: e.tensor_copy(out=cbt[:, off:512], in_=pcb[:, off:512]), reads=[pcbk], writes=[cbk])
                    ps, psk = P[PSB[n % 3]], ("ps", PSB[n % 3])
                    et, ek = Ef[n % 3], ("Ef", n % 3)
                    mt, mk = Mt[n % 3], ("Mt", n % 3)
                    c3t, c3k = c3q[qt % 2], ("c3q", qt % 2)
                    if kb == 0 and hl == 0:
                        a_, hloc_ = (8 * g) // 16, (8 * g) % 16
                        S.dma("sp", lambda e, c3t=c3t, a_=a_, hloc_=hloc_: e.dma_start(
                            out=c3t[0:3, :, :], in_=self.scr[1 + a_, :, :].rearrange("i (h t) -> i h t", h=16)[:, hloc_:hloc_ + 8, t0:t0 + 512]),
                            reads=[("scrA", a_)], writes=[c3k])
                    if qt > 0 and kb == 0 and hl == 0:
                        for h2 in range(8):
                            pa, pak = P[PSB[h2 % 3]], ("ps", PSB[h2 % 3])
                            S.pe(lambda e, h2=h2, pa=pa: e.matmul(pa[:, 0:512], lhsT=ones3p[:], rhs=c3t[:, h2, :],
                                                                 start=True, stop=True), reads=[c3k, "onesb"], writes=[pak])
                            if False:
                                pass
                            else:
                                S.dve(lambda e, h2=h2, pa=pa: e.tensor_copy(out=Ab8[:, h2, :], in_=pa[:, 0:512]), reads=[pak], writes=[("Ab8", h2)])
                    if diag:
                        S.pe(lambda e: e.matmul(ps[:, off:512], lhsT=ones3p[:], rhs=c3t[:, hl, off:512],
                                                start=True, stop=False), reads=[c3k, "ones3p"], writes=[psk])
                        S.pe(lambda e: e.matmul(ps[:, off:off + 128], lhsT=self.identb[:], rhs=self.negm[:], start=False, stop=True),
                             reads=["identb", "negm"], writes=[psk])
                        S.act(lambda e: e.activation(out=et[:, off:512], in_=ps[:, off:512], func=AF.Exp, bias=nAT[:, kb, h:h + 1], scale=1.0),
                              reads=[psk, "nAT"], writes=[ek])
                    else:
                        S.act(lambda e: e.activation(out=et[:, 0:512], in_=Ab8[:, hl, :], func=AF.Exp, bias=nAT[:, kb, h:h + 1], scale=1.0),
                              reads=[("Ab8", hl), "nAT"], writes=[ek])
                    S.dve(lambda e: e.tensor_tensor(out=mt[:, off:512], in0=et[:, off:512], in1=cbt[:, off:512], op=ALU.mult),
                          reads=[ek, cbk], writes=[mk])

                def d2(it, n, g=g):
                    qt, kb, hl = it["qt"], it["kb"], it["hl"]
                    t0, j0 = qt * 512, kb * 128
                    off = max(0, j0 - t0)
                    hq, half = it["hq"], it["half"]
                    po, pok = P[3 + hq], ("ps", 3 + hq)
                    mt, mk = Mt[n % 3], ("Mt", n % 3)
                    pc0 = (hl // 2) * 128
                    S.pe(lambda e: e.matmul(po[:, off:512], lhsT=Vg[:, kb, pc0:pc0 + 128], rhs=mt[:, off:512],
                                            start=it["first"], stop=it["last"]), reads=[mk, ("Vg", kb // 4)], writes=[pok])
                    if it["epi"]:
                        epi_half(qt, half)
                        if half == 1:
                            epilogue(qt)
                self.run_pipeline(its, [d1, d2], [2])
            S.flush(barrier=True)
        ynb = [self.sb(sctx, "ynb", [128, 16, 128], BF16) for _ in range(2)]

        def pre_block(b):
            S.dma("sp", lambda e: e.dma_start(out=ynb[b % 2][:], in_=ysp[:, :, b * 128:(b + 1) * 128].rearrange("c p t -> p c t")),
                  writes=[("ynb", b % 2)])
        self.mixer_out(sctx, layer, lambda k, b: ynb[b % 2][:, k, :], lambda k, b: [("ynb", b % 2)], 16, Wo, L, pre_block=pre_block)


_CACHE = {}


def run(inputs, stages):
    key = tuple(stages)
    if key not in _CACHE:
        bld = Builder(stages)
        nc = bld.build()
        _CACHE[key] = (bld, nc)
    bld, nc = _CACHE[key]
    x = np.asarray(inputs["x"], dtype=np.float32)
    in_maps = []
    shared = {n: np.ascontiguousarray(np.asarray(inputs[n], dtype=np.float32)) for n in bld.used_inputs if n != "x"}
    for c in range(8):
        m = dict(shared)
        m["x"] = np.ascontiguousarray(x[c])
        in_maps.append(m)
    res = run_bass_kernel_spmd(nc, in_maps, core_ids=list(range(8)))
    return np.stack([np.asarray(r["y"]) for r in res.results], axis=0).astype(np.float32)


ALL_INPUT_NAMES = (
    "x",
    "l0_ssd_w_in", "l0_ssd_conv_w", "l0_ssd_conv_b", "l0_ssd_dt_bias", "l0_ssd_a_log", "l0_ssd_d_skip", "l0_ssd_norm_w", "l0_ssd_w_out",
    "l0_ln_mix_g", "l0_ln_mix_b", "l0_ffn_w_gate", "l0_ffn_w_up", "l0_ffn_w_down", "l0_ln_ffn_g", "l0_ln_ffn_b",
    "l1_sb_w_qkv", "l1_sb_w_out", "l1_ln_mix_g", "l1_ln_mix_b",
    "l1_moe_w_router", "l1_moe_w_gate", "l1_moe_w_up", "l1_moe_w_down", "l1_ln_ffn_g", "l1_ln_ffn_b",
    "l2_fox_w_qkvf", "l2_fox_b_f", "l2_fox_w_out", "l2_ln_mix_g", "l2_ln_mix_b",
    "l2_ffn_w_gate", "l2_ffn_w_up", "l2_ffn_w_down", "l2_ln_ffn_g", "l2_ln_ffn_b",
    "l3_ssd_w_in", "l3_ssd_conv_w", "l3_ssd_conv_b", "l3_ssd_dt_bias", "l3_ssd_a_log", "l3_ssd_d_skip", "l3_ssd_norm_w", "l3_ssd_w_out",
    "l3_ln_mix_g", "l3_ln_mix_b",
    "l3_moe_w_router", "l3_moe_w_gate", "l3_moe_w_up", "l3_moe_w_down", "l3_ln_ffn_g", "l3_ln_ffn_b",
)


def kernel(**inputs):
    missing = [n for n in ALL_INPUT_NAMES if n not in inputs]
    assert not missing, missing
    return run(inputs, list(range(8)))
```

```python
import numpy as np
from contextlib import ExitStack
import concourse.bass as bass
import concourse.mybir as mybir
from concourse.bass_utils import run_bass_kernel_spmd

F32 = mybir.dt.float32
BF16 = mybir.dt.bfloat16
AF = mybir.ActivationFunctionType
ALU = mybir.AluOpType

T = 2048
D = 1024
NB = 16
ALPHA = (2.0 * 4) ** 0.25
LN_EPS = 1e-5
NEG = -30000.0


class Op:
    __slots__ = ("eng", "fn", "reads", "writes", "dma", "signal", "count", "deps", "semkey", "kind", "kw")

    def __init__(self, eng, fn, reads, writes, dma, semkey):
        self.eng, self.fn, self.reads, self.writes = eng, fn, reads, writes
        self.dma, self.semkey = dma, semkey
        self.signal = False
        self.count = 0
        self.deps = []


class Sched:
    def __init__(self, nc, ctx):
        self.nc = nc
        self.ctx = ctx
        self.ops = []
        self.engobj = {"pe": nc.tensor, "act": nc.scalar, "dve": nc.vector,
                       "pool": nc.gpsimd, "sp": nc.sync}
        self.sems = {}
        self.cnt = {}
        self.last_w = {}
        self.readers = {}
        self.seen = {e: {} for e in self.engobj}
        self.last_op = {}
        self.pending_barrier = None
        self.dma_last = {}
        self.nops = 0
        self.if_state = None
        self.cregs = None

    def add(self, eng, fn, reads=(), writes=(), dma=False, semkey=None):
        op = Op(eng, fn, tuple(reads), tuple(writes), dma, semkey)
        self.ops.append(op)
        return op

    def pe(self, fn, reads=(), writes=()):
        return self.add("pe", fn, reads, writes)

    def act(self, fn, reads=(), writes=()):
        return self.add("act", fn, reads, writes)

    def dve(self, fn, reads=(), writes=()):
        return self.add("dve", fn, reads, writes)

    def pool(self, fn, reads=(), writes=()):
        return self.add("pool", fn, reads, writes)

    def dma(self, q, fn, reads=(), writes=(), semkey=None):
        return self.add(q, fn, reads, writes, dma=True, semkey=(writes[0] if semkey is None else semkey))

    def ctl(self, kind, **kw):
        op = Op("ctl", None, (), (), False, None)
        op.kind, op.kw = kind, kw
        self.ops.append(op)
        return op

    def _sem(self, key):
        s = self.sems.get(key)
        if s is None:
            s = self.ctx.enter_context(self.nc.semaphore("s%d" % len(self.sems)))
            self.sems[key] = s
        return s

    def flush(self, barrier=True):
        ops = self.ops
        self.ops = []
        last_w, readers = self.last_w, self.readers
        first_after = {}
        bar = self.pending_barrier
        for op in ops:
            if op.eng == "ctl":
                continue
            deps = set()
            for k in op.reads:
                w = last_w.get(k)
                if w is not None:
                    deps.add(w)
            for k in op.writes:
                w = last_w.get(k)
                if w is not None:
                    deps.add(w)
                for r in readers.get(k, ()):
                    deps.add(r)
            deps.discard(op)
            fin = []
            for d in deps:
                if d.dma:
                    fin.append(d)
                    continue
                if d.eng == op.eng and not op.dma:
                    if not any(k in d.writes for k in op.reads):
                        continue
                fin.append(d)
            if bar is not None and op.eng not in first_after:
                first_after[op.eng] = True
                fin.extend(bar)
            op.deps = fin
            for d in fin:
                d.signal = True
            for k in op.writes:
                last_w[k] = op
                readers[k] = []
            for k in op.reads:
                readers.setdefault(k, []).append(op)
            self.last_op[op.eng] = op
            if op.dma:
                self.dma_last[op.semkey] = op
        if bar is not None:
            pass
        if barrier:
            blist = [o for o in self.last_op.values() if not o.dma]
            blist += list(self.dma_last.values())
            if bar is not None and len(first_after) < len(self.engobj):
                blist += [o for o in bar if o not in blist]
            for o in blist:
                o.signal = True
            self.pending_barrier = blist
            self.last_w = {}
            self.readers = {}
            self.dma_last = {}
        else:
            self.pending_barrier = None
        cnt = self.cnt
        for op in ops:
            if op.eng == "ctl":
                continue
            if op.dma:
                key = ("dma", op.semkey)
                cnt[key] = cnt.get(key, 0) + 16
                op.count = cnt[key]
            elif op.signal:
                key = ("eng", op.eng)
                cnt[key] = cnt.get(key, 0) + 1
                op.count = cnt[key]
        for idx, op in enumerate(ops):
            if op.eng == "ctl":
                self._emit_ctl(op, ops, idx)
                continue
            eng = self.engobj[op.eng]
            need = {}
            for d in op.deps:
                key = ("dma", d.semkey) if d.dma else ("eng", d.eng)
                if d.count > need.get(key, 0):
                    need[key] = d.count
            sv = self.seen[op.eng]
            for key, c in need.items():
                if sv.get(key, 0) >= c:
                    continue
                eng.wait_ge(self._sem(key), c)
                sv[key] = c
            inst = op.fn(eng)
            if op.dma:
                inst.then_inc(self._sem(("dma", op.semkey)), 16)
            elif op.signal:
                inst.then_inc(self._sem(("eng", op.eng)), 1)
        self.nops += len(ops)

    def _emit_ctl(self, op, ops, idx):
        nc = self.nc
        if op.kind == "if":
            inc = {}
            base = {}
            j = idx + 1
            while not (ops[j].eng == "ctl" and ops[j].kind == "endif"):
                o = ops[j]
                if o.eng != "ctl":
                    if o.dma:
                        k2 = (("dma", o.semkey), o.eng)
                        inc[k2] = inc.get(k2, 0) + 16
                        if k2 not in base:
                            base[k2] = o.count - 16
                    elif o.signal:
                        k2 = (("eng", o.eng), o.eng)
                        inc[k2] = inc.get(k2, 0) + 1
                j += 1
            if self.cregs is None:
                self.cregs = nc.alloc_registers("cnd")
            nc.regs_load(self.cregs, op.kw["ap"])
            cm = nc.If_lt(self.cregs, op.kw["thresh"] + 1)
            cm.__enter__()
            for (key, engname), amt in inc.items():
                if (key, engname) in base and base[(key, engname)] > 0:
                    self.engobj[engname].wait_ge(self._sem(key), base[(key, engname)])
                self.engobj[engname].drain().then_inc(self._sem(key), amt)
            cm.__exit__(None, None, None)
            cm2 = nc.Else()
            cm2.__enter__()
            self.if_state = dict(cm=cm2, seen={e: dict(v) for e, v in self.seen.items()})
        elif op.kind == "endif":
            st = self.if_state
            st["cm"].__exit__(None, None, None)
            self.seen = st["seen"]
            self.if_state = None

    def final_wait(self, eng_name="sp"):
        eng = self.engobj[eng_name]
        for key, s in self.sems.items():
            eng.wait_ge(s, self.cnt[key])


class Builder:
    def __init__(self, stages):
        self.stages = stages
        self.nc = bass.Bass("TRN2", target_bir_lowering=False)
        self.din = {}
        self.used_inputs = []

    def inp(self, name, shape):
        if name not in self.din:
            self.din[name] = self.nc.dram_tensor(name, list(shape), F32, kind="ExternalInput").ap()
            self.used_inputs.append(name)
        return self.din[name]

    def sb(self, ctx, name, shape, dt):
        self.uid += 1
        return ctx.enter_context(self.nc.sbuf_tensor("%s_%d" % (name, self.uid), list(shape), dt))

    def build(self):
        nc = self.nc
        self.uid = 0
        self.x_in = self.inp("x", [T, D])
        self.y = nc.dram_tensor("y", [T, D], F32, kind="ExternalOutput").ap()
        self.scr = nc.dram_tensor("scr", [4, 3, 16 * T], BF16, kind="Internal").ap()
        with ExitStack() as ctx:
            self.ctx = ctx
            S = self.S = Sched(nc, ctx)
            self.P = [ctx.enter_context(nc.psum_tensor("ps%d" % i, [128, 512], F32)) for i in range(8)]
            self.xT = self.sb(ctx, "xT", [128, 8, T], BF16)
            self.gates = self.sb(ctx, "gates", [128, NB, 8], F32)
            self.identf = self.sb(ctx, "identf", [128, 128], F32)
            self.identb = self.sb(ctx, "identb", [128, 128], BF16)
            self.onesb = self.sb(ctx, "onesb", [128, 128], BF16)
            self.negm = self.sb(ctx, "negm", [128, 128], BF16)
            self.negs = self.sb(ctx, "negs", [128, 128], BF16)
            self.trige = self.sb(ctx, "trige", [128, 128], BF16)
            self.mstrict = self.sb(ctx, "mstrict", [128, 128], BF16)
            self.oneb = self.sb(ctx, "oneb", [128, 1], F32)
            self.consts()
            self.prep()
            S.flush(barrier=True)
            for st in self.stages:
                with ExitStack() as sctx:
                    layer, kind = st // 2, st % 2
                    if kind == 0:
                        if layer in (0, 3):
                            self.ssd_stage(sctx, layer)
                        elif layer == 1:
                            self.attn_stage(sctx, layer, "sb")
                        else:
                            self.attn_stage(sctx, layer, "fox")
                    else:
                        if layer % 2 == 1:
                            self.moe_stage(sctx, layer)
                        else:
                            self.ffn_stage(sctx, layer, moe=False)
                    S.flush(barrier=True)
            S.flush(barrier=True)
            S.final_wait("sp")
        return nc

    def consts(self):
        nc, S = self.nc, self.S
        S.pool(lambda e: e.memset(self.identf[:], 1.0), writes=["identf"])
        S.pool(lambda e: e.affine_select(out=self.identf[:], in_=self.identf[:], pattern=[[-1, 128]],
                                         compare_op=ALU.is_equal, fill=0.0, base=0, channel_multiplier=1),
               reads=["identf"], writes=["identf"])
        S.dve(lambda e: e.tensor_copy(out=self.identb[:], in_=self.identf[:]), reads=["identf"], writes=["identb"])
        S.pool(lambda e: e.memset(self.onesb[:], 1.0), writes=["onesb"])
        S.pool(lambda e: e.memset(self.oneb[:], 1.0), writes=["oneb"])
        S.pool(lambda e: e.memset(self.negm[:], 0.0), writes=["negm"])
        S.pool(lambda e: e.affine_select(out=self.negm[:], in_=self.negm[:], pattern=[[1, 128]],
                                         compare_op=ALU.is_ge, fill=NEG, base=0, channel_multiplier=-1),
               reads=["negm"], writes=["negm"])
        S.pool(lambda e: e.memset(self.negs[:], 0.0), writes=["negs"])
        S.pool(lambda e: e.affine_select(out=self.negs[:], in_=self.negs[:], pattern=[[1, 128]],
                                         compare_op=ALU.is_gt, fill=NEG, base=0, channel_multiplier=-1),
               reads=["negs"], writes=["negs"])
        S.pool(lambda e: e.memset(self.trige[:], 1.0), writes=["trige"])
        S.pool(lambda e: e.affine_select(out=self.trige[:], in_=self.trige[:], pattern=[[-1, 128]],
                                         compare_op=ALU.is_ge, fill=0.0, base=0, channel_multiplier=1),
               reads=["trige"], writes=["trige"])
        S.pool(lambda e: e.memset(self.mstrict[:], 1.0), writes=["mstrict"])
        S.pool(lambda e: e.affine_select(out=self.mstrict[:], in_=self.mstrict[:], pattern=[[1, 128]],
                                         compare_op=ALU.is_gt, fill=0.0, base=0, channel_multiplier=-1),
               reads=["mstrict"], writes=["mstrict"])

    def prep(self):
        S = self.S
        with ExitStack() as c:
            xb = [self.sb(c, "px", [128, D], F32) for _ in range(2)]
            for b in range(NB):
                t = xb[b % 2]
                rk = ("px", b % 2)
                S.dma("sp", lambda e, t=t, b=b: e.dma_start(out=t[:], in_=self.x_in[b * 128:(b + 1) * 128, :]),
                      writes=[rk])
                S.dma("sp", lambda e, t=t, b=b: e.dma_start(out=self.y[b * 128:(b + 1) * 128, :], in_=t[:]),
                      reads=[rk], writes=[("y", b)], semkey=("yst", b % 2))
                self.make_xT(t, rk, b)
            S.flush(barrier=True)

    def make_xT(self, src, rk, b, xtf=None):
        S, P = self.S, self.P
        for half in range(2):
            pb = P[6 + half]
            pk = ("ps", 6 + half)
            for kk in range(4):
                k = half * 4 + kk
                S.pe(lambda e, pb=pb, kk=kk, k=k: e.transpose(out=pb[:, kk * 128:(kk + 1) * 128],
                                                              in_=src[:, k * 128:(k + 1) * 128],
                                                              identity=self.identf[:]),
                     reads=[rk, "identf"], writes=[pk])
            dst = self.xT[:, half * 4:half * 4 + 4, b * 128:(b + 1) * 128]
            srcp = pb[:].rearrange("p (a c) -> p a c", a=4)
            if xtf is not None:
                S.dve(lambda e, half=half, srcp=srcp: e.tensor_copy(out=xtf[:, half * 4:half * 4 + 4, :], in_=srcp),
                      reads=[pk], writes=[("xtf", half)])
            S.act(lambda e, dst=dst, srcp=srcp: e.activation(out=dst, in_=srcp, func=AF.Copy),
                  reads=[pk] + ([("xtf", half)] if xtf is not None else []), writes=[("xT", b)])

    def ln_setup(self, sctx, layer, which):
        S = self.S
        g = self.inp("l%d_ln_%s_g" % (layer, which), [D])
        bta = self.inp("l%d_ln_%s_b" % (layer, which), [D])
        L = {}
        L["g"] = self.sb(sctx, "lng", [128, D], F32)
        L["b"] = self.sb(sctx, "lnb", [128, D], F32)
        S.dma("sp", lambda e: e.dma_start(out=L["g"][:], in_=g.partition_broadcast(128)), writes=["lng"])
        S.dma("sp", lambda e: e.dma_start(out=L["b"][:], in_=bta.partition_broadcast(128)), writes=["lnb"])
        L["xr"] = [self.sb(sctx, "lnxr", [128, D], F32) for _ in range(2)]
        L["s"] = [self.sb(sctx, "lns", [128, D], F32) for _ in range(2)]
        L["st"] = self.sb(sctx, "lnst", [128, 2, 2, 6], F32)
        L["mv"] = self.sb(sctx, "lnmv", [128, 2, 2], F32)
        L["sm"] = self.sb(sctx, "lnsm", [128, 2, 4], F32)
        L["n"] = 0
        return L

    def ln_load_x(self, L, b, i=None):
        S = self.S
        if i is None:
            i = L["n"] % 2
        t = L["xr"][i]
        S.dma("sp", lambda e: e.dma_start(out=t[:], in_=self.y[b * 128:(b + 1) * 128, :]),
              reads=[("y", b)], writes=[("lnxr", i)])
        return t, ("lnxr", i)

    def ln_finish(self, L, b, s, sk, router=None, i=None, part="all"):
        S = self.S
        if i is None:
            i = L["n"] % 2
            L["n"] += 1
        st, mv, sm = L["st"], L["mv"], L["sm"]
        if part in ("all", "stats"):
            self._ln_stats(L, s, sk, i)
        if part in ("all", "apply"):
            self._ln_apply(L, b, s, sk, i, router)

    def _ln_stats(self, L, s, sk, i):
        S = self.S
        st, mv, sm = L["st"], L["mv"], L["sm"]
        for h in range(2):
            S.dve(lambda e, h=h: e.bn_stats(out=st[:, i, h, :], in_=s[:, h * 512:(h + 1) * 512]),
                  reads=[sk], writes=[("lnst", i)])
        S.dve(lambda e: e.bn_aggr(out=mv[:, i, :], in_=st[:, i, :, :].rearrange("p a b -> p (a b)")),
              reads=[("lnst", i)], writes=[("lnmv", i)])
        S.act(lambda e: e.activation(out=sm[:, i, 0:1], in_=mv[:, i, 1:2], func=AF.Sqrt, bias=self.epsb[:, 0:1], scale=1.0),
              reads=[("lnmv", i), "epsb"], writes=[("lnsm0", i)])
        S.dve(lambda e: e.reciprocal(out=sm[:, i, 1:2], in_=sm[:, i, 0:1]), reads=[("lnsm0", i)], writes=[("lnsm1", i)])
        S.dve(lambda e: e.tensor_scalar(out=sm[:, i, 2:3], in0=mv[:, i, 0:1], scalar1=sm[:, i, 1:2], scalar2=-1.0,
                                        op0=ALU.mult, op1=ALU.mult),
              reads=[("lnmv", i), ("lnsm1", i)], writes=[("lnsm2", i)])

    def _ln_apply(self, L, b, s, sk, i, router):
        S = self.S
        sm = L["sm"]
        S.act(lambda e: e.activation(out=s[:], in_=s[:], func=AF.Identity, bias=sm[:, i, 2:3], scale=sm[:, i, 1:2]),
              reads=[sk, ("lnsm1", i), ("lnsm2", i)], writes=[sk])
        S.dve(lambda e: e.tensor_tensor(out=s[:], in0=s[:], in1=L["g"][:], op=ALU.mult), reads=[sk, "lng"], writes=[sk])
        S.dve(lambda e: e.tensor_tensor(out=s[:], in0=s[:], in1=L["b"][:], op=ALU.add), reads=[sk, "lnb"], writes=[sk])
        S.dma("sp", lambda e: e.dma_start(out=self.y[b * 128:(b + 1) * 128, :], in_=s[:]), reads=[sk], writes=[("y", b)], semkey=("yst", i))
        self.make_xT(s, sk, b, xtf=(router["xtf"] if router else None))
        if router:
            self.route(router, b)

    def ln_block(self, L, b, pouts, pkeys, scale_ap=None, acc=None, acck=None, i=None):
        S = self.S
        if i is None:
            i = L["n"] % 2
        s = L["s"][i]
        sk = ("lns", i)
        if acc is None:
            xr, xk = self.ln_load_x(L, b, i=i)
            for h in range(2):
                S.dve(lambda e, h=h, xr=xr: e.scalar_tensor_tensor(out=s[:, h * 512:(h + 1) * 512], in0=xr[:, h * 512:(h + 1) * 512],
                                                                   scalar=ALPHA, in1=pouts[h][:], op0=ALU.mult, op1=ALU.add),
                      reads=[xk, pkeys[h]], writes=[sk])
        else:
            for h in range(2):
                sc = 1.0 if scale_ap is None else scale_ap
                S.dve(lambda e, h=h, sc=sc: e.scalar_tensor_tensor(out=s[:, h * 512:(h + 1) * 512], in0=pouts[h][:],
                                                                   scalar=sc, in1=acc[:, h * 512:(h + 1) * 512],
                                                                   op0=ALU.mult, op1=ALU.add),
                      reads=[acck, pkeys[h], "gates"], writes=[sk])
        return s, sk

    def run_pipeline(self, its, stages, lags):
        offs = [0]
        for l in lags:
            offs.append(offs[-1] + l)
        n = len(its)
        for step in range(n + offs[-1]):
            for s in range(len(stages)):
                i = step - offs[s]
                if 0 <= i < n:
                    stages[s](its[i], i)

    def wload(self, dst, src_ap, key, reads=(), semkey=None):
        self.S.dma("pool", lambda e: e.dma_start(out=dst, in_=src_ap), reads=reads, writes=[key], semkey=semkey)

    def ffn_stage(self, sctx, layer, moe):
        S, P = self.S, self.P
        L = self.ln_setup(sctx, layer, "ffn")
        self.epsb = self.sb(sctx, "epsb", [128, 1], F32)
        S.dve(lambda e: e.memset(self.epsb[:], LN_EPS), writes=["epsb"])
        if moe:
            F = 3584
            experts = list(range(8))
            Wg = self.inp("l%d_moe_w_gate" % layer, [8, D, F])
            Wu = self.inp("l%d_moe_w_up" % layer, [8, D, F])
            Wd = self.inp("l%d_moe_w_down" % layer, [8, F, D])
            passes = [(e, j0, 7) for e in experts for j0 in (0, 7, 14, 21)]
        else:
            F = 2816
            Wg = self.inp("l%d_ffn_w_gate" % layer, [D, F])
            Wu = self.inp("l%d_ffn_w_up" % layer, [D, F])
            Wd = self.inp("l%d_ffn_w_down" % layer, [F, D])
            passes = [(None, 0, 8), (None, 8, 7), (None, 15, 7)]
        NH = 8
        acc = self.sb(sctx, "acc", [128, NB, D], F32)
        hT = self.sb(sctx, "hT", [128, NH, T], BF16)
        wd = self.sb(sctx, "wd", [128, NH, D], BF16)
        GW = 2
        wg = [self.sb(sctx, "wg", [128, 8, GW * 128], BF16) for _ in range(2)]
        wu = [self.sb(sctx, "wu", [128, 8, GW * 128], BF16) for _ in range(2)]
        sg = [self.sb(sctx, "sg", [128, 512], F32) for _ in range(2)]
        gi = 0
        si = 0
        for pi, (ex, j0, nch) in enumerate(passes):
            first, last = pi == 0, pi == len(passes) - 1
            wgs = Wg if ex is None else Wg[ex]
            wus = Wu if ex is None else Wu[ex]
            wds = Wd if ex is None else Wd[ex]
            for g0 in range(0, nch, GW):
                gn = min(GW, nch - g0)
                slot = gi % 2
                gi += 1
                c0 = (j0 + g0) * 128
                self.wload(wg[slot][:, :, 0:gn * 128], wgs[:, c0:c0 + gn * 128].rearrange("(k p) c -> p k c", p=128), ("wg", slot))
                self.wload(wu[slot][:, :, 0:gn * 128], wus[:, c0:c0 + gn * 128].rearrange("(k p) c -> p k c", p=128), ("wu", slot))
                for jj in range(g0, g0 + gn):
                    jl = jj - g0
                    for tt in range(4):
                        pg, pu = P[(si % 2)], P[2 + (si % 2)]
                        kg, ku = ("ps", si % 2), ("ps", 2 + si % 2)
                        sgt, sgk = sg[si % 2], ("sg", si % 2)
                        si += 1
                        xr = [("xT", tt * 4 + q) for q in range(4)]
                        for k in range(8):
                            S.pe(lambda e, pg=pg, k=k, slot=slot, jl=jl, tt=tt: e.matmul(
                                pg[:], lhsT=wg[slot][:, k, jl * 128:(jl + 1) * 128], rhs=self.xT[:, k, tt * 512:(tt + 1) * 512],
                                start=(k == 0), stop=(k == 7)), reads=[("wg", slot)] + xr, writes=[kg])
                        for k in range(8):
                            S.pe(lambda e, pu=pu, k=k, slot=slot, jl=jl, tt=tt: e.matmul(
                                pu[:], lhsT=wu[slot][:, k, jl * 128:(jl + 1) * 128], rhs=self.xT[:, k, tt * 512:(tt + 1) * 512],
                                start=(k == 0), stop=(k == 7)), reads=[("wu", slot)] + xr, writes=[ku])
                        S.act(lambda e, pg=pg, sgt=sgt: e.activation(out=sgt[:], in_=pg[:], func=AF.Silu),
                              reads=[kg], writes=[sgk])
                        S.dve(lambda e, pu=pu, sgt=sgt, jj=jj, tt=tt: e.tensor_tensor(
                            out=hT[:, jj, tt * 512:(tt + 1) * 512], in0=sgt[:], in1=pu[:], op=ALU.mult),
                            reads=[sgk, ku], writes=[("hT", jj, tt)])
            for jj in range(nch):
                r0 = (j0 + jj) * 128
                self.wload(wd[:, jj, :], wds[r0:r0 + 128, :], ("wd", jj))
            for b in range(NB):
                po = [P[4 + 2 * (b % 2)], P[5 + 2 * (b % 2)]]
                pk = [("ps", 4 + 2 * (b % 2)), ("ps", 5 + 2 * (b % 2))]
                for jj in range(nch):
                    for h in range(2):
                        S.pe(lambda e, jj=jj, h=h, b=b, po=po: e.matmul(
                            po[h][:], lhsT=hT[:, jj, b * 128:(b + 1) * 128], rhs=wd[:, jj, h * 512:(h + 1) * 512],
                            start=(jj == 0), stop=(jj == nch - 1)),
                            reads=[("hT", jj, b // 4), ("wd", jj)], writes=[pk[h]])
                gate = None if ex is None else self.gates[:, b, ex:ex + 1]
                acck = ("acc", b)
                if first:
                    xr, xk = self.ln_load_x(L, b)
                    L["n"] += 1
                    for h in range(2):
                        if gate is None:
                            S.dve(lambda e, h=h, xr=xr, b=b, po=po: e.scalar_tensor_tensor(
                                out=acc[:, b, h * 512:(h + 1) * 512], in0=xr[:, h * 512:(h + 1) * 512], scalar=ALPHA,
                                in1=po[h][:], op0=ALU.mult, op1=ALU.add), reads=[xk, pk[h]], writes=[acck])
                        else:
                            S.act(lambda e, h=h, xr=xr, b=b: e.activation(out=acc[:, b, h * 512:(h + 1) * 512],
                                                                          in_=xr[:, h * 512:(h + 1) * 512], func=AF.Copy, scale=ALPHA),
                                  reads=[xk], writes=[acck])
                            S.dve(lambda e, h=h, b=b, po=po, gate=gate: e.scalar_tensor_tensor(
                                out=acc[:, b, h * 512:(h + 1) * 512], in0=po[h][:], scalar=gate,
                                in1=acc[:, b, h * 512:(h + 1) * 512], op0=ALU.mult, op1=ALU.add),
                                reads=[acck, pk[h], "gates"], writes=[acck])
                elif not last:
                    for h in range(2):
                        S.dve(lambda e, h=h, b=b, po=po, gate=gate: e.scalar_tensor_tensor(
                            out=acc[:, b, h * 512:(h + 1) * 512], in0=po[h][:], scalar=(1.0 if gate is None else gate),
                            in1=acc[:, b, h * 512:(h + 1) * 512], op0=ALU.mult, op1=ALU.add),
                            reads=[acck, pk[h], "gates"], writes=[acck])
                else:
                    s, sk = self.ln_block(L, b, po, pk, scale_ap=gate, acc=acc[:, b, :], acck=acck, i=b % 2)
                    self.ln_finish(L, b, s, sk, i=b % 2, part="stats")
                    if b > 0:
                        self.ln_finish(L, b - 1, prev_s[0], prev_s[1], i=(b - 1) % 2, part="apply")
                    prev_s = (s, sk)
                    if b == NB - 1:
                        self.ln_finish(L, b, s, sk, i=b % 2, part="apply")

    def moe_stage(self, sctx, layer):
        S, P, nc = self.S, self.P, self.nc
        import os
        NSB = int(os.environ.get("KDBG_NSB", "5"))
        CS = NSB * 128
        tiles = [(0, min(512, CS))] + ([(512, CS - 512)] if CS > 512 else [])
        F = 3584
        Wg = self.inp("l%d_moe_w_gate" % layer, [8, D, F])
        Wu = self.inp("l%d_moe_w_up" % layer, [8, D, F])
        Wd = self.inp("l%d_moe_w_down" % layer, [8, F, D])
        self.epsb = self.sb(sctx, "epsb", [128, 1], F32)
        S.dve(lambda e: e.memset(self.epsb[:], LN_EPS), writes=["epsb"])
        acc = self.sb(sctx, "acc", [128, NB, D], F32)
        with ExitStack() as mx:
            xtok = self.sb(mx, "xtok", [128, NB, D], BF16)
            flat = self.xT[:].rearrange("p k t -> p (k t)")
            xgT = flat[:, 0:8 * CS].rearrange("p (k c) -> p k c", k=8)
            hT2 = flat[:, 8 * CS:15 * CS].rearrange("p (k c) -> p k c", k=7)
            yb = flat[:, 15 * CS:15 * CS + NSB * D].rearrange("p (s d) -> p s d", s=NSB)
            Sx = self.sb(mx, "Sx", [128, NB, CS], BF16)
            STr = [self.sb(mx, "STr", [128, NSB, 512], BF16) for _ in range(1)]
            wd = self.sb(mx, "wd", [128, 7, D], BF16)
            wg = [self.sb(mx, "wg", [128, 8, 128], BF16) for _ in range(2)]
            wu = [self.sb(mx, "wu", [128, 8, 128], BF16) for _ in range(2)]
            sg = [self.sb(mx, "sg", [128, 512], F32) for _ in range(1)]
            yacc = self.sb(mx, "yacc", [128, NSB, D], F32)
            maskf = self.sb(mx, "maskf", [128, NB, 8], F32)
            maskb = self.sb(mx, "maskb", [128, NB, 32], BF16)
            nmb = self.sb(mx, "nmb", [128, NB, 8], BF16)
            rk = self.sb(mx, "rk", [128, NB, 8], F32)
            ioi = self.sb(mx, "ioi", [128, CS], mybir.dt.int32)
            cii = self.sb(mx, "cii", [128, NSB], mybir.dt.int32)
            cidx = self.sb(mx, "cidx", [128, NSB], F32)
            io = ioi
            rk2 = self.sb(mx, "rk2", [128, NB, 8], F32)
            cid2 = self.sb(mx, "cid2", [128, NSB], F32)
            cnti = self.sb(mx, "cnti", [128, 32], mybir.dt.int32)
            cntD = nc.dram_tensor("cntD%d" % layer, [1, 32], mybir.dt.int32, kind="Internal").ap()
            UW = self.sb(mx, "UW", [128, 896], BF16)
            ones5 = self.onesb[:, 0:1].to_broadcast([128, 512])
            for b in range(NB):
                self.wload(xtok[:, b, :], self.y[b * 128:(b + 1) * 128, :], ("xtok", b), semkey="xtokall")
                S.dma("sp", lambda e, b=b: e.dma_start(out=acc[:, b, :], in_=self.y[b * 128:(b + 1) * 128, :]), writes=[("accld", b)],
                      semkey="accld")
            for b in range(NB):
                S.act(lambda e, b=b: e.activation(out=acc[:, b, :], in_=acc[:, b, :], func=AF.Copy, scale=ALPHA),
                      reads=[("accld", bb) for bb in range(NB)], writes=[("acc", b)])
            S.dve(lambda e: e.tensor_scalar(out=maskf[:], in0=self.gates[:], scalar1=0.0, scalar2=None, op0=ALU.is_gt),
                  reads=["gates"], writes=["maskf"])
            S.dve(lambda e: e.memset(maskb[:], 0.0), writes=["maskb"])
            S.dve(lambda e: e.tensor_copy(out=maskb[:, :, 0:8], in_=maskf[:]), reads=["maskf", "maskb"], writes=["maskb"])
            S.dve(lambda e: e.tensor_scalar(out=nmb[:], in0=maskf[:], scalar1=-4096.0, scalar2=4096.0, op0=ALU.mult, op1=ALU.add),
                  reads=["maskf"], writes=["nmb"])
            S.pool(lambda e: e.iota(out=ioi[:], pattern=[[1, CS]], base=0, channel_multiplier=0), writes=["ioi"])
            S.dve(lambda e: e.memset(cidx[:], 0.0), reads=["ioi"], writes=["io"])
            S.pool(lambda e: e.iota(out=cii[:], pattern=[[128, NSB]], base=0, channel_multiplier=1), writes=["cii"])
            S.dve(lambda e: e.tensor_copy(out=cidx[:], in_=cii[:]), reads=["cii"], writes=["cidx"])
            S.dve(lambda e: e.memset(UW[:, 0:384], 0.0), writes=["U4"])
            S.dve(lambda e: e.tensor_copy(out=UW[:, 384:512], in_=self.mstrict[:]), reads=["mstrict", "U4"], writes=["U4"])
            S.dve(lambda e: e.memset(UW[:, 512:896], 1.0), reads=["U4"], writes=["U4"])
            for b in range(NB):
                pr, prk = P[b % 2], ("ps", b % 2)
                for b2 in range(b + 1):
                    lhs = self.onesb[:] if b2 < b else self.mstrict[:]
                    S.pe(lambda e, b2=b2, lhs=lhs, pr=pr, b=b: e.matmul(pr[:, 0:32], lhsT=lhs, rhs=maskb[:, b2, :], start=(b2 == 0), stop=(b2 == b)),
                         reads=["maskb", "onesb", "mstrict"], writes=[prk])
                S.dve(lambda e, b=b, pr=pr: e.tensor_copy(out=rk[:, b, :], in_=pr[:, 0:8]), reads=[prk], writes=["rk"])
            for b in range(NB):
                S.pe(lambda e, b=b: e.matmul(P[2][:, 0:32], lhsT=self.onesb[:], rhs=maskb[:, b, :], start=(b == 0), stop=(b == NB - 1)),
                     reads=["maskb", "onesb"], writes=[("ps", 2)])
            S.dve(lambda e: e.tensor_copy(out=cnti[:], in_=P[2][:, 0:32]), reads=[("ps", 2)], writes=["cnti"])
            S.dma("sp", lambda e: e.dma_start(out=cntD, in_=cnti[0:1, :]), reads=["cnti"], writes=["cntD"])
            gi = 0
            si = 0
            gn = 0
            NROUND = (T + CS - 1) // CS
            for ex in range(8):
                for rnd in range(NROUND):
                    R0 = rnd * CS
                    if rnd == 0:
                        rkR, cidR = rk, cidx
                    else:
                        if rnd == 1:
                            for en in ("pe", "act", "dve", "pool", "sp"):
                                S.add(en, lambda e: e.nop(), reads=["cntD"])
                            S.ctl("if", ap=cntD[0:1, ex:ex + 1], thresh=CS)
                        rkR, cidR = rk2, cid2
                        S.dve(lambda e, R0=R0: e.tensor_scalar(out=rk2[:], in0=rk[:], scalar1=float(-R0), scalar2=None, op0=ALU.add),
                              reads=["rk"], writes=["rk2"])
                        S.dve(lambda e, R0=R0: e.tensor_scalar(out=cid2[:], in0=cidx[:], scalar1=float(R0), scalar2=None, op0=ALU.add),
                              reads=["cidx"], writes=["cid2"])
                    for b in range(NB):
                        S.dve(lambda e, b=b, ex=ex, rkR=rkR: e.tensor_scalar(out=Sx[:, b, :], in0=io[:], scalar1=rkR[:, b, ex:ex + 1],
                                                                    scalar2=maskf[:, b, ex:ex + 1], op0=ALU.is_equal, op1=ALU.mult),
                              reads=["io", "rk", "rk2", "maskf"], writes=[("Sx", b)])
                    for k in range(8):
                        for (c0, cw) in tiles:
                            pgk = gn % 2
                            gn += 1
                            pg, pgkk = P[pgk], ("ps", pgk)
                            for b in range(NB):
                                S.pe(lambda e, b=b, k=k, c0=c0, cw=cw, pg=pg: e.matmul(pg[:, 0:cw], lhsT=xtok[:, b, k * 128:(k + 1) * 128],
                                                                                      rhs=Sx[:, b, c0:c0 + cw], start=(b == 0), stop=(b == NB - 1)),
                                     reads=[("xtok", bb) for bb in range(NB)] + [("Sx", b)], writes=[pgkk])
                            S.act(lambda e, k=k, c0=c0, cw=cw, pg=pg: e.activation(out=xgT[:, k, c0:c0 + cw], in_=pg[:, 0:cw], func=AF.Copy),
                                  reads=[pgkk], writes=[("xgT", k)])
                    xgk = [("xgT", k) for k in range(8)]
                    for pi, j0 in enumerate((0, 7, 14, 21)):
                        nch = 7
                        firstp, lastp = pi == 0, pi == 3
                        for jj in range(nch):
                            slot = gi % 2
                            gi += 1
                            c0 = (j0 + jj) * 128
                            self.wload(wg[slot][:], Wg[ex][:, c0:c0 + 128].rearrange("(k p) c -> p k c", p=128), ("wg", slot))
                            self.wload(wu[slot][:], Wu[ex][:, c0:c0 + 128].rearrange("(k p) c -> p k c", p=128), ("wu", slot))
                            for (t0, tw) in tiles:
                                pg, pu = P[(si % 2)], P[2 + (si % 2)]
                                kg, ku = ("ps", si % 2), ("ps", 2 + si % 2)
                                sgt, sgk = sg[0], ("sg", 0)
                                si += 1
                                for k in range(8):
                                    S.pe(lambda e, pg=pg, k=k, slot=slot, t0=t0, tw=tw: e.matmul(
                                        pg[:, 0:tw], lhsT=wg[slot][:, k, :], rhs=xgT[:, k, t0:t0 + tw], start=(k == 0), stop=(k == 7)),
                                        reads=[("wg", slot)] + xgk, writes=[kg])
                                for k in range(8):
                                    S.pe(lambda e, pu=pu, k=k, slot=slot, t0=t0, tw=tw: e.matmul(
                                        pu[:, 0:tw], lhsT=wu[slot][:, k, :], rhs=xgT[:, k, t0:t0 + tw], start=(k == 0), stop=(k == 7)),
                                        reads=[("wu", slot)] + xgk, writes=[ku])
                                S.act(lambda e, pg=pg, sgt=sgt, tw=tw: e.activation(out=sgt[:, 0:tw], in_=pg[:, 0:tw], func=AF.Silu),
                                      reads=[kg], writes=[sgk])
                                S.dve(lambda e, pu=pu, sgt=sgt, jj=jj, t0=t0, tw=tw: e.tensor_tensor(
                                    out=hT2[:, jj, t0:t0 + tw], in0=sgt[:, 0:tw], in1=pu[:, 0:tw], op=ALU.mult),
                                    reads=[sgk, ku], writes=[("hT2", jj)])
                        for jj in range(nch):
                            r0 = (j0 + jj) * 128
                            self.wload(wd[:, jj, :], Wd[ex][r0:r0 + 128, :], ("wd", jj))
                        for sbk in range(NSB):
                            po = [P[4 + 2 * (sbk % 2)], P[5 + 2 * (sbk % 2)]]
                            pk = [("ps", 4 + 2 * (sbk % 2)), ("ps", 5 + 2 * (sbk % 2))]
                            for jj in range(nch):
                                for h in range(2):
                                    S.pe(lambda e, jj=jj, h=h, sbk=sbk, po=po: e.matmul(
                                        po[h][:], lhsT=hT2[:, jj, sbk * 128:(sbk + 1) * 128], rhs=wd[:, jj, h * 512:(h + 1) * 512],
                                        start=(jj == 0), stop=(jj == nch - 1)), reads=[("hT2", jj), ("wd", jj)], writes=[pk[h]])
                            for h in range(2):
                                if firstp:
                                    S.act(lambda e, h=h, sbk=sbk, po=po: e.activation(out=yacc[:, sbk, h * 512:(h + 1) * 512], in_=po[h][:], func=AF.Copy),
                                          reads=[pk[h]], writes=[("yacc", sbk)])
                                elif not lastp:
                                    S.dve(lambda e, h=h, sbk=sbk, po=po: e.tensor_tensor(out=yacc[:, sbk, h * 512:(h + 1) * 512], in0=yacc[:, sbk, h * 512:(h + 1) * 512],
                                                                                       in1=po[h][:], op=ALU.add), reads=[pk[h], ("yacc", sbk)], writes=[("yacc", sbk)])
                                else:
                                    S.dve(lambda e, h=h, sbk=sbk, po=po: e.tensor_tensor(out=yb[:, sbk, h * 512:(h + 1) * 512], in0=yacc[:, sbk, h * 512:(h + 1) * 512],
                                                                                       in1=po[h][:], op=ALU.add), reads=[pk[h], ("yacc", sbk)], writes=[("yb", sbk)])
                    ybk = [("yb", s_) for s_ in range(NSB)]
                    for tq in range(4):
                        prb, prbk = P[0], ("ps", 0)
                        nb2 = 4 * tq + 4
                        for b2 in range(nb2):
                            rhs = ones5 if b2 < 4 * tq else UW[:, (3 - (b2 - 4 * tq)) * 128:(3 - (b2 - 4 * tq)) * 128 + 512]
                            S.pe(lambda e, b2=b2, rhs=rhs, ex=ex: e.matmul(prb[:], lhsT=maskb[:, b2, ex:ex + 1].to_broadcast([128, 128]), rhs=rhs,
                                                                          start=(b2 == 0), stop=False),
                                 reads=["maskb", "onesb", "U4"], writes=[prbk])
                        for q in range(4):
                            b2 = 4 * tq + q
                            S.pe(lambda e, b2=b2, q=q, ex=ex: e.matmul(prb[:, q * 128:(q + 1) * 128], lhsT=nmb[:, b2, ex:ex + 1].to_broadcast([128, 128]),
                                                                      rhs=self.identb[:], start=False, stop=(q == 3)),
                                 reads=["nmb", "identb"], writes=[prbk])
                        st_ = STr[0]
                        for sbk in range(NSB):
                            S.dve(lambda e, sbk=sbk, cidR=cidR: e.tensor_scalar(out=st_[:, sbk, :], in0=prb[:], scalar1=cidR[:, sbk:sbk + 1], scalar2=None,
                                                                     op0=ALU.is_equal), reads=[prbk, "cidx", "cid2"], writes=[("STr", sbk)])
                        for q in range(4):
                            b = 4 * tq + q
                            po = [P[4 + 2 * (b % 2)], P[5 + 2 * (b % 2)]]
                            pk = [("ps", 4 + 2 * (b % 2)), ("ps", 5 + 2 * (b % 2))]
                            for sbk in range(NSB):
                                for h in range(2):
                                    S.pe(lambda e, sbk=sbk, h=h, q=q, po=po: e.matmul(po[h][:], lhsT=st_[:, sbk, q * 128:(q + 1) * 128],
                                                                                  rhs=yb[:, sbk, h * 512:(h + 1) * 512],
                                                                                  start=(sbk == 0), stop=(sbk == NSB - 1)),
                                         reads=[("STr", sbk), ("yb", sbk)], writes=[pk[h]])
                            gate = self.gates[:, b, ex:ex + 1]
                            for h in range(2):
                                S.dve(lambda e, h=h, b=b, po=po, gate=gate: e.scalar_tensor_tensor(
                                    out=acc[:, b, h * 512:(h + 1) * 512], in0=po[h][:], scalar=gate,
                                    in1=acc[:, b, h * 512:(h + 1) * 512], op0=ALU.mult, op1=ALU.add),
                                    reads=[("acc", b), pk[h], "gates"], writes=[("acc", b)])

                    if rnd == NROUND - 1:
                        S.ctl("endif")
            S.flush(barrier=True)
        L = self.ln_setup(sctx, layer, "ffn")
        self.run_pipeline(list(range(NB)),
                          [lambda b, n: self.ln_finish(L, b, acc[:, b, :], ("acc", b), i=b % 2, part="stats"),
                           lambda b, n: self.ln_finish(L, b, acc[:, b, :], ("acc", b), i=b % 2, part="apply")], [1])

    def next_router(self, layer, which):
        import os
        if os.environ.get("KDBG_NOROUTER"):
            return None
        return self.router if (which == "mix" and layer % 2 == 1) else None

    def router_setup(self, sctx, layer):
        S = self.S
        R = {}
        wr = self.inp("l%d_moe_w_router" % layer, [D, 8])
        R["wr"] = self.sb(sctx, "wr", [128, 8, 8], F32)
        with self.nc.allow_non_contiguous_dma(reason="tiny router weight"):
            S.dma("sp", lambda e: e.dma_start(out=R["wr"][:], in_=wr.rearrange("(k p) e -> p k e", p=128)), writes=["wr"])
        R["xtf"] = self.sb(sctx, "xtf", [128, 8, 128], F32)
        R["xh"] = self.sb(sctx, "xh", [128, 8, 128], BF16)
        R["xl"] = self.sb(sctx, "xl", [128, 8, 128], BF16)
        R["wrh"] = self.sb(sctx, "wrh", [128, 8, 32], BF16)
        R["wrl"] = self.sb(sctx, "wrl", [128, 8, 32], BF16)
        S.dve(lambda e: e.memset(R["wrh"][:], 0.0), writes=["wrh"])
        S.dve(lambda e: e.memset(R["wrl"][:], 0.0), writes=["wrl"])
        R["t"] = self.sb(sctx, "rt", [128, 8, 8], F32)
        S.dve(lambda e: e.tensor_copy(out=R["wrh"][:, :, 0:8], in_=R["wr"][:]), reads=["wr", "wrh"], writes=["wrh"])
        S.dve(lambda e: e.tensor_tensor(out=R["wrl"][:, :, 0:8], in0=R["wr"][:], in1=R["wrh"][:, :, 0:8], op=ALU.subtract),
              reads=["wr", "wrh", "wrl"], writes=["wrl"])
        self.router = R
        return R

    def route(self, R, b):
        S, P = self.S, self.P
        pr, pk = P[5], ("ps", 5)
        t = R["t"]
        xk = [("xtf", 0), ("xtf", 1)]
        S.dve(lambda e: e.tensor_copy(out=R["xh"][:], in_=R["xtf"][:]), reads=xk, writes=["xh"])
        S.dve(lambda e: e.tensor_tensor(out=R["xl"][:], in0=R["xtf"][:], in1=R["xh"][:], op=ALU.subtract),
              reads=xk + ["xh"], writes=["xl"])
        combos = [("xh", "wrh"), ("xh", "wrl"), ("xl", "wrh")]
        n = 0
        for k in range(8):
            for (xa, wa) in combos:
                S.pe(lambda e, k=k, xa=xa, wa=wa, n=n: e.matmul(pr[:, 0:32], lhsT=R[xa][:, k, :], rhs=R[wa][:, k, :],
                                                              start=(n == 0), stop=(n == 23)),
                     reads=[xa, wa], writes=[pk])
                n += 1
        lg, mask, ex = t[:, 0, :], t[:, 2, :], t[:, 4, :]
        m1, m2, nm1, den = t[:, 1, 0:1], t[:, 1, 1:2], t[:, 3, 0:1], t[:, 5, 0:1]
        w4, w2, lg2 = t[:, 6, 0:4], t[:, 6, 4:6], t[:, 7, :]
        S.dve(lambda e: e.tensor_copy(out=lg, in_=pr[:, 0:8]), reads=[pk], writes=["r_lg"])

        def max8(src_ap, dst, key_in, key_out):
            S.dve(lambda e: e.tensor_tensor(out=w4, in0=src_ap[:, 0:4], in1=src_ap[:, 4:8], op=ALU.max), reads=[key_in], writes=["r_w4"])
            S.dve(lambda e: e.tensor_tensor(out=w2, in0=t[:, 6, 0:2], in1=t[:, 6, 2:4], op=ALU.max), reads=["r_w4"], writes=["r_w2"])
            S.dve(lambda e: e.tensor_tensor(out=dst, in0=t[:, 6, 4:5], in1=t[:, 6, 5:6], op=ALU.max), reads=["r_w2"], writes=[key_out])
        max8(lg, m1, "r_lg", "r_m1")
        S.dve(lambda e: e.tensor_scalar(out=lg2, in0=lg, scalar1=m1, scalar2=-1e30, op0=ALU.is_equal, op1=ALU.mult),
              reads=["r_lg", "r_m1"], writes=["r_lg2"])
        S.dve(lambda e: e.tensor_tensor(out=lg2, in0=lg2, in1=lg, op=ALU.add), reads=["r_lg2", "r_lg"], writes=["r_lg2"])
        max8(lg2, m2, "r_lg2", "r_m2")
        S.dve(lambda e: e.tensor_scalar(out=mask, in0=lg, scalar1=m2, scalar2=None, op0=ALU.is_ge),
              reads=["r_lg", "r_m2"], writes=["r_mask"])
        S.dve(lambda e: e.tensor_scalar(out=nm1, in0=m1, scalar1=-1.0, scalar2=None, op0=ALU.mult),
              reads=["r_m1"], writes=["r_nm1"])
        S.act(lambda e: e.activation(out=ex, in_=lg, func=AF.Exp, bias=nm1, scale=1.0), reads=["r_lg", "r_nm1"], writes=["r_ex"])
        S.dve(lambda e: e.tensor_tensor(out=ex, in0=ex, in1=mask, op=ALU.mult), reads=["r_ex", "r_mask"], writes=["r_ex"])
        S.dve(lambda e: e.tensor_tensor(out=w4, in0=t[:, 4, 0:4], in1=t[:, 4, 4:8], op=ALU.add), reads=["r_ex"], writes=["r_w4"])
        S.dve(lambda e: e.tensor_tensor(out=w2, in0=t[:, 6, 0:2], in1=t[:, 6, 2:4], op=ALU.add), reads=["r_w4"], writes=["r_w2"])
        S.dve(lambda e: e.tensor_tensor(out=den, in0=t[:, 6, 4:5], in1=t[:, 6, 5:6], op=ALU.add), reads=["r_w2"], writes=["r_den"])
        S.dve(lambda e: e.reciprocal(out=den, in_=den), reads=["r_den"], writes=["r_den"])
        S.dve(lambda e: e.tensor_scalar(out=self.gates[:, b, :], in0=ex, scalar1=den, scalar2=None, op0=ALU.mult),
              reads=["r_ex", "r_den"], writes=["gates"])

    def mixer_out(self, sctx, layer, lhs_fn, lhs_keys_fn, nk, Wo_ap, L, pre_block=None):
        S, P = self.S, self.P
        wo = self.sb(sctx, "wo", [128, nk, D], BF16)
        for k in range(nk):
            self.wload(wo[:, k, :], Wo_ap[k * 128:(k + 1) * 128, :], ("wo", k), semkey=("wo", k % 4))
        rt = self.next_router(layer, "mix")
        hold = {}

        def stA(b, n):
            po = [P[2 * (b % 2)], P[1 + 2 * (b % 2)]]
            pk = [("ps", 2 * (b % 2)), ("ps", 1 + 2 * (b % 2))]
            if pre_block is not None:
                pre_block(b)
            for k in range(nk):
                for h in range(2):
                    S.pe(lambda e, k=k, h=h, b=b, po=po: e.matmul(po[h][:], lhsT=lhs_fn(k, b), rhs=wo[:, k, h * 512:(h + 1) * 512],
                                                              start=(k == 0), stop=(k == nk - 1)),
                         reads=lhs_keys_fn(k, b) + [("wo", kk) for kk in range(k % 4, nk, 4)], writes=[pk[h]])
            s, sk = self.ln_block(L, b, po, pk, i=b % 2)
            self.ln_finish(L, b, s, sk, i=b % 2, part="stats")
            hold[b] = (s, sk)

        def stB(b, n):
            s, sk = hold.pop(b)
            self.ln_finish(L, b, s, sk, router=rt, i=b % 2, part="apply")
        self.run_pipeline(list(range(NB)), [stA, stB], [1])

    def attn_stage(self, sctx, layer, kind):
        S, P = self.S, self.P
        fox = kind == "fox"
        L = self.ln_setup(sctx, layer, "mix")
        self.epsb = self.sb(sctx, "epsb", [128, 1], F32)
        S.dve(lambda e: e.memset(self.epsb[:], LN_EPS), writes=["epsb"])
        if layer % 2 == 1:
            self.router_setup(sctx, layer)
        if fox:
            Wq = self.inp("l%d_fox_w_qkvf" % layer, [D, 3088])
            Wo = self.inp("l%d_fox_w_out" % layer, [D, D])
            bf = self.inp("l%d_fox_b_f" % layer, [16])
        else:
            Wq = self.inp("l%d_sb_w_qkv" % layer, [D, 3072])
            Wo = self.inp("l%d_sb_w_out" % layer, [D, D])
        OT = self.sb(sctx, "OT", [128, 8, T], BF16)
        qT = [self.sb(sctx, "qT", [128, T], BF16) for _ in range(2)]
        kT = [None, None]
        vv = [self.sb(sctx, "vv", [128, NB, 128], BF16) for _ in range(2)]
        wq = [self.sb(sctx, "wq", [128, 8, 384], BF16) for _ in range(2)]
        E = [self.sb(sctx, "E", [128, 512], BF16) for _ in range(4)]
        if fox:
            wf = self.sb(sctx, "wf", [128, 8, 16], BF16)
            nbf = self.sb(sctx, "nbf", [16, 1], F32)
            cnT = self.sb(sctx, "cnT", [128, NB, 16], F32)
            c3 = [self.sb(sctx, "c3", [128, 2 * T], BF16) for _ in range(2)]
            rl = self.sb(sctx, "rl", [128, 512], F32)
            kTp = [[self.sb(sctx, "kTp", [128, T], BF16) for _ in range(2)] for _ in range(2)]
            ones3p = self.sb(sctx, "ones3p", [128, 128], BF16)
            fpx = ExitStack()
            spf = self.sb(fpx, "spf", [16, T], F32)
            cn = self.sb(fpx, "cn", [16, T], F32)
            tmpf = self.sb(fpx, "tmpf", [16, T], F32)
            c3b = self.sb(fpx, "c3b", [16, 3, T], BF16)
            S.dve(lambda e: e.memset(ones3p[:], 0.0), writes=["ones3p"])
            S.dve(lambda e: e.memset(ones3p[0:3, :], 1.0), reads=["ones3p"], writes=["ones3p"])
            for i_ in range(2):
                S.dve(lambda e, i_=i_: e.memset(c3[i_][:], 0.0), writes=[("c3", i_)])
                for j_ in range(2):
                    S.dve(lambda e, i_=i_, j_=j_: e.memset(kTp[i_][j_][:], 0.0), writes=[("kTpz", i_, j_)])
            with self.nc.allow_non_contiguous_dma(reason="tiny"):
                self.wload(wf[:], Wq[:, 3072:3088].rearrange("(k p) c -> p k c", p=128), "wf")
                S.dma("sp", lambda e: e.dma_start(out=nbf[:], in_=bf.rearrange("(p o) -> p o", o=1)), writes=["nbf"])
            S.dve(lambda e: e.tensor_scalar(out=nbf[:], in0=nbf[:], scalar1=-1.0, scalar2=None, op0=ALU.mult),
                  reads=["nbf"], writes=["nbf"])
            for tt in range(4):
                pf, pfk = P[6 + tt % 2], ("ps", 6 + tt % 2)
                for k in range(8):
                    S.pe(lambda e, k=k, tt=tt, pf=pf: e.matmul(pf[0:16, :], lhsT=wf[:, k, :], rhs=self.xT[:, k, tt * 512:(tt + 1) * 512],
                                                            start=(k == 0), stop=(k == 7)),
                         reads=["wf"] + [("xT", tt * 4 + q) for q in range(4)], writes=[pfk])
                S.act(lambda e, tt=tt, pf=pf: e.activation(out=tmpf[:, tt * 512:(tt + 1) * 512], in_=pf[0:16, :], func=AF.Exp,
                                                        bias=nbf[:, 0:1], scale=-1.0), reads=[pfk, "nbf"], writes=[("tmpf", tt)])
                S.act(lambda e, tt=tt: e.activation(out=spf[:, tt * 512:(tt + 1) * 512], in_=tmpf[:, tt * 512:(tt + 1) * 512],
                                                    func=AF.Ln, bias=self.oneb[0:16, 0:1], scale=1.0),
                      reads=[("tmpf", tt), "oneb"], writes=[("spf", tt)])
            allsp = [("spf", tt) for tt in range(4)]
            S.dve(lambda e: e.memset(tmpf[:], 1.0), writes=[("tmpf", tt) for tt in range(4)] + ["m8"])
            S.dve(lambda e: e.tensor_tensor_scan(out=cn[:], data0=tmpf[:], data1=spf[:], initial=0.0, op0=ALU.mult, op1=ALU.add),
                  reads=allsp + ["m8"], writes=["cn"])
            for b in range(NB):
                pt, ptk = P[6 + b % 2], ("ps", 6 + b % 2)
                S.pe(lambda e, b=b, pt=pt: e.transpose(out=pt[:, 0:16], in_=cn[:, b * 128:(b + 1) * 128], identity=self.identf[0:16, 0:16]),
                     reads=["cn", "identf"], writes=[ptk])
                S.dve(lambda e, b=b, pt=pt: e.tensor_copy(out=cnT[:, b, :], in_=pt[:, 0:16]), reads=[ptk], writes=["cnT"])
            S.dve(lambda e: e.tensor_scalar(out=tmpf[:], in0=cn[:], scalar1=-8.0, scalar2=None, op0=ALU.mult),
                  reads=["cn"] + [("tmpf", tt) for tt in range(4)], writes=["m8"])
            for i in range(3):
                S.dve(lambda e, i=i: e.tensor_copy(out=c3b[:, i, :], in_=tmpf[:]), reads=["m8"], writes=[("c3b", i)])
                if i < 2:
                    S.dve(lambda e, i=i: e.tensor_tensor(out=tmpf[:], in0=tmpf[:], in1=c3b[:, i, :], op=ALU.subtract),
                          reads=["m8", ("c3b", i)], writes=["m8"])
            S.dma("sp", lambda e: e.dma_start(out=self.scr[0, :, :].rearrange("i (h t) -> h i t", h=16), in_=c3b[:]),
                  reads=[("c3b", i) for i in range(3)], writes=["scr0"])
            S.flush(barrier=True)
            fpx.close()
        else:
            zs = [self.sb(sctx, "zs", [128, 512], F32) for _ in range(4)]
            ez = [self.sb(sctx, "ez", [128, 512], F32) for _ in range(2)]
            spb = [self.sb(sctx, "spb", [128, 512], BF16) for _ in range(4)]
            lw = [self.sb(sctx, "lw", [128, 512], F32) for _ in range(2)]
            zerob = self.sb(sctx, "zerob", [128, 128], BF16)
            S.dve(lambda e: e.memset(zerob[:], 0.0), writes=["zerob"])
            kTp = [[self.sb(sctx, "kTp", [128, T], BF16) for _ in range(2)] for _ in range(2)]
            for i_ in range(2):
                for j_ in range(2):
                    S.dve(lambda e, i_=i_, j_=j_: e.memset(kTp[i_][j_][:], 0.0), writes=[("kTpz", i_, j_)])
        self.oneb_needed = True
        ei = 0
        for c in range(8):
            sl = c % 2
            for i, off in enumerate((0, 1024, 2048)):
                self.wload(wq[sl][:, :, i * 128:(i + 1) * 128],
                           Wq[:, off + c * 128:off + (c + 1) * 128].rearrange("(k p) c -> p k c", p=128), ("wq", sl, i))
            if fox:
                S.dma("sp", lambda e, sl=sl, c=c: e.dma_start(out=c3[sl][0:3, :], in_=self.scr[0, :, 2 * c * T:(2 * c + 2) * T]),
                      reads=["scr0"], writes=[("c3", sl)])
            for i, dst in enumerate((qT[sl], kT[sl])):
                nm = ("qT", "kT")[i]
                for tt in range(4):
                    pp, ppk = P[6 + tt % 2], ("ps", 6 + tt % 2)
                    for k in range(8):
                        S.pe(lambda e, k=k, tt=tt, pp=pp, i=i, sl=sl: e.matmul(pp[:], lhsT=wq[sl][:, k, i * 128:(i + 1) * 128],
                                                                          rhs=self.xT[:, k, tt * 512:(tt + 1) * 512],
                                                                          start=(k == 0), stop=(k == 7)),
                             reads=[("wq", sl, i)] + [("xT", tt * 4 + q) for q in range(4)], writes=[ppk])
                    if i == 1:
                        S.act(lambda e, tt=tt, pp=pp, sl=sl: e.activation(out=kTp[sl][0][0:64, tt * 512:(tt + 1) * 512], in_=pp[0:64, :], func=AF.Copy),
                              reads=[ppk, ("kTpz", sl, 0)], writes=[(nm, sl, tt)])
                        S.act(lambda e, tt=tt, pp=pp, sl=sl: e.activation(out=kTp[sl][1][64:128, tt * 512:(tt + 1) * 512], in_=pp[64:128, :], func=AF.Copy),
                              reads=[ppk, ("kTpz", sl, 1)], writes=[(nm, sl, tt)])
                    else:
                        S.act(lambda e, dst=dst, tt=tt, pp=pp: e.activation(out=dst[:, tt * 512:(tt + 1) * 512], in_=pp[:], func=AF.Copy),
                              reads=[ppk], writes=[(nm, sl, tt)])
            for bq in range(4):
                pp, ppk = P[6 + bq % 2], ("ps", 6 + bq % 2)
                for q in range(4):
                    b = bq * 4 + q
                    for k in range(8):
                        S.pe(lambda e, k=k, b=b, q=q, pp=pp, sl=sl: e.matmul(pp[:, q * 128:(q + 1) * 128], lhsT=self.xT[:, k, b * 128:(b + 1) * 128],
                                                                        rhs=wq[sl][:, k, 256:384], start=(k == 0), stop=(k == 7)),
                             reads=[("wq", sl, 2), ("xT", b)], writes=[ppk])
                S.act(lambda e, bq=bq, pp=pp, sl=sl: e.activation(out=vv[sl][:, bq * 4:bq * 4 + 4, :],
                                                                 in_=pp[:].rearrange("p (a c) -> p a c", a=4), func=AF.Copy),
                      reads=[ppk], writes=[("vv", sl, bq)])
            its = []
            for qt in range(4):
                nkb = 4 * qt + 4
                if fox:
                    seq = [(hh, kb) for hh in (0, 1) for kb in range(nkb)]
                else:
                    seq = [(hh, kb) for kb in range(nkb - 1, -1, -1) for hh in (0, 1)]
                for idx, (hh, kb) in enumerate(seq):
                    first = (kb == 0) if fox else (kb == nkb - 1)
                    last = (kb == nkb - 1) if fox else (kb == 0)
                    its.append(dict(qt=qt, hh=hh, kb=kb, first=first, last=last, epi=(idx == len(seq) - 1)))

            def geom(it):
                t0 = it["qt"] * 512
                j0 = it["kb"] * 128
                off = max(0, j0 - t0)
                return t0, j0, off, j0 >= t0, 64 * it["hh"]

            if fox:
                PSB = (0, 1, 4)
                POB = ((2, 5), (3, 6))

                def f1(it, n, c=c, sl=sl):
                    t0, j0, off, diag, r0 = geom(it)
                    hh, kb, qt = it["hh"], it["kb"], it["qt"]
                    h = 2 * c + hh
                    ps, psk = P[PSB[n % 3]], ("ps", PSB[n % 3])
                    Et, Ek = E[n % 4], ("E", n % 4)
                    kTh = kTp[sl][hh]
                    S.pe(lambda e: e.matmul(ps[:, off:512], lhsT=kTh[:, j0:j0 + 128], rhs=qT[sl][:, t0 + off:t0 + 512],
                                            start=True, stop=False), reads=[("kT", sl, kb // 4), ("qT", sl, qt)], writes=[psk])
                    S.pe(lambda e: e.matmul(ps[:, off:512], lhsT=ones3p[:], rhs=c3[sl][:, hh * T + t0 + off:hh * T + t0 + 512],
                                            start=False, stop=(not diag)), reads=[("c3", sl), "ones3p"], writes=[psk])
                    if diag:
                        S.pe(lambda e: e.matmul(ps[:, off:off + 128], lhsT=self.identb[:], rhs=self.negm[:], start=False, stop=True),
                             reads=["identb", "negm"], writes=[psk])
                    S.act(lambda e: e.activation(out=Et[:, off:512], in_=ps[:, off:512], func=AF.Exp, bias=cnT[:, kb, h:h + 1], scale=0.125),
                          reads=[psk, "cnT"], writes=[Ek])

                def f2(it, n, c=c, sl=sl):
                    t0, j0, off, diag, r0 = geom(it)
                    kb, qt, hh = it["kb"], it["qt"], it["hh"]
                    Et, Ek = E[n % 4], ("E", n % 4)
                    po, pok = P[POB[hh][0]], ("ps", POB[hh][0])
                    pl, plk = P[POB[hh][1]], ("ps", POB[hh][1])
                    first, last = it["first"], it["last"]
                    S.pe(lambda e: e.matmul(po[:, off:512], lhsT=vv[sl][:, kb, :], rhs=Et[:, off:512], start=first, stop=last),
                         reads=[Ek, ("vv", sl, kb // 4)], writes=[pok])
                    S.pe(lambda e: e.matmul(pl[:, off:512], lhsT=self.onesb[:], rhs=Et[:, off:512], start=first, stop=last),
                         reads=[Ek, "onesb"], writes=[plk])
                    if last:
                        S.dve(lambda e: e.reciprocal(out=rl[r0:r0 + 64, :], in_=pl[r0:r0 + 64, :]), reads=[plk], writes=[("rl", hh)])
                        S.dve(lambda e: e.tensor_tensor(out=OT[r0:r0 + 64, c, t0:t0 + 512], in0=po[r0:r0 + 64, :], in1=rl[r0:r0 + 64, :], op=ALU.mult),
                              reads=[pok, ("rl", hh)], writes=[("OT", c, qt)])
                self.run_pipeline(its, [f1, f2], [3])
            else:
                def s1(it, n, c=c, sl=sl):
                    t0, j0, off, diag, r0 = geom(it)
                    kb, qt = it["kb"], it["qt"]
                    ps, psk = P[(0, 1, 5)[n % 3]], ("ps", (0, 1, 5)[n % 3])
                    zt, zk = zs[n % 4], ("zs", n % 4)
                    et, ek = ez[n % 2], ("ez", n % 2)
                    st_, stk = spb[n % 4], ("spb", n % 4)
                    S.pe(lambda e, kTh=kTp[sl][it["hh"]]: e.matmul(ps[:, off:512], lhsT=kTh[:, j0:j0 + 128], rhs=qT[sl][:, t0 + off:t0 + 512],
                                                                 start=True, stop=(not diag)), reads=[("kT", sl, kb // 4), ("qT", sl, qt)], writes=[psk])
                    if diag:
                        S.pe(lambda e: e.matmul(ps[:, off:off + 128], lhsT=self.identb[:], rhs=self.negs[:], start=False, stop=True),
                             reads=["identb", "negs"], writes=[psk])
                    S.dve(lambda e: e.tensor_scalar(out=zt[:, off:512], in0=ps[:, off:512], scalar1=0.125, scalar2=None, op0=ALU.mult),
                          reads=[psk], writes=[zk])
                    S.act(lambda e: e.activation(out=et[:, off:512], in_=zt[:, off:512], func=AF.Exp), reads=[zk], writes=[ek])
                    S.act(lambda e: e.activation(out=st_[:, off:512], in_=et[:, off:512], func=AF.Ln, bias=self.oneb[:, 0:1], scale=1.0),
                          reads=[ek, "oneb"], writes=[stk])

                def s2(it, n, c=c, sl=sl):
                    t0, j0, off, diag, r0 = geom(it)
                    hh = it["hh"]
                    zt, zk = zs[n % 4], ("zs", n % 4)
                    st_, stk = spb[n % 4], ("spb", n % 4)
                    pc, pck = P[4], ("ps", 4)
                    pr, prk = P[6 + hh], ("ps", 6 + hh)
                    lt, lk = lw[n % 2], ("lw", n % 2)
                    Et, Ek = E[n % 3], ("E", n % 3)
                    first, last = it["first"], it["last"]
                    if it["first"]:
                        S.pe(lambda e, hh=it["hh"]: e.matmul(P[6 + hh][:, 0:512], lhsT=zerob[:], rhs=self.onesb[:, 0:1].to_broadcast([128, 512]),
                                                            start=True, stop=False), reads=["zerob", "onesb"], writes=[("ps", 6 + it["hh"])])
                    S.pe(lambda e: e.matmul(pc[:, off:512], lhsT=self.trige[:], rhs=st_[:, off:512], start=True, stop=True),
                         reads=[stk, "trige"], writes=[pck])
                    S.dve(lambda e: e.tensor_tensor(out=lt[:, off:512], in0=zt[:, off:512], in1=pc[:, off:512], op=ALU.subtract),
                          reads=[zk, pck], writes=[lk])
                    if not first:
                        S.dve(lambda e: e.tensor_tensor(out=lt[:, off:512], in0=lt[:, off:512], in1=pr[:, off:512], op=ALU.subtract),
                              reads=[lk, prk], writes=[lk])
                    if not last:
                        S.pe(lambda e: e.matmul(pr[:, off:512], lhsT=self.onesb[:], rhs=st_[:, off:512], start=False, stop=(it["kb"] == 1)),
                             reads=[stk, "onesb"], writes=[prk])
                    S.act(lambda e: e.activation(out=Et[:, off:512], in_=lt[:, off:512], func=AF.Exp), reads=[lk], writes=[Ek])

                def s3(it, n, c=c, sl=sl):
                    t0, j0, off, diag, r0 = geom(it)
                    kb, qt = it["kb"], it["qt"]
                    Et, Ek = E[n % 3], ("E", n % 3)
                    po, pok = P[2 + it["hh"]], ("ps", 2 + it["hh"])
                    S.pe(lambda e: e.matmul(po[:, off:512], lhsT=vv[sl][:, kb, :], rhs=Et[:, off:512],
                                            start=it["first"], stop=it["last"], skip_group_check=True),
                         reads=[Ek, ("vv", sl, kb // 4)], writes=[pok])
                    if it["last"]:
                        S.act(lambda e: e.activation(out=OT[r0:r0 + 64, c, t0:t0 + 512], in_=po[r0:r0 + 64, :], func=AF.Copy),
                              reads=[pok], writes=[("OT", c, qt)])
                self.run_pipeline(its, [s1, s2, s3], [2, 2])
        self.mixer_out(sctx, layer, lambda k, b: OT[:, k, b * 128:(b + 1) * 128], lambda k, b: [("OT", k, b // 4)], 8, Wo, L)

    def colvec(self, ctx2, vec_ap, n, dst, key):
        S, P = self.S, self.P
        tmp = self.sb(ctx2, "cvt", [n, 128], F32)
        tk = ("cvt", key)
        S.dma("sp", lambda e: e.dma_start(out=tmp[:], in_=vec_ap.rearrange("(c p) -> c p", p=128)), writes=[tk])
        S.pe(lambda e: e.transpose(out=P[7][:, 0:n], in_=tmp[:], identity=self.identf[0:n, 0:n]), reads=[tk, "identf"], writes=[("ps", 7)])
        S.dve(lambda e: e.tensor_copy(out=dst, in_=P[7][:, 0:n]), reads=[("ps", 7)], writes=[key])

    def ssd_stage(self, sctx, layer):
        S, P, nc = self.S, self.P, self.nc
        pre = "l%d_ssd_" % layer
        Win = self.inp(pre + "w_in", [D, 5152])
        convw = self.inp(pre + "conv_w", [4, 3072])
        convb = self.inp(pre + "conv_b", [3072])
        dtbias = self.inp(pre + "dt_bias", [32])
        alog = self.inp(pre + "a_log", [32])
        dskip = self.inp(pre + "d_skip", [32])
        normw = self.inp(pre + "norm_w", [2048])
        Wo = self.inp(pre + "w_out", [2048, D])
        L = self.ln_setup(sctx, layer, "mix")
        self.epsb = self.sb(sctx, "epsb", [128, 1], F32)
        S.dve(lambda e: e.memset(self.epsb[:], LN_EPS), writes=["epsb"])
        if layer % 2 == 1:
            self.router_setup(sctx, layer)
        ysp = nc.dram_tensor("ysp%d" % layer, [16, 128, T], BF16, kind="Internal").ap()
        cw = self.sb(sctx, "cw", [128, 24, 4], F32)
        cb = self.sb(sctx, "cb", [128, 24], F32)
        nwp = self.sb(sctx, "nwp", [128, 16], F32)
        dskp = self.sb(sctx, "dskp", [128, 16], F32)
        nAT = self.sb(sctx, "nAT", [128, NB, 32], F32)
        dtk = self.sb(sctx, "dtk", [128, NB, 32], F32)
        with ExitStack() as c2:
            self.colvec(c2, convb, 24, cb[:], "cb")
            self.colvec(c2, normw, 16, nwp[:], "nwp")
            cwr = self.sb(c2, "cwr", [4, 3072], F32)
            S.dma("sp", lambda e: e.dma_start(out=cwr[:], in_=convw), writes=["cwr"])
            for fc in range(24):
                S.pe(lambda e, fc=fc: e.transpose(out=P[6][:, fc * 4:fc * 4 + 4], in_=cwr[:, fc * 128:(fc + 1) * 128],
                                                  identity=self.identf[0:4, 0:4]), reads=["cwr", "identf"], writes=[("ps", 6)])
            S.dve(lambda e: e.tensor_copy(out=cw[:].rearrange("p a b -> p (a b)"), in_=P[6][:, 0:96]), reads=[("ps", 6)], writes=["cw"])
            with nc.allow_non_contiguous_dma(reason="tiny"):
                pass
            d2 = dskip.rearrange("(c two) -> two c", two=2)
            S.dma("sp", lambda e: e.dma_start(out=dskp[0:64, :], in_=d2[0].partition_broadcast(64), allow_slow_non_contiguous=True), writes=["dskp0"])
            S.dma("sp", lambda e: e.dma_start(out=dskp[64:128, :], in_=d2[1].partition_broadcast(64), allow_slow_non_contiguous=True), writes=["dskp1"])
            wdt = self.sb(c2, "wdt", [128, 8, 32], BF16)
            self.wload(wdt[:], Win[:, 5120:5152].rearrange("(k p) c -> p k c", p=128), "wdt")
            dtb = self.sb(c2, "dtb", [32, 1], F32)
            al = self.sb(c2, "al", [32, 1], F32)
            S.dma("sp", lambda e: e.dma_start(out=dtb[:], in_=dtbias.rearrange("(p o) -> p o", o=1)), writes=["dtb"])
            S.dma("sp", lambda e: e.dma_start(out=al[:], in_=alog.rearrange("(p o) -> p o", o=1)), writes=["al"])
            S.act(lambda e: e.activation(out=al[:], in_=al[:], func=AF.Exp), reads=["al"], writes=["al"])
            S.dve(lambda e: e.tensor_scalar(out=al[:], in0=al[:], scalar1=-1.0, scalar2=None, op0=ALU.mult), reads=["al"], writes=["al"])
            dtT = self.sb(c2, "dtT", [32, T], F32)
            An = self.sb(c2, "An", [32, T], F32)
            tm = self.sb(c2, "tm", [32, T], F32)
            Ad = self.sb(c2, "Ad", [32, T], F32)
            c3b = self.sb(c2, "c3b", [32, 3, T], BF16)
            for tt in range(4):
                pf, pfk = P[tt % 2], ("ps", tt % 2)
                for k in range(8):
                    S.pe(lambda e, k=k, tt=tt, pf=pf: e.matmul(pf[0:32, :], lhsT=wdt[:, k, :], rhs=self.xT[:, k, tt * 512:(tt + 1) * 512],
                                                            start=(k == 0), stop=(k == 7)),
                         reads=["wdt"] + [("xT", tt * 4 + q) for q in range(4)], writes=[pfk])
                S.act(lambda e, tt=tt, pf=pf: e.activation(out=tm[:, tt * 512:(tt + 1) * 512], in_=pf[0:32, :], func=AF.Exp,
                                                        bias=dtb[:, 0:1], scale=1.0), reads=[pfk, "dtb"], writes=[("tm", tt)])
                S.act(lambda e, tt=tt: e.activation(out=dtT[:, tt * 512:(tt + 1) * 512], in_=tm[:, tt * 512:(tt + 1) * 512],
                                                    func=AF.Ln, bias=self.oneb[0:32, 0:1], scale=1.0),
                      reads=[("tm", tt), "oneb"], writes=[("dtT", tt)])
            alld = [("dtT", tt) for tt in range(4)]
            allt = [("tm", tt) for tt in range(4)]
            S.dve(lambda e: e.tensor_scalar(out=Ad[:], in0=dtT[:], scalar1=al[:, 0:1], scalar2=None, op0=ALU.mult),
                  reads=alld + ["al"], writes=["da"])
            S.dve(lambda e: e.memset(tm[:], 1.0), writes=allt + ["tm1"])
            S.dve(lambda e: e.tensor_tensor_scan(out=An[:], data0=tm[:], data1=Ad[:], initial=0.0, op0=ALU.mult, op1=ALU.add),
                  reads=["da", "tm1"], writes=["An"])
            for b in range(NB):
                pt, ptk = P[2 + b % 2], ("ps", 2 + b % 2)
                S.pe(lambda e, b=b, pt=pt: e.transpose(out=pt[:, 0:32], in_=An[:, b * 128:(b + 1) * 128], identity=self.identf[0:32, 0:32]),
                     reads=["An", "identf"], writes=[ptk])
                S.pe(lambda e, b=b, pt=pt: e.transpose(out=pt[:, 32:64], in_=dtT[:, b * 128:(b + 1) * 128], identity=self.identf[0:32, 0:32]),
                     reads=alld + ["identf"], writes=[ptk])
                S.dve(lambda e, b=b, pt=pt: e.tensor_scalar(out=nAT[:, b, :], in0=pt[:, 0:32], scalar1=-1.0, scalar2=None, op0=ALU.mult),
                      reads=[ptk], writes=["nAT"])
                S.dve(lambda e, b=b, pt=pt: e.tensor_copy(out=dtk[:, b, :], in_=pt[:, 32:64]), reads=[ptk], writes=["dtk"])
            S.dve(lambda e: e.tensor_copy(out=tm[:], in_=An[:]), reads=["An", "tm1"], writes=["m8"])
            for i in range(3):
                S.dve(lambda e, i=i: e.tensor_copy(out=c3b[:, i, :], in_=tm[:]), reads=["m8"], writes=[("c3b", i)])
                if i < 2:
                    S.dve(lambda e, i=i: e.tensor_tensor(out=tm[:], in0=tm[:], in1=c3b[:, i, :], op=ALU.subtract),
                          reads=["m8", ("c3b", i)], writes=["m8"])
            for a in range(2):
                S.dma("sp", lambda e, a=a: e.dma_start(out=self.scr[1 + a, :, :].rearrange("i (h t) -> h i t", h=16), in_=c3b[a * 16:(a + 1) * 16, :, :]),
                      reads=[("c3b", i) for i in range(3)], writes=[("scrA", a)])
            S.flush(barrier=True)
        with ExitStack() as c3x:
            xsT = self.sb(c3x, "xsT", [128, 4, T], BF16)
            zT = self.sb(c3x, "zT", [128, 4, T], BF16)
            Vg = self.sb(c3x, "Vg", [128, NB, 512], BF16)
            BT = self.sb(c3x, "BT", [128, T], BF16)
            CT = self.sb(c3x, "CT", [128, T], BF16)
            raw = [self.sb(c3x, "raw", [128, 3 + T], BF16) for _ in range(2)]
            Dg = self.sb(c3x, "Dg", [128, 6, 4, 128], BF16)
            wch = [self.sb(c3x, "wch", [128, 8, 128], BF16) for _ in range(2)]
            tmpf = [self.sb(c3x, "tmpf", [128, 512], F32) for _ in range(2)]
            c3q = [self.sb(c3x, "c3q", [128, 8, 512], BF16) for _ in range(2)]
            ones3p = self.sb(c3x, "ones3p", [128, 128], BF16)
            S.dve(lambda e: e.memset(ones3p[:], 0.0), writes=["ones3p"])
            S.dve(lambda e: e.memset(ones3p[0:3, :], 1.0), reads=["ones3p"], writes=["ones3p"])
            for i_ in range(2):
                S.dve(lambda e, i_=i_: e.memset(c3q[i_][:], 0.0), writes=[("c3q", i_)])
            Ab8 = self.sb(c3x, "Ab8", [128, 8, 512], F32)
            CBs = [self.sb(c3x, "CBs", [128, 512], BF16) for _ in range(3)]
            Ef = [self.sb(c3x, "Ef", [128, 512], BF16) for _ in range(3)]
            Mt = [self.sb(c3x, "Mt", [128, 512], BF16) for _ in range(3)]
            yz = self.sb(c3x, "yz", [128, 4, 512], F32)
            sq = [self.sb(c3x, "sq", [128, 512], BF16) for _ in range(2)]
            rs = self.sb(c3x, "rs", [128, 512], F32)
            ynt = [self.sb(c3x, "ynt", [128, 512], BF16) for _ in range(2)]
            for i in range(2):
                S.dve(lambda e, i=i: e.memset(raw[i][:, 0:3], 0.0), writes=[("rawpad", i)])
            wi = 0
            ri = 0
            ti = 0
            for g in range(4):
                a, hloc = (8 * g) // 16, (8 * g) % 16
                chunks = [("xs", i, 2048 + g * 512 + i * 128, g * 4 + i) for i in range(4)]
                chunks += [("B", 0, 4096 + g * 128, 16 + g), ("C", 0, 4608 + g * 128, 20 + g)]
                chunks += [("z", i, g * 512 + i * 128, None) for i in range(4)]
                ci = 0
                for kind, idx, col0, fc in chunks:
                    ws = wi % 2
                    wi += 1
                    self.wload(wch[ws][:], Win[:, col0:col0 + 128].rearrange("(k p) c -> p k c", p=128), ("wch", ws))
                    if fc is not None:
                        for k in range(4):
                            S.dve(lambda e, ci=ci, k=k, fc=fc: e.tensor_scalar(out=Dg[:, ci, k, :], in0=self.identb[:], scalar1=cw[:, fc, k:k + 1],
                                                                              scalar2=None, op0=ALU.mult),
                                  reads=["identb", "cw"], writes=[("Dg", ci)])
                        rw = ri % 2
                        ri += 1
                    for tt in range(4):
                        pp, ppk = P[tt % 2], ("ps", tt % 2)
                        for k in range(8):
                            S.pe(lambda e, k=k, tt=tt, pp=pp, ws=ws: e.matmul(pp[:], lhsT=wch[ws][:, k, :], rhs=self.xT[:, k, tt * 512:(tt + 1) * 512],
                                                                         start=(k == 0), stop=(k == 7)),
                                 reads=[("wch", ws)] + [("xT", tt * 4 + q) for q in range(4)], writes=[ppk])
                        if fc is None:
                            S.act(lambda e, tt=tt, pp=pp, idx=idx: e.activation(out=zT[:, idx, tt * 512:(tt + 1) * 512], in_=pp[:], func=AF.Silu),
                                  reads=[ppk], writes=[("zT", idx, tt)])
                        else:
                            S.act(lambda e, tt=tt, pp=pp, rw=rw: e.activation(out=raw[rw][:, 3 + tt * 512:3 + (tt + 1) * 512], in_=pp[:], func=AF.Copy),
                                  reads=[ppk, ("rawpad", rw)], writes=[("raw", rw, tt)])
                    if fc is not None:
                        for tt in range(4):
                            pc, pck = P[2 + tt % 2], ("ps", 2 + tt % 2)
                            rr = [("raw", rw, tt), ("rawpad", rw)] + ([("raw", rw, tt - 1)] if tt > 0 else [])
                            for k in range(4):
                                S.pe(lambda e, k=k, tt=tt, pc=pc, ci=ci, rw=rw: e.matmul(pc[:], lhsT=Dg[:, ci, k, :],
                                                                                   rhs=raw[rw][:, tt * 512 + k:tt * 512 + k + 512],
                                                                                   start=(k == 0), stop=(k == 3)),
                                     reads=rr + [("Dg", ci)], writes=[pck])
                            if kind == "xs":
                                tf, tfk = tmpf[ti % 2], ("tmpf", ti % 2)
                                ti += 1
                                S.act(lambda e, pc=pc, tf=tf, fc=fc: e.activation(out=tf[:], in_=pc[:], func=AF.Silu, bias=cb[:, fc:fc + 1], scale=1.0),
                                      reads=[pck, "cb"], writes=[tfk])
                                S.dve(lambda e, tf=tf, idx=idx, tt=tt: e.tensor_copy(out=xsT[:, idx, tt * 512:(tt + 1) * 512], in_=tf[:]),
                                      reads=[tfk], writes=[("xsT", idx, tt)])
                                pt, ptk = P[4 + tt % 2], ("ps", 4 + tt % 2)
                                for q in range(4):
                                    S.pe(lambda e, q=q, pt=pt, tf=tf: e.transpose(out=pt[:, q * 128:(q + 1) * 128], in_=tf[:, q * 128:(q + 1) * 128],
                                                                                identity=self.identf[:]), reads=[tfk, "identf"], writes=[ptk])
                                h0 = 8 * g + 2 * idx
                                S.dve(lambda e, pt=pt, tt=tt, idx=idx, h0=h0: e.tensor_tensor(
                                    out=Vg[:, tt * 4:tt * 4 + 4, idx * 128:(idx + 1) * 128].rearrange("p b (h d) -> p b h d", h=2),
                                    in0=pt[:].rearrange("p (b h d) -> p b h d", b=4, h=2),
                                    in1=dtk[:, tt * 4:tt * 4 + 4, h0:h0 + 2].unsqueeze(3).to_broadcast([128, 4, 2, 64]), op=ALU.mult),
                                    reads=[ptk, "dtk"], writes=[("Vg", tt)])
                            else:
                                dstT = BT if kind == "B" else CT
                                S.act(lambda e, pc=pc, dstT=dstT, tt=tt, fc=fc: e.activation(out=dstT[:, tt * 512:(tt + 1) * 512], in_=pc[:], func=AF.Silu,
                                                                                          bias=cb[:, fc:fc + 1], scale=1.0),
                                      reads=[pck, "cb"], writes=[(kind + "T", tt)])
                        ci += 1
                its = []
                for qt in range(4):
                    nkb = 4 * qt + 4
                    for half in range(2):
                        for kb in range(nkb):
                            for hq in range(4):
                                its.append(dict(qt=qt, kb=kb, hl=4 * half + hq, hq=hq, half=half, first=(kb == 0), last=(kb == nkb - 1),
                                                cbn=len(its) // 4, epi=(kb == nkb - 1 and hq == 3)))
                PSB = (0, 1, 7)

                def epi_half(qt, half, g=g):
                    t0 = qt * 512
                    for hq in range(4):
                        i = 2 * half + hq // 2
                        r0 = 64 * (hq % 2)
                        fcg = g * 4 + i
                        po, pok = P[3 + hq], ("ps", 3 + hq)
                        S.dve(lambda e, i=i, po=po, fcg=fcg, t0=t0, r0=r0: e.scalar_tensor_tensor(
                            out=yz[r0:r0 + 64, i, :], in0=xsT[r0:r0 + 64, i, t0:t0 + 512], scalar=dskp[r0:r0 + 64, fcg:fcg + 1], in1=po[r0:r0 + 64, :],
                            op0=ALU.mult, op1=ALU.add), reads=[("xsT", i, qt), "dskp0", "dskp1", pok], writes=[("yz", i)])
                        S.dve(lambda e, i=i, t0=t0, r0=r0: e.tensor_tensor(out=yz[r0:r0 + 64, i, :], in0=yz[r0:r0 + 64, i, :],
                                                                        in1=zT[r0:r0 + 64, i, t0:t0 + 512], op=ALU.mult),
                              reads=[("yz", i), ("zT", i, qt)], writes=[("yz", i)])

                def epilogue(qt, g=g):
                    t0 = qt * 512
                    pss, pssk = P[7], ("ps", 7)
                    for i in range(4):
                        fcg = g * 4 + i
                        sqt, sqk = sq[i % 2], ("sq", i % 2)
                        S.act(lambda e, i=i, sqt=sqt: e.activation(out=sqt[:], in_=yz[:, i, :], func=AF.Square), reads=[("yz", i)], writes=[sqk])
                        S.pe(lambda e, i=i, sqt=sqt: e.matmul(pss[:], lhsT=self.onesb[:], rhs=sqt[:], start=(i == 0), stop=(i == 3)),
                             reads=[sqk, "onesb"], writes=[pssk])
                    S.act(lambda e: e.activation(out=rs[:], in_=pss[:], func=AF.Sqrt, bias=self.epsb[:, 0:1], scale=1.0 / 512.0),
                          reads=[pssk, "epsb"], writes=["rs"])
                    S.dve(lambda e: e.reciprocal(out=rs[:], in_=rs[:]), reads=["rs"], writes=["rs"])
                    for i in range(4):
                        fcg = g * 4 + i
                        yt, ytk = ynt[i % 2], ("ynt", i % 2)
                        S.dve(lambda e, i=i, yt=yt, fcg=fcg: e.scalar_tensor_tensor(out=yt[:], in0=yz[:, i, :], scalar=nwp[:, fcg:fcg + 1], in1=rs[:],
                                                                                  op0=ALU.mult, op1=ALU.mult),
                              reads=[("yz", i), "nwp", "rs"], writes=[ytk])
                        S.dma("sp", lambda e, yt=yt, fcg=fcg, t0=t0: e.dma_start(out=ysp[fcg, :, t0:t0 + 512], in_=yt[:]),
                              reads=[ytk], writes=[("ysp", fcg, qt)], semkey=("yspst", i % 2))

                def d1(it, n, g=g):
                    qt, kb, hl = it["qt"], it["kb"], it["hl"]
                    t0, j0 = qt * 512, kb * 128
                    off = max(0, j0 - t0)
                    diag = j0 >= t0
                    h = 8 * g + hl
                    cbt, cbk = CBs[it["cbn"] % 3], ("CBs", it["cbn"] % 3)
                    if it["hq"] == 0:
                        pcb, pcbk = P[2], ("ps", 2)
                        S.pe(lambda e: e.matmul(pcb[:, off:512], lhsT=BT[:, j0:j0 + 128], rhs=CT[:, t0 + off:t0 + 512], start=True, stop=True),
                             reads=[("BT", kb // 4), ("CT", qt)], writes=[pcbk])
                        S.dve(lambda e: e.tensor_copy(out=cbt[:, off:512], in_=pcb[:, off:512]), reads=[pcbk], writes=[cbk])
                    ps, psk = P[PSB[n % 3]], ("ps", PSB[n % 3])
                    et, ek = Ef[n % 3], ("Ef", n % 3)
                    mt, mk = Mt[n % 3], ("Mt", n % 3)
                    c3t, c3k = c3q[qt % 2], ("c3q", qt % 2)
                    if kb == 0 and hl == 0:
                        a_, hloc_ = (8 * g) // 16, (8 * g) % 16
                        S.dma("sp", lambda e, c3t=c3t, a_=a_, hloc_=hloc_: e.dma_start(
                            out=c3t[0:3, :, :], in_=self.scr[1 + a_, :, :].rearrange("i (h t) -> i h t", h=16)[:, hloc_:hloc_ + 8, t0:t0 + 512]),
                            reads=[("scrA", a_)], writes=[c3k])
                    if qt > 0 and kb == 0 and hl == 0:
                        for h2 in range(8):
                            pa, pak = P[PSB[h2 % 3]], ("ps", PSB[h2 % 3])
                            S.pe(lambda e, h2=h2, pa=pa: e.matmul(pa[:, 0:512], lhsT=ones3p[:], rhs=c3t[:, h2, :],
                                                                 start=True, stop=True), reads=[c3k, "onesb"], writes=[pak])
                            if False:
                                pass
                            else:
                                S.dve(lambda e, h2=h2, pa=pa: e.tensor_copy(out=Ab8[:, h2, :], in_=pa[:, 0:512]), reads=[pak], writes=[("Ab8", h2)])
                    if diag:
                        S.pe(lambda e: e.matmul(ps[:, off:512], lhsT=ones3p[:], rhs=c3t[:, hl, off:512],
                                                start=True, stop=False), reads=[c3k, "ones3p"], writes=[psk])
                        S.pe(lambda e: e.matmul(ps[:, off:off + 128], lhsT=self.identb[:], rhs=self.negm[:], start=False, stop=True),
                             reads=["identb", "negm"], writes=[psk])
                        S.act(lambda e: e.activation(out=et[:, off:512], in_=ps[:, off:512], func=AF.Exp, bias=nAT[:, kb, h:h + 1], scale=1.0),
                              reads=[psk, "nAT"], writes=[ek])
                    else:
                        S.act(lambda e: e.activation(out=et[:, 0:512], in_=Ab8[:, hl, :], func=AF.Exp, bias=nAT[:, kb, h:h + 1], scale=1.0),
                              reads=[("Ab8", hl), "nAT"], writes=[ek])
                    S.dve(lambda e: e.tensor_tensor(out=mt[:, off:512], in0=et[:, off:512], in1=cbt[:, off:512], op=ALU.mult),
                          reads=[ek, cbk], writes=[mk])

                def d2(it, n, g=g):
                    qt, kb, hl = it["qt"], it["kb"], it["hl"]
                    t0, j0 = qt * 512, kb * 128
                    off = max(0, j0 - t0)
                    hq, half = it["hq"], it["half"]
                    po, pok = P[3 + hq], ("ps", 3 + hq)
                    mt, mk = Mt[n % 3], ("Mt", n % 3)
                    pc0 = (hl // 2) * 128
                    S.pe(lambda e: e.matmul(po[:, off:512], lhsT=Vg[:, kb, pc0:pc0 + 128], rhs=mt[:, off:512],
                                            start=it["first"], stop=it["last"]), reads=[mk, ("Vg", kb // 4)], writes=[pok])
                    if it["epi"]:
                        epi_half(qt, half)
                        if half == 1:
                            epilogue(qt)
                self.run_pipeline(its, [d1, d2], [2])
            S.flush(barrier=True)
        ynb = [self.sb(sctx, "ynb", [128, 16, 128], BF16) for _ in range(2)]

        def pre_block(b):
            S.dma("sp", lambda e: e.dma_start(out=ynb[b % 2][:], in_=ysp[:, :, b * 128:(b + 1) * 128].rearrange("c p t -> p c t")),
                  writes=[("ynb", b % 2)])
        self.mixer_out(sctx, layer, lambda k, b: ynb[b % 2][:, k, :], lambda k, b: [("ynb", b % 2)], 16, Wo, L, pre_block=pre_block)


_CACHE = {}


def run(inputs, stages):
    key = tuple(stages)
    if key not in _CACHE:
        bld = Builder(stages)
        nc = bld.build()
        _CACHE[key] = (bld, nc)
    bld, nc = _CACHE[key]
    x = np.asarray(inputs["x"], dtype=np.float32)
    in_maps = []
    shared = {n: np.ascontiguousarray(np.asarray(inputs[n], dtype=np.float32)) for n in bld.used_inputs if n != "x"}
    for c in range(8):
        m = dict(shared)
        m["x"] = np.ascontiguousarray(x[c])
        in_maps.append(m)
    res = run_bass_kernel_spmd(nc, in_maps, core_ids=list(range(8)))
    return np.stack([np.asarray(r["y"]) for r in res.results], axis=0).astype(np.float32)


ALL_INPUT_NAMES = (
    "x",
    "l0_ssd_w_in", "l0_ssd_conv_w", "l0_ssd_conv_b", "l0_ssd_dt_bias", "l0_ssd_a_log", "l0_ssd_d_skip", "l0_ssd_norm_w", "l0_ssd_w_out",
    "l0_ln_mix_g", "l0_ln_mix_b", "l0_ffn_w_gate", "l0_ffn_w_up", "l0_ffn_w_down", "l0_ln_ffn_g", "l0_ln_ffn_b",
    "l1_sb_w_qkv", "l1_sb_w_out", "l1_ln_mix_g", "l1_ln_mix_b",
    "l1_moe_w_router", "l1_moe_w_gate", "l1_moe_w_up", "l1_moe_w_down", "l1_ln_ffn_g", "l1_ln_ffn_b",
    "l2_fox_w_qkvf", "l2_fox_b_f", "l2_fox_w_out", "l2_ln_mix_g", "l2_ln_mix_b",
    "l2_ffn_w_gate", "l2_ffn_w_up", "l2_ffn_w_down", "l2_ln_ffn_g", "l2_ln_ffn_b",
    "l3_ssd_w_in", "l3_ssd_conv_w", "l3_ssd_conv_b", "l3_ssd_dt_bias", "l3_ssd_a_log", "l3_ssd_d_skip", "l3_ssd_norm_w", "l3_ssd_w_out",
    "l3_ln_mix_g", "l3_ln_mix_b",
    "l3_moe_w_router", "l3_moe_w_gate", "l3_moe_w_up", "l3_moe_w_down", "l3_ln_ffn_g", "l3_ln_ffn_b",
)


def kernel(**inputs):
    missing = [n for n in ALL_INPUT_NAMES if n not in inputs]
    assert not missing, missing
    return run(inputs, list(range(8)))
```

```python
import numpy as np
from contextlib import ExitStack
import concourse.bass as bass
import concourse.mybir as mybir
from concourse.bass_utils import run_bass_kernel_spmd

F32 = mybir.dt.float32
BF16 = mybir.dt.bfloat16
AF = mybir.ActivationFunctionType
ALU = mybir.AluOpType

T = 2048
D = 1024
NB = 16
ALPHA = (2.0 * 4) ** 0.25
LN_EPS = 1e-5
NEG = -30000.0


class Op:
    __slots__ = ("eng", "fn", "reads", "writes", "dma", "signal", "count", "deps", "semkey", "kind", "kw")

    def __init__(self, eng, fn, reads, writes, dma, semkey):
        self.eng, self.fn, self.reads, self.writes = eng, fn, reads, writes
        self.dma, self.semkey = dma, semkey
        self.signal = False
        self.count = 0
        self.deps = []


class Sched:
    def __init__(self, nc, ctx):
        self.nc = nc
        self.ctx = ctx
        self.ops = []
        self.engobj = {"pe": nc.tensor, "act": nc.scalar, "dve": nc.vector,
                       "pool": nc.gpsimd, "sp": nc.sync}
        self.sems = {}
        self.cnt = {}
        self.last_w = {}
        self.readers = {}
        self.seen = {e: {} for e in self.engobj}
        self.last_op = {}
        self.pending_barrier = None
        self.dma_last = {}
        self.nops = 0
        self.if_state = None
        self.cregs = None

    def add(self, eng, fn, reads=(), writes=(), dma=False, semkey=None):
        op = Op(eng, fn, tuple(reads), tuple(writes), dma, semkey)
        self.ops.append(op)
        return op

    def pe(self, fn, reads=(), writes=()):
        return self.add("pe", fn, reads, writes)

    def act(self, fn, reads=(), writes=()):
        return self.add("act", fn, reads, writes)

    def dve(self, fn, reads=(), writes=()):
        return self.add("dve", fn, reads, writes)

    def pool(self, fn, reads=(), writes=()):
        return self.add("pool", fn, reads, writes)

    def dma(self, q, fn, reads=(), writes=(), semkey=None):
        return self.add(q, fn, reads, writes, dma=True, semkey=(writes[0] if semkey is None else semkey))

    def ctl(self, kind, **kw):
        op = Op("ctl", None, (), (), False, None)
        op.kind, op.kw = kind, kw
        self.ops.append(op)
        return op

    def _sem(self, key):
        s = self.sems.get(key)
        if s is None:
            s = self.ctx.enter_context(self.nc.semaphore("s%d" % len(self.sems)))
            self.sems[key] = s
        return s

    def flush(self, barrier=True):
        ops = self.ops
        self.ops = []
        last_w, readers = self.last_w, self.readers
        first_after = {}
        bar = self.pending_barrier
        for op in ops:
            if op.eng == "ctl":
                continue
            deps = set()
            for k in op.reads:
                w = last_w.get(k)
                if w is not None:
                    deps.add(w)
            for k in op.writes:
                w = last_w.get(k)
                if w is not None:
                    deps.add(w)
                for r in readers.get(k, ()):
                    deps.add(r)
            deps.discard(op)
            fin = []
            for d in deps:
                if d.dma:
                    fin.append(d)
                    continue
                if d.eng == op.eng and not op.dma:
                    if not any(k in d.writes for k in op.reads):
                        continue
                fin.append(d)
            if bar is not None and op.eng not in first_after:
                first_after[op.eng] = True
                fin.extend(bar)
            op.deps = fin
            for d in fin:
                d.signal = True
            for k in op.writes:
                last_w[k] = op
                readers[k] = []
            for k in op.reads:
                readers.setdefault(k, []).append(op)
            self.last_op[op.eng] = op
            if op.dma:
                self.dma_last[op.semkey] = op
        if bar is not None:
            pass
        if barrier:
            blist = [o for o in self.last_op.values() if not o.dma]
            blist += list(self.dma_last.values())
            if bar is not None and len(first_after) < len(self.engobj):
                blist += [o for o in bar if o not in blist]
            for o in blist:
                o.signal = True
            self.pending_barrier = blist
            self.last_w = {}
            self.readers = {}
            self.dma_last = {}
        else:
            self.pending_barrier = None
        cnt = self.cnt
        for op in ops:
            if op.eng == "ctl":
                continue
            if op.dma:
                key = ("dma", op.semkey)
                cnt[key] = cnt.get(key, 0) + 16
                op.count = cnt[key]
            elif op.signal:
                key = ("eng", op.eng)
                cnt[key] = cnt.get(key, 0) + 1
                op.count = cnt[key]
        for idx, op in enumerate(ops):
            if op.eng == "ctl":
                self._emit_ctl(op, ops, idx)
                continue
            eng = self.engobj[op.eng]
            need = {}
            for d in op.deps:
                key = ("dma", d.semkey) if d.dma else ("eng", d.eng)
                if d.count > need.get(key, 0):
                    need[key] = d.count
            sv = self.seen[op.eng]
            for key, c in need.items():
                if sv.get(key, 0) >= c:
                    continue
                eng.wait_ge(self._sem(key), c)
                sv[key] = c
            inst = op.fn(eng)
            if op.dma:
                inst.then_inc(self._sem(("dma", op.semkey)), 16)
            elif op.signal:
                inst.then_inc(self._sem(("eng", op.eng)), 1)
        self.nops += len(ops)

    def _emit_ctl(self, op, ops, idx):
        nc = self.nc
        if op.kind == "if":
            inc = {}
            base = {}
            j = idx + 1
            while not (ops[j].eng == "ctl" and ops[j].kind == "endif"):
                o = ops[j]
                if o.eng != "ctl":
                    if o.dma:
                        k2 = (("dma", o.semkey), o.eng)
                        inc[k2] = inc.get(k2, 0) + 16
                        if k2 not in base:
                            base[k2] = o.count - 16
                    elif o.signal:
                        k2 = (("eng", o.eng), o.eng)
                        inc[k2] = inc.get(k2, 0) + 1
                j += 1
            if self.cregs is None:
                self.cregs = nc.alloc_registers("cnd")
            nc.regs_load(self.cregs, op.kw["ap"])
            cm = nc.If_lt(self.cregs, op.kw["thresh"] + 1)
            cm.__enter__()
            for (key, engname), amt in inc.items():
                if (key, engname) in base and base[(key, engname)] > 0:
                    self.engobj[engname].wait_ge(self._sem(key), base[(key, engname)])
                self.engobj[engname].drain().then_inc(self._sem(key), amt)
            cm.__exit__(None, None, None)
            cm2 = nc.Else()
            cm2.__enter__()
            self.if_state = dict(cm=cm2, seen={e: dict(v) for e, v in self.seen.items()})
        elif op.kind == "endif":
            st = self.if_state
            st["cm"].__exit__(None, None, None)
            self.seen = st["seen"]
            self.if_state = None

    def final_wait(self, eng_name="sp"):
        eng = self.engobj[eng_name]
        for key, s in self.sems.items():
            eng.wait_ge(s, self.cnt[key])


class Builder:
    def __init__(self, stages):
        self.stages = stages
        self.nc = bass.Bass("TRN2", target_bir_lowering=False)
        self.din = {}
        self.used_inputs = []

    def inp(self, name, shape):
        if name not in self.din:
            self.din[name] = self.nc.dram_tensor(name, list(shape), F32, kind="ExternalInput").ap()
            self.used_inputs.append(name)
        return self.din[name]

    def sb(self, ctx, name, shape, dt):
        self.uid += 1
        return ctx.enter_context(self.nc.sbuf_tensor("%s_%d" % (name, self.uid), list(shape), dt))

    def build(self):
        nc = self.nc
        self.uid = 0
        self.x_in = self.inp("x", [T, D])
        self.y = nc.dram_tensor("y", [T, D], F32, kind="ExternalOutput").ap()
        self.scr = nc.dram_tensor("scr", [4, 3, 16 * T], BF16, kind="Internal").ap()
        with ExitStack() as ctx:
            self.ctx = ctx
            S = self.S = Sched(nc, ctx)
            self.P = [ctx.enter_context(nc.psum_tensor("ps%d" % i, [128, 512], F32)) for i in range(8)]
            self.xT = self.sb(ctx, "xT", [128, 8, T], BF16)
            self.gates = self.sb(ctx, "gates", [128, NB, 8], F32)
            self.identf = self.sb(ctx, "identf", [128, 128], F32)
            self.identb = self.sb(ctx, "identb", [128, 128], BF16)
            self.onesb = self.sb(ctx, "onesb", [128, 128], BF16)
            self.negm = self.sb(ctx, "negm", [128, 128], BF16)
            self.negs = self.sb(ctx, "negs", [128, 128], BF16)
            self.trige = self.sb(ctx, "trige", [128, 128], BF16)
            self.mstrict = self.sb(ctx, "mstrict", [128, 128], BF16)
            self.oneb = self.sb(ctx, "oneb", [128, 1], F32)
            self.consts()
            self.prep()
            S.flush(barrier=True)
            for st in self.stages:
                with ExitStack() as sctx:
                    layer, kind = st // 2, st % 2
                    if kind == 0:
                        if layer in (0, 3):
                            self.ssd_stage(sctx, layer)
                        elif layer == 1:
                            self.attn_stage(sctx, layer, "sb")
                        else:
                            self.attn_stage(sctx, layer, "fox")
                    else:
                        if layer % 2 == 1:
                            self.moe_stage(sctx, layer)
                        else:
                            self.ffn_stage(sctx, layer, moe=False)
                    S.flush(barrier=True)
            S.flush(barrier=True)
            S.final_wait("sp")
        return nc

    def consts(self):
        nc, S = self.nc, self.S
        S.pool(lambda e: e.memset(self.identf[:], 1.0), writes=["identf"])
        S.pool(lambda e: e.affine_select(out=self.identf[:], in_=self.identf[:], pattern=[[-1, 128]],
                                         compare_op=ALU.is_equal, fill=0.0, base=0, channel_multiplier=1),
               reads=["identf"], writes=["identf"])
        S.dve(lambda e: e.tensor_copy(out=self.identb[:], in_=self.identf[:]), reads=["identf"], writes=["identb"])
        S.pool(lambda e: e.memset(self.onesb[:], 1.0), writes=["onesb"])
        S.pool(lambda e: e.memset(self.oneb[:], 1.0), writes=["oneb"])
        S.pool(lambda e: e.memset(self.negm[:], 0.0), writes=["negm"])
        S.pool(lambda e: e.affine_select(out=self.negm[:], in_=self.negm[:], pattern=[[1, 128]],
                                         compare_op=ALU.is_ge, fill=NEG, base=0, channel_multiplier=-1),
               reads=["negm"], writes=["negm"])
        S.pool(lambda e: e.memset(self.negs[:], 0.0), writes=["negs"])
        S.pool(lambda e: e.affine_select(out=self.negs[:], in_=self.negs[:], pattern=[[1, 128]],
                                         compare_op=ALU.is_gt, fill=NEG, base=0, channel_multiplier=-1),
               reads=["negs"], writes=["negs"])
        S.pool(lambda e: e.memset(self.trige[:], 1.0), writes=["trige"])
        S.pool(lambda e: e.affine_select(out=self.trige[:], in_=self.trige[:], pattern=[[-1, 128]],
                                         compare_op=ALU.is_ge, fill=0.0, base=0, channel_multiplier=1),
               reads=["trige"], writes=["trige"])
        S.pool(lambda e: e.memset(self.mstrict[:], 1.0), writes=["mstrict"])
        S.pool(lambda e: e.affine_select(out=self.mstrict[:], in_=self.mstrict[:], pattern=[[1, 128]],
                                         compare_op=ALU.is_gt, fill=0.0, base=0, channel_multiplier=-1),
               reads=["mstrict"], writes=["mstrict"])

    def prep(self):
        S = self.S
        with ExitStack() as c:
            xb = [self.sb(c, "px", [128, D], F32) for _ in range(2)]
            for b in range(NB):
                t = xb[b % 2]
                rk = ("px", b % 2)
                S.dma("sp", lambda e, t=t, b=b: e.dma_start(out=t[:], in_=self.x_in[b * 128:(b + 1) * 128, :]),
                      writes=[rk])
                S.dma("sp", lambda e, t=t, b=b: e.dma_start(out=self.y[b * 128:(b + 1) * 128, :], in_=t[:]),
                      reads=[rk], writes=[("y", b)], semkey=("yst", b % 2))
                self.make_xT(t, rk, b)
            S.flush(barrier=True)

    def make_xT(self, src, rk, b, xtf=None):
        S, P = self.S, self.P
        for half in range(2):
            pb = P[6 + half]
            pk = ("ps", 6 + half)
            for kk in range(4):
                k = half * 4 + kk
                S.pe(lambda e, pb=pb, kk=kk, k=k: e.transpose(out=pb[:, kk * 128:(kk + 1) * 128],
                                                              in_=src[:, k * 128:(k + 1) * 128],
                                                              identity=self.identf[:]),
                     reads=[rk, "identf"], writes=[pk])
            dst = self.xT[:, half * 4:half * 4 + 4, b * 128:(b + 1) * 128]
            srcp = pb[:].rearrange("p (a c) -> p a c", a=4)
            if xtf is not None:
                S.dve(lambda e, half=half, srcp=srcp: e.tensor_copy(out=xtf[:, half * 4:half * 4 + 4, :], in_=srcp),
                      reads=[pk], writes=[("xtf", half)])
            S.act(lambda e, dst=dst, srcp=srcp: e.activation(out=dst, in_=srcp, func=AF.Copy),
                  reads=[pk] + ([("xtf", half)] if xtf is not None else []), writes=[("xT", b)])

    def ln_setup(self, sctx, layer, which):
        S = self.S
        g = self.inp("l%d_ln_%s_g" % (layer, which), [D])
        bta = self.inp("l%d_ln_%s_b" % (layer, which), [D])
        L = {}
        L["g"] = self.sb(sctx, "lng", [128, D], F32)
        L["b"] = self.sb(sctx, "lnb", [128, D], F32)
        S.dma("sp", lambda e: e.dma_start(out=L["g"][:], in_=g.partition_broadcast(128)), writes=["lng"])
        S.dma("sp", lambda e: e.dma_start(out=L["b"][:], in_=bta.partition_broadcast(128)), writes=["lnb"])
        L["xr"] = [self.sb(sctx, "lnxr", [128, D], F32) for _ in range(2)]
        L["s"] = [self.sb(sctx, "lns", [128, D], F32) for _ in range(2)]
        L["st"] = self.sb(sctx, "lnst", [128, 2, 2, 6], F32)
        L["mv"] = self.sb(sctx, "lnmv", [128, 2, 2], F32)
        L["sm"] = self.sb(sctx, "lnsm", [128, 2, 4], F32)
        L["n"] = 0
        return L

    def ln_load_x(self, L, b, i=None):
        S = self.S
        if i is None:
            i = L["n"] % 2
        t = L["xr"][i]
        S.dma("sp", lambda e: e.dma_start(out=t[:], in_=self.y[b * 128:(b + 1) * 128, :]),
              reads=[("y", b)], writes=[("lnxr", i)])
        return t, ("lnxr", i)

    def ln_finish(self, L, b, s, sk, router=None, i=None, part="all"):
        S = self.S
        if i is None:
            i = L["n"] % 2
            L["n"] += 1
        st, mv, sm = L["st"], L["mv"], L["sm"]
        if part in ("all", "stats"):
            self._ln_stats(L, s, sk, i)
        if part in ("all", "apply"):
            self._ln_apply(L, b, s, sk, i, router)

    def _ln_stats(self, L, s, sk, i):
        S = self.S
        st, mv, sm = L["st"], L["mv"], L["sm"]
        for h in range(2):
            S.dve(lambda e, h=h: e.bn_stats(out=st[:, i, h, :], in_=s[:, h * 512:(h + 1) * 512]),
                  reads=[sk], writes=[("lnst", i)])
        S.dve(lambda e: e.bn_aggr(out=mv[:, i, :], in_=st[:, i, :, :].rearrange("p a b -> p (a b)")),
              reads=[("lnst", i)], writes=[("lnmv", i)])
        S.act(lambda e: e.activation(out=sm[:, i, 0:1], in_=mv[:, i, 1:2], func=AF.Sqrt, bias=self.epsb[:, 0:1], scale=1.0),
              reads=[("lnmv", i), "epsb"], writes=[("lnsm0", i)])
        S.dve(lambda e: e.reciprocal(out=sm[:, i, 1:2], in_=sm[:, i, 0:1]), reads=[("lnsm0", i)], writes=[("lnsm1", i)])
        S.dve(lambda e: e.tensor_scalar(out=sm[:, i, 2:3], in0=mv[:, i, 0:1], scalar1=sm[:, i, 1:2], scalar2=-1.0,
                                        op0=ALU.mult, op1=ALU.mult),
              reads=[("lnmv", i), ("lnsm1", i)], writes=[("lnsm2", i)])

    def _ln_apply(self, L, b, s, sk, i, router):
        S = self.S
        sm = L["sm"]
        S.act(lambda e: e.activation(out=s[:], in_=s[:], func=AF.Identity, bias=sm[:, i, 2:3], scale=sm[:, i, 1:2]),
              reads=[sk, ("lnsm1", i), ("lnsm2", i)], writes=[sk])
        S.dve(lambda e: e.tensor_tensor(out=s[:], in0=s[:], in1=L["g"][:], op=ALU.mult), reads=[sk, "lng"], writes=[sk])
        S.dve(lambda e: e.tensor_tensor(out=s[:], in0=s[:], in1=L["b"][:], op=ALU.add), reads=[sk, "lnb"], writes=[sk])
        S.dma("sp", lambda e: e.dma_start(out=self.y[b * 128:(b + 1) * 128, :], in_=s[:]), reads=[sk], writes=[("y", b)], semkey=("yst", i))
        self.make_xT(s, sk, b, xtf=(router["xtf"] if router else None))
        if router:
            self.route(router, b)

    def ln_block(self, L, b, pouts, pkeys, scale_ap=None, acc=None, acck=None, i=None):
        S = self.S
        if i is None:
            i = L["n"] % 2
        s = L["s"][i]
        sk = ("lns", i)
        if acc is None:
            xr, xk = self.ln_load_x(L, b, i=i)
            for h in range(2):
                S.dve(lambda e, h=h, xr=xr: e.scalar_tensor_tensor(out=s[:, h * 512:(h + 1) * 512], in0=xr[:, h * 512:(h + 1) * 512],
                                                                   scalar=ALPHA, in1=pouts[h][:], op0=ALU.mult, op1=ALU.add),
                      reads=[xk, pkeys[h]], writes=[sk])
        else:
            for h in range(2):
                sc = 1.0 if scale_ap is None else scale_ap
                S.dve(lambda e, h=h, sc=sc: e.scalar_tensor_tensor(out=s[:, h * 512:(h + 1) * 512], in0=pouts[h][:],
                                                                   scalar=sc, in1=acc[:, h * 512:(h + 1) * 512],
                                                                   op0=ALU.mult, op1=ALU.add),
                      reads=[acck, pkeys[h], "gates"], writes=[sk])
        return s, sk

    def run_pipeline(self, its, stages, lags):
        offs = [0]
        for l in lags:
            offs.append(offs[-1] + l)
        n = len(its)
        for step in range(n + offs[-1]):
            for s in range(len(stages)):
                i = step - offs[s]
                if 0 <= i < n:
                    stages[s](its[i], i)

    def wload(self, dst, src_ap, key, reads=(), semkey=None):
        self.S.dma("pool", lambda e: e.dma_start(out=dst, in_=src_ap), reads=reads, writes=[key], semkey=semkey)

    def ffn_stage(self, sctx, layer, moe):
        S, P = self.S, self.P
        L = self.ln_setup(sctx, layer, "ffn")
        self.epsb = self.sb(sctx, "epsb", [128, 1], F32)
        S.dve(lambda e: e.memset(self.epsb[:], LN_EPS), writes=["epsb"])
        if moe:
            F = 3584
            experts = list(range(8))
            Wg = self.inp("l%d_moe_w_gate" % layer, [8, D, F])
            Wu = self.inp("l%d_moe_w_up" % layer, [8, D, F])
            Wd = self.inp("l%d_moe_w_down" % layer, [8, F, D])
            passes = [(e, j0, 7) for e in experts for j0 in (0, 7, 14, 21)]
        else:
            F = 2816
            Wg = self.inp("l%d_ffn_w_gate" % layer, [D, F])
            Wu = self.inp("l%d_ffn_w_up" % layer, [D, F])
            Wd = self.inp("l%d_ffn_w_down" % layer, [F, D])
            passes = [(None, 0, 8), (None, 8, 7), (None, 15, 7)]
        NH = 8
        acc = self.sb(sctx, "acc", [128, NB, D], F32)
        hT = self.sb(sctx, "hT", [128, NH, T], BF16)
        wd = self.sb(sctx, "wd", [128, NH, D], BF16)
        GW = 4
        wg = [self.sb(sctx, "wg", [128, 8, GW * 128], BF16) for _ in range(2)]
        wu = [self.sb(sctx, "wu", [128, 8, GW * 128], BF16) for _ in range(2)]
        sg = [self.sb(sctx, "sg", [128, 512], F32) for _ in range(2)]
        gi = 0
        si = 0
        for pi, (ex, j0, nch) in enumerate(passes):
            first, last = pi == 0, pi == len(passes) - 1
            wgs = Wg if ex is None else Wg[ex]
            wus = Wu if ex is None else Wu[ex]
            wds = Wd if ex is None else Wd[ex]
            for g0 in range(0, nch, GW):
                gn = min(GW, nch - g0)
                slot = gi % 2
                gi += 1
                c0 = (j0 + g0) * 128
                self.wload(wg[slot][:, :, 0:gn * 128], wgs[:, c0:c0 + gn * 128].rearrange("(k p) c -> p k c", p=128), ("wg", slot))
                self.wload(wu[slot][:, :, 0:gn * 128], wus[:, c0:c0 + gn * 128].rearrange("(k p) c -> p k c", p=128), ("wu", slot))
                for jj in range(g0, g0 + gn):
                    jl = jj - g0
                    for tt in range(4):
                        pg, pu = P[(si % 2)], P[2 + (si % 2)]
                        kg, ku = ("ps", si % 2), ("ps", 2 + si % 2)
                        sgt, sgk = sg[si % 2], ("sg", si % 2)
                        si += 1
                        xr = [("xT", tt * 4 + q) for q in range(4)]
                        for k in range(8):
                            S.pe(lambda e, pg=pg, k=k, slot=slot, jl=jl, tt=tt: e.matmul(
                                pg[:], lhsT=wg[slot][:, k, jl * 128:(jl + 1) * 128], rhs=self.xT[:, k, tt * 512:(tt + 1) * 512],
                                start=(k == 0), stop=(k == 7)), reads=[("wg", slot)] + xr, writes=[kg])
                        for k in range(8):
                            S.pe(lambda e, pu=pu, k=k, slot=slot, jl=jl, tt=tt: e.matmul(
                                pu[:], lhsT=wu[slot][:, k, jl * 128:(jl + 1) * 128], rhs=self.xT[:, k, tt * 512:(tt + 1) * 512],
                                start=(k == 0), stop=(k == 7)), reads=[("wu", slot)] + xr, writes=[ku])
                        S.act(lambda e, pg=pg, sgt=sgt: e.activation(out=sgt[:], in_=pg[:], func=AF.Silu),
                              reads=[kg], writes=[sgk])
                        S.dve(lambda e, pu=pu, sgt=sgt, jj=jj, tt=tt: e.tensor_tensor(
                            out=hT[:, jj, tt * 512:(tt + 1) * 512], in0=sgt[:], in1=pu[:], op=ALU.mult),
                            reads=[sgk, ku], writes=[("hT", jj, tt)])
            for jj in range(nch):
                r0 = (j0 + jj) * 128
                self.wload(wd[:, jj, :], wds[r0:r0 + 128, :], ("wd", jj))
            for b in range(NB):
                po = [P[4 + 2 * (b % 2)], P[5 + 2 * (b % 2)]]
                pk = [("ps", 4 + 2 * (b % 2)), ("ps", 5 + 2 * (b % 2))]
                for jj in range(nch):
                    for h in range(2):
                        S.pe(lambda e, jj=jj, h=h, b=b, po=po: e.matmul(
                            po[h][:], lhsT=hT[:, jj, b * 128:(b + 1) * 128], rhs=wd[:, jj, h * 512:(h + 1) * 512],
                            start=(jj == 0), stop=(jj == nch - 1)),
                            reads=[("hT", jj, b // 4), ("wd", jj)], writes=[pk[h]])
                gate = None if ex is None else self.gates[:, b, ex:ex + 1]
                acck = ("acc", b)
                if first:
                    xr, xk = self.ln_load_x(L, b)
                    L["n"] += 1
                    for h in range(2):
                        if gate is None:
                            S.dve(lambda e, h=h, xr=xr, b=b, po=po: e.scalar_tensor_tensor(
                                out=acc[:, b, h * 512:(h + 1) * 512], in0=xr[:, h * 512:(h + 1) * 512], scalar=ALPHA,
                                in1=po[h][:], op0=ALU.mult, op1=ALU.add), reads=[xk, pk[h]], writes=[acck])
                        else:
                            S.act(lambda e, h=h, xr=xr, b=b: e.activation(out=acc[:, b, h * 512:(h + 1) * 512],
                                                                          in_=xr[:, h * 512:(h + 1) * 512], func=AF.Copy, scale=ALPHA),
                                  reads=[xk], writes=[acck])
                            S.dve(lambda e, h=h, b=b, po=po, gate=gate: e.scalar_tensor_tensor(
                                out=acc[:, b, h * 512:(h + 1) * 512], in0=po[h][:], scalar=gate,
                                in1=acc[:, b, h * 512:(h + 1) * 512], op0=ALU.mult, op1=ALU.add),
                                reads=[acck, pk[h], "gates"], writes=[acck])
                elif not last:
                    for h in range(2):
                        S.dve(lambda e, h=h, b=b, po=po, gate=gate: e.scalar_tensor_tensor(
                            out=acc[:, b, h * 512:(h + 1) * 512], in0=po[h][:], scalar=(1.0 if gate is None else gate),
                            in1=acc[:, b, h * 512:(h + 1) * 512], op0=ALU.mult, op1=ALU.add),
                            reads=[acck, pk[h], "gates"], writes=[acck])
                else:
                    s, sk = self.ln_block(L, b, po, pk, scale_ap=gate, acc=acc[:, b, :], acck=acck, i=b % 2)
                    self.ln_finish(L, b, s, sk, i=b % 2, part="stats")
                    if b > 0:
                        self.ln_finish(L, b - 1, prev_s[0], prev_s[1], i=(b - 1) % 2, part="apply")
                    prev_s = (s, sk)
                    if b == NB - 1:
                        self.ln_finish(L, b, s, sk, i=b % 2, part="apply")

    def moe_stage(self, sctx, layer):
        S, P, nc = self.S, self.P, self.nc
        import os
        NSB = int(os.environ.get("KDBG_NSB", "5"))
        CS = NSB * 128
        tiles = [(0, min(512, CS))] + ([(512, CS - 512)] if CS > 512 else [])
        F = 3584
        Wg = self.inp("l%d_moe_w_gate" % layer, [8, D, F])
        Wu = self.inp("l%d_moe_w_up" % layer, [8, D, F])
        Wd = self.inp("l%d_moe_w_down" % layer, [8, F, D])
        self.epsb = self.sb(sctx, "epsb", [128, 1], F32)
        S.dve(lambda e: e.memset(self.epsb[:], LN_EPS), writes=["epsb"])
        acc = self.sb(sctx, "acc", [128, NB, D], F32)
        with ExitStack() as mx:
            xtok = self.sb(mx, "xtok", [128, NB, D], BF16)
            flat = self.xT[:].rearrange("p k t -> p (k t)")
            xgT = flat[:, 0:8 * CS].rearrange("p (k c) -> p k c", k=8)
            hT2 = flat[:, 8 * CS:15 * CS].rearrange("p (k c) -> p k c", k=7)
            yb = flat[:, 15 * CS:15 * CS + NSB * D].rearrange("p (s d) -> p s d", s=NSB)
            Sx = self.sb(mx, "Sx", [128, NB, CS], BF16)
            STr = [self.sb(mx, "STr", [128, NSB, 512], BF16) for _ in range(1)]
            wd = self.sb(mx, "wd", [128, 7, D], BF16)
            wg = [self.sb(mx, "wg", [128, 8, 128], BF16) for _ in range(2)]
            wu = [self.sb(mx, "wu", [128, 8, 128], BF16) for _ in range(2)]
            sg = [self.sb(mx, "sg", [128, 512], F32) for _ in range(1)]
            yacc = self.sb(mx, "yacc", [128, NSB, D], F32)
            maskf = self.sb(mx, "maskf", [128, NB, 8], F32)
            maskb = self.sb(mx, "maskb", [128, NB, 32], BF16)
            nmb = self.sb(mx, "nmb", [128, NB, 8], BF16)
            rk = self.sb(mx, "rk", [128, NB, 8], F32)
            ioi = self.sb(mx, "ioi", [128, CS], mybir.dt.int32)
            cii = self.sb(mx, "cii", [128, NSB], mybir.dt.int32)
            cidx = self.sb(mx, "cidx", [128, NSB], F32)
            io = ioi
            rk2 = self.sb(mx, "rk2", [128, NB, 8], F32)
            cid2 = self.sb(mx, "cid2", [128, NSB], F32)
            cnti = self.sb(mx, "cnti", [128, 32], mybir.dt.int32)
            cntD = nc.dram_tensor("cntD%d" % layer, [1, 32], mybir.dt.int32, kind="Internal").ap()
            UW = self.sb(mx, "UW", [128, 896], BF16)
            ones5 = self.onesb[:, 0:1].to_broadcast([128, 512])
            for b in range(NB):
                self.wload(xtok[:, b, :], self.y[b * 128:(b + 1) * 128, :], ("xtok", b), semkey="xtokall")
                S.dma("sp", lambda e, b=b: e.dma_start(out=acc[:, b, :], in_=self.y[b * 128:(b + 1) * 128, :]), writes=[("accld", b)],
                      semkey="accld")
            for b in range(NB):
                S.act(lambda e, b=b: e.activation(out=acc[:, b, :], in_=acc[:, b, :], func=AF.Copy, scale=ALPHA),
                      reads=[("accld", bb) for bb in range(NB)], writes=[("acc", b)])
            S.dve(lambda e: e.tensor_scalar(out=maskf[:], in0=self.gates[:], scalar1=0.0, scalar2=None, op0=ALU.is_gt),
                  reads=["gates"], writes=["maskf"])
            S.dve(lambda e: e.memset(maskb[:], 0.0), writes=["maskb"])
            S.dve(lambda e: e.tensor_copy(out=maskb[:, :, 0:8], in_=maskf[:]), reads=["maskf", "maskb"], writes=["maskb"])
            S.dve(lambda e: e.tensor_scalar(out=nmb[:], in0=maskf[:], scalar1=-4096.0, scalar2=4096.0, op0=ALU.mult, op1=ALU.add),
                  reads=["maskf"], writes=["nmb"])
            S.pool(lambda e: e.iota(out=ioi[:], pattern=[[1, CS]], base=0, channel_multiplier=0), writes=["ioi"])
            S.dve(lambda e: e.memset(cidx[:], 0.0), reads=["ioi"], writes=["io"])
            S.pool(lambda e: e.iota(out=cii[:], pattern=[[128, NSB]], base=0, channel_multiplier=1), writes=["cii"])
            S.dve(lambda e: e.tensor_copy(out=cidx[:], in_=cii[:]), reads=["cii"], writes=["cidx"])
            S.dve(lambda e: e.memset(UW[:, 0:384], 0.0), writes=["U4"])
            S.dve(lambda e: e.tensor_copy(out=UW[:, 384:512], in_=self.mstrict[:]), reads=["mstrict", "U4"], writes=["U4"])
            S.dve(lambda e: e.memset(UW[:, 512:896], 1.0), reads=["U4"], writes=["U4"])
            for b in range(NB):
                pr, prk = P[b % 2], ("ps", b % 2)
                for b2 in range(b + 1):
                    lhs = self.onesb[:] if b2 < b else self.mstrict[:]
                    S.pe(lambda e, b2=b2, lhs=lhs, pr=pr, b=b: e.matmul(pr[:, 0:32], lhsT=lhs, rhs=maskb[:, b2, :], start=(b2 == 0), stop=(b2 == b)),
                         reads=["maskb", "onesb", "mstrict"], writes=[prk])
                S.dve(lambda e, b=b, pr=pr: e.tensor_copy(out=rk[:, b, :], in_=pr[:, 0:8]), reads=[prk], writes=["rk"])
            for b in range(NB):
                S.pe(lambda e, b=b: e.matmul(P[2][:, 0:32], lhsT=self.onesb[:], rhs=maskb[:, b, :], start=(b == 0), stop=(b == NB - 1)),
                     reads=["maskb", "onesb"], writes=[("ps", 2)])
            S.dve(lambda e: e.tensor_copy(out=cnti[:], in_=P[2][:, 0:32]), reads=[("ps", 2)], writes=["cnti"])
            S.dma("sp", lambda e: e.dma_start(out=cntD, in_=cnti[0:1, :]), reads=["cnti"], writes=["cntD"])
            gi = 0
            si = 0
            gn = 0
            NROUND = (T + CS - 1) // CS
            for ex in range(8):
                for rnd in range(NROUND):
                    R0 = rnd * CS
                    if rnd == 0:
                        rkR, cidR = rk, cidx
                    else:
                        if rnd == 1:
                            for en in ("pe", "act", "dve", "pool", "sp"):
                                S.add(en, lambda e: e.nop(), reads=["cntD"])
                            S.ctl("if", ap=cntD[0:1, ex:ex + 1], thresh=CS)
                        rkR, cidR = rk2, cid2
                        S.dve(lambda e, R0=R0: e.tensor_scalar(out=rk2[:], in0=rk[:], scalar1=float(-R0), scalar2=None, op0=ALU.add),
                              reads=["rk"], writes=["rk2"])
                        S.dve(lambda e, R0=R0: e.tensor_scalar(out=cid2[:], in0=cidx[:], scalar1=float(R0), scalar2=None, op0=ALU.add),
                              reads=["cidx"], writes=["cid2"])
                    for b in range(NB):
                        S.dve(lambda e, b=b, ex=ex, rkR=rkR: e.tensor_scalar(out=Sx[:, b, :], in0=io[:], scalar1=rkR[:, b, ex:ex + 1],
                                                                    scalar2=maskf[:, b, ex:ex + 1], op0=ALU.is_equal, op1=ALU.mult),
                              reads=["io", "rk", "rk2", "maskf"], writes=[("Sx", b)])
                    for k in range(8):
                        for (c0, cw) in tiles:
                            pgk = gn % 2
                            gn += 1
                            pg, pgkk = P[pgk], ("ps", pgk)
                            for b in range(NB):
                                S.pe(lambda e, b=b, k=k, c0=c0, cw=cw, pg=pg: e.matmul(pg[:, 0:cw], lhsT=xtok[:, b, k * 128:(k + 1) * 128],
                                                                                      rhs=Sx[:, b, c0:c0 + cw], start=(b == 0), stop=(b == NB - 1)),
                                     reads=[("xtok", bb) for bb in range(NB)] + [("Sx", b)], writes=[pgkk])
                            S.act(lambda e, k=k, c0=c0, cw=cw, pg=pg: e.activation(out=xgT[:, k, c0:c0 + cw], in_=pg[:, 0:cw], func=AF.Copy),
                                  reads=[pgkk], writes=[("xgT", k)])
                    xgk = [("xgT", k) for k in range(8)]
                    for pi, j0 in enumerate((0, 7, 14, 21)):
                        nch = 7
                        firstp, lastp = pi == 0, pi == 3
                        for jj in range(nch):
                            slot = gi % 2
                            gi += 1
                            c0 = (j0 + jj) * 128
                            self.wload(wg[slot][:], Wg[ex][:, c0:c0 + 128].rearrange("(k p) c -> p k c", p=128), ("wg", slot))
                            self.wload(wu[slot][:], Wu[ex][:, c0:c0 + 128].rearrange("(k p) c -> p k c", p=128), ("wu", slot))
                            for (t0, tw) in tiles:
                                pg, pu = P[(si % 2)], P[2 + (si % 2)]
                                kg, ku = ("ps", si % 2), ("ps", 2 + si % 2)
                                sgt, sgk = sg[0], ("sg", 0)
                                si += 1
                                for k in range(8):
                                    S.pe(lambda e, pg=pg, k=k, slot=slot, t0=t0, tw=tw: e.matmul(
                                        pg[:, 0:tw], lhsT=wg[slot][:, k, :], rhs=xgT[:, k, t0:t0 + tw], start=(k == 0), stop=(k == 7)),
                                        reads=[("wg", slot)] + xgk, writes=[kg])
                                for k in range(8):
                                    S.pe(lambda e, pu=pu, k=k, slot=slot, t0=t0, tw=tw: e.matmul(
                                        pu[:, 0:tw], lhsT=wu[slot][:, k, :], rhs=xgT[:, k, t0:t0 + tw], start=(k == 0), stop=(k == 7)),
                                        reads=[("wu", slot)] + xgk, writes=[ku])
                                S.act(lambda e, pg=pg, sgt=sgt, tw=tw: e.activation(out=sgt[:, 0:tw], in_=pg[:, 0:tw], func=AF.Silu),
                                      reads=[kg], writes=[sgk])
                                S.dve(lambda e, pu=pu, sgt=sgt, jj=jj, t0=t0, tw=tw: e.tensor_tensor(
                                    out=hT2[:, jj, t0:t0 + tw], in0=sgt[:, 0:tw], in1=pu[:, 0:tw], op=ALU.mult),
                                    reads=[sgk, ku], writes=[("hT2", jj)])
                        for jj in range(nch):
                            r0 = (j0 + jj) * 128
                            self.wload(wd[:, jj, :], Wd[ex][r0:r0 + 128, :], ("wd", jj))
                        for sbk in range(NSB):
                            po = [P[4 + 2 * (sbk % 2)], P[5 + 2 * (sbk % 2)]]
                            pk = [("ps", 4 + 2 * (sbk % 2)), ("ps", 5 + 2 * (sbk % 2))]
                            for jj in range(nch):
                                for h in range(2):
                                    S.pe(lambda e, jj=jj, h=h, sbk=sbk, po=po: e.matmul(
                                        po[h][:], lhsT=hT2[:, jj, sbk * 128:(sbk + 1) * 128], rhs=wd[:, jj, h * 512:(h + 1) * 512],
                                        start=(jj == 0), stop=(jj == nch - 1)), reads=[("hT2", jj), ("wd", jj)], writes=[pk[h]])
                            for h in range(2):
                                if firstp:
                                    S.act(lambda e, h=h, sbk=sbk, po=po: e.activation(out=yacc[:, sbk, h * 512:(h + 1) * 512], in_=po[h][:], func=AF.Copy),
                                          reads=[pk[h]], writes=[("yacc", sbk)])
                                elif not lastp:
                                    S.dve(lambda e, h=h, sbk=sbk, po=po: e.tensor_tensor(out=yacc[:, sbk, h * 512:(h + 1) * 512], in0=yacc[:, sbk, h * 512:(h + 1) * 512],
                                                                                       in1=po[h][:], op=ALU.add), reads=[pk[h], ("yacc", sbk)], writes=[("yacc", sbk)])
                                else:
                                    S.dve(lambda e, h=h, sbk=sbk, po=po: e.tensor_tensor(out=yb[:, sbk, h * 512:(h + 1) * 512], in0=yacc[:, sbk, h * 512:(h + 1) * 512],
                                                                                       in1=po[h][:], op=ALU.add), reads=[pk[h], ("yacc", sbk)], writes=[("yb", sbk)])
                    ybk = [("yb", s_) for s_ in range(NSB)]
                    for tq in range(4):
                        prb, prbk = P[0], ("ps", 0)
                        nb2 = 4 * tq + 4
                        for b2 in range(nb2):
                            rhs = ones5 if b2 < 4 * tq else UW[:, (3 - (b2 - 4 * tq)) * 128:(3 - (b2 - 4 * tq)) * 128 + 512]
                            S.pe(lambda e, b2=b2, rhs=rhs, ex=ex: e.matmul(prb[:], lhsT=maskb[:, b2, ex:ex + 1].to_broadcast([128, 128]), rhs=rhs,
                                                                          start=(b2 == 0), stop=False),
                                 reads=["maskb", "onesb", "U4"], writes=[prbk])
                        for q in range(4):
                            b2 = 4 * tq + q
                            S.pe(lambda e, b2=b2, q=q, ex=ex: e.matmul(prb[:, q * 128:(q + 1) * 128], lhsT=nmb[:, b2, ex:ex + 1].to_broadcast([128, 128]),
                                                                      rhs=self.identb[:], start=False, stop=(q == 3)),
                                 reads=["nmb", "identb"], writes=[prbk])
                        st_ = STr[0]
                        for sbk in range(NSB):
                            S.dve(lambda e, sbk=sbk, cidR=cidR: e.tensor_scalar(out=st_[:, sbk, :], in0=prb[:], scalar1=cidR[:, sbk:sbk + 1], scalar2=None,
                                                                     op0=ALU.is_equal), reads=[prbk, "cidx", "cid2"], writes=[("STr", sbk)])
                        for q in range(4):
                            b = 4 * tq + q
                            po = [P[4 + 2 * (b % 2)], P[5 + 2 * (b % 2)]]
                            pk = [("ps", 4 + 2 * (b % 2)), ("ps", 5 + 2 * (b % 2))]
                            for sbk in range(NSB):
                                for h in range(2):
                                    S.pe(lambda e, sbk=sbk, h=h, q=q, po=po: e.matmul(po[h][:], lhsT=st_[:, sbk, q * 128:(q + 1) * 128],
                                                                                  rhs=yb[:, sbk, h * 512:(h + 1) * 512],
                                                                                  start=(sbk == 0), stop=(sbk == NSB - 1)),
                                         reads=[("STr", sbk), ("yb", sbk)], writes=[pk[h]])
                            gate = self.gates[:, b, ex:ex + 1]
                            for h in range(2):
                                S.dve(lambda e, h=h, b=b, po=po, gate=gate: e.scalar_tensor_tensor(
                                    out=acc[:, b, h * 512:(h + 1) * 512], in0=po[h][:], scalar=gate,
                                    in1=acc[:, b, h * 512:(h + 1) * 512], op0=ALU.mult, op1=ALU.add),
                                    reads=[("acc", b), pk[h], "gates"], writes=[("acc", b)])

                    if rnd == NROUND - 1:
                        S.ctl("endif")
            S.flush(barrier=True)
        L = self.ln_setup(sctx, layer, "ffn")
        self.run_pipeline(list(range(NB)),
                          [lambda b, n: self.ln_finish(L, b, acc[:, b, :], ("acc", b), i=b % 2, part="stats"),
                           lambda b, n: self.ln_finish(L, b, acc[:, b, :], ("acc", b), i=b % 2, part="apply")], [1])

    def next_router(self, layer, which):
        import os
        if os.environ.get("KDBG_NOROUTER"):
            return None
        return self.router if (which == "mix" and layer % 2 == 1) else None

    def router_setup(self, sctx, layer):
        S = self.S
        R = {}
        wr = self.inp("l%d_moe_w_router" % layer, [D, 8])
        R["wr"] = self.sb(sctx, "wr", [128, 8, 8], F32)
        with self.nc.allow_non_contiguous_dma(reason="tiny router weight"):
            S.dma("sp", lambda e: e.dma_start(out=R["wr"][:], in_=wr.rearrange("(k p) e -> p k e", p=128)), writes=["wr"])
        R["xtf"] = self.sb(sctx, "xtf", [128, 8, 128], F32)
        R["xh"] = self.sb(sctx, "xh", [128, 8, 128], BF16)
        R["xl"] = self.sb(sctx, "xl", [128, 8, 128], BF16)
        R["wrh"] = self.sb(sctx, "wrh", [128, 8, 32], BF16)
        R["wrl"] = self.sb(sctx, "wrl", [128, 8, 32], BF16)
        S.dve(lambda e: e.memset(R["wrh"][:], 0.0), writes=["wrh"])
        S.dve(lambda e: e.memset(R["wrl"][:], 0.0), writes=["wrl"])
        R["t"] = self.sb(sctx, "rt", [128, 8, 8], F32)
        S.dve(lambda e: e.tensor_copy(out=R["wrh"][:, :, 0:8], in_=R["wr"][:]), reads=["wr", "wrh"], writes=["wrh"])
        S.dve(lambda e: e.tensor_tensor(out=R["wrl"][:, :, 0:8], in0=R["wr"][:], in1=R["wrh"][:, :, 0:8], op=ALU.subtract),
              reads=["wr", "wrh", "wrl"], writes=["wrl"])
        self.router = R
        return R

    def route(self, R, b):
        S, P = self.S, self.P
        pr, pk = P[5], ("ps", 5)
        t = R["t"]
        xk = [("xtf", 0), ("xtf", 1)]
        S.dve(lambda e: e.tensor_copy(out=R["xh"][:], in_=R["xtf"][:]), reads=xk, writes=["xh"])
        S.dve(lambda e: e.tensor_tensor(out=R["xl"][:], in0=R["xtf"][:], in1=R["xh"][:], op=ALU.subtract),
              reads=xk + ["xh"], writes=["xl"])
        combos = [("xh", "wrh"), ("xh", "wrl"), ("xl", "wrh")]
        n = 0
        for k in range(8):
            for (xa, wa) in combos:
                S.pe(lambda e, k=k, xa=xa, wa=wa, n=n: e.matmul(pr[:, 0:32], lhsT=R[xa][:, k, :], rhs=R[wa][:, k, :],
                                                              start=(n == 0), stop=(n == 23)),
                     reads=[xa, wa], writes=[pk])
                n += 1
        lg, mask, ex = t[:, 0, :], t[:, 2, :], t[:, 4, :]
        m1, m2, nm1, den = t[:, 1, 0:1], t[:, 1, 1:2], t[:, 3, 0:1], t[:, 5, 0:1]
        w4, w2, lg2 = t[:, 6, 0:4], t[:, 6, 4:6], t[:, 7, :]
        S.dve(lambda e: e.tensor_copy(out=lg, in_=pr[:, 0:8]), reads=[pk], writes=["r_lg"])

        def max8(src_ap, dst, key_in, key_out):
            S.dve(lambda e: e.tensor_tensor(out=w4, in0=src_ap[:, 0:4], in1=src_ap[:, 4:8], op=ALU.max), reads=[key_in], writes=["r_w4"])
            S.dve(lambda e: e.tensor_tensor(out=w2, in0=t[:, 6, 0:2], in1=t[:, 6, 2:4], op=ALU.max), reads=["r_w4"], writes=["r_w2"])
            S.dve(lambda e: e.tensor_tensor(out=dst, in0=t[:, 6, 4:5], in1=t[:, 6, 5:6], op=ALU.max), reads=["r_w2"], writes=[key_out])
        max8(lg, m1, "r_lg", "r_m1")
        S.dve(lambda e: e.tensor_scalar(out=lg2, in0=lg, scalar1=m1, scalar2=-1e30, op0=ALU.is_equal, op1=ALU.mult),
              reads=["r_lg", "r_m1"], writes=["r_lg2"])
        S.dve(lambda e: e.tensor_tensor(out=lg2, in0=lg2, in1=lg, op=ALU.add), reads=["r_lg2", "r_lg"], writes=["r_lg2"])
        max8(lg2, m2, "r_lg2", "r_m2")
        S.dve(lambda e: e.tensor_scalar(out=mask, in0=lg, scalar1=m2, scalar2=None, op0=ALU.is_ge),
              reads=["r_lg", "r_m2"], writes=["r_mask"])
        S.dve(lambda e: e.tensor_scalar(out=nm1, in0=m1, scalar1=-1.0, scalar2=None, op0=ALU.mult),
              reads=["r_m1"], writes=["r_nm1"])
        S.act(lambda e: e.activation(out=ex, in_=lg, func=AF.Exp, bias=nm1, scale=1.0), reads=["r_lg", "r_nm1"], writes=["r_ex"])
        S.dve(lambda e: e.tensor_tensor(out=ex, in0=ex, in1=mask, op=ALU.mult), reads=["r_ex", "r_mask"], writes=["r_ex"])
        S.dve(lambda e: e.tensor_tensor(out=w4, in0=t[:, 4, 0:4], in1=t[:, 4, 4:8], op=ALU.add), reads=["r_ex"], writes=["r_w4"])
        S.dve(lambda e: e.tensor_tensor(out=w2, in0=t[:, 6, 0:2], in1=t[:, 6, 2:4], op=ALU.add), reads=["r_w4"], writes=["r_w2"])
        S.dve(lambda e: e.tensor_tensor(out=den, in0=t[:, 6, 4:5], in1=t[:, 6, 5:6], op=ALU.add), reads=["r_w2"], writes=["r_den"])
        S.dve(lambda e: e.reciprocal(out=den, in_=den), reads=["r_den"], writes=["r_den"])
        S.dve(lambda e: e.tensor_scalar(out=self.gates[:, b, :], in0=ex, scalar1=den, scalar2=None, op0=ALU.mult),
              reads=["r_ex", "r_den"], writes=["gates"])

    def mixer_out(self, sctx, layer, lhs_fn, lhs_keys_fn, nk, Wo_ap, L, pre_block=None):
        S, P = self.S, self.P
        wo = self.sb(sctx, "wo", [128, nk, D], BF16)
        for k in range(nk):
            self.wload(wo[:, k, :], Wo_ap[k * 128:(k + 1) * 128, :], ("wo", k), semkey=("wo", k % 4))
        rt = self.next_router(layer, "mix")
        hold = {}

        def stA(b, n):
            po = [P[2 * (b % 2)], P[1 + 2 * (b % 2)]]
            pk = [("ps", 2 * (b % 2)), ("ps", 1 + 2 * (b % 2))]
            if pre_block is not None:
                pre_block(b)
            for k in range(nk):
                for h in range(2):
                    S.pe(lambda e, k=k, h=h, b=b, po=po: e.matmul(po[h][:], lhsT=lhs_fn(k, b), rhs=wo[:, k, h * 512:(h + 1) * 512],
                                                              start=(k == 0), stop=(k == nk - 1)),
                         reads=lhs_keys_fn(k, b) + [("wo", kk) for kk in range(k % 4, nk, 4)], writes=[pk[h]])
            s, sk = self.ln_block(L, b, po, pk, i=b % 2)
            self.ln_finish(L, b, s, sk, i=b % 2, part="stats")
            hold[b] = (s, sk)

        def stB(b, n):
            s, sk = hold.pop(b)
            self.ln_finish(L, b, s, sk, router=rt, i=b % 2, part="apply")
        self.run_pipeline(list(range(NB)), [stA, stB], [1])

    def attn_stage(self, sctx, layer, kind):
        S, P = self.S, self.P
        fox = kind == "fox"
        L = self.ln_setup(sctx, layer, "mix")
        self.epsb = self.sb(sctx, "epsb", [128, 1], F32)
        S.dve(lambda e: e.memset(self.epsb[:], LN_EPS), writes=["epsb"])
        if layer % 2 == 1:
            self.router_setup(sctx, layer)
        if fox:
            Wq = self.inp("l%d_fox_w_qkvf" % layer, [D, 3088])
            Wo = self.inp("l%d_fox_w_out" % layer, [D, D])
            bf = self.inp("l%d_fox_b_f" % layer, [16])
        else:
            Wq = self.inp("l%d_sb_w_qkv" % layer, [D, 3072])
            Wo = self.inp("l%d_sb_w_out" % layer, [D, D])
        OT = self.sb(sctx, "OT", [128, 8, T], BF16)
        qT = [self.sb(sctx, "qT", [128, T], BF16) for _ in range(2)]
        kT = [None, None]
        vv = [self.sb(sctx, "vv", [128, NB, 128], BF16) for _ in range(2)]
        wq = [self.sb(sctx, "wq", [128, 8, 384], BF16) for _ in range(2)]
        E = [self.sb(sctx, "E", [128, 512], BF16) for _ in range(4)]
        if fox:
            wf = self.sb(sctx, "wf", [128, 8, 16], BF16)
            nbf = self.sb(sctx, "nbf", [16, 1], F32)
            cnT = self.sb(sctx, "cnT", [128, NB, 16], F32)
            c3 = [self.sb(sctx, "c3", [128, 2 * T], BF16) for _ in range(2)]
            rl = self.sb(sctx, "rl", [128, 512], F32)
            kTp = [[self.sb(sctx, "kTp", [128, T], BF16) for _ in range(2)] for _ in range(2)]
            ones3p = self.sb(sctx, "ones3p", [128, 128], BF16)
            fpx = ExitStack()
            spf = self.sb(fpx, "spf", [16, T], F32)
            cn = self.sb(fpx, "cn", [16, T], F32)
            tmpf = self.sb(fpx, "tmpf", [16, T], F32)
            c3b = self.sb(fpx, "c3b", [16, 3, T], BF16)
            S.dve(lambda e: e.memset(ones3p[:], 0.0), writes=["ones3p"])
            S.dve(lambda e: e.memset(ones3p[0:3, :], 1.0), reads=["ones3p"], writes=["ones3p"])
            for i_ in range(2):
                S.dve(lambda e, i_=i_: e.memset(c3[i_][:], 0.0), writes=[("c3", i_)])
                for j_ in range(2):
                    S.dve(lambda e, i_=i_, j_=j_: e.memset(kTp[i_][j_][:], 0.0), writes=[("kTpz", i_, j_)])
            with self.nc.allow_non_contiguous_dma(reason="tiny"):
                self.wload(wf[:], Wq[:, 3072:3088].rearrange("(k p) c -> p k c", p=128), "wf")
                S.dma("sp", lambda e: e.dma_start(out=nbf[:], in_=bf.rearrange("(p o) -> p o", o=1)), writes=["nbf"])
            S.dve(lambda e: e.tensor_scalar(out=nbf[:], in0=nbf[:], scalar1=-1.0, scalar2=None, op0=ALU.mult),
                  reads=["nbf"], writes=["nbf"])
            for tt in range(4):
                pf, pfk = P[6 + tt % 2], ("ps", 6 + tt % 2)
                for k in range(8):
                    S.pe(lambda e, k=k, tt=tt, pf=pf: e.matmul(pf[0:16, :], lhsT=wf[:, k, :], rhs=self.xT[:, k, tt * 512:(tt + 1) * 512],
                                                            start=(k == 0), stop=(k == 7)),
                         reads=["wf"] + [("xT", tt * 4 + q) for q in range(4)], writes=[pfk])
                S.act(lambda e, tt=tt, pf=pf: e.activation(out=tmpf[:, tt * 512:(tt + 1) * 512], in_=pf[0:16, :], func=AF.Exp,
                                                        bias=nbf[:, 0:1], scale=-1.0), reads=[pfk, "nbf"], writes=[("tmpf", tt)])
                S.act(lambda e, tt=tt: e.activation(out=spf[:, tt * 512:(tt + 1) * 512], in_=tmpf[:, tt * 512:(tt + 1) * 512],
                                                    func=AF.Ln, bias=self.oneb[0:16, 0:1], scale=1.0),
                      reads=[("tmpf", tt), "oneb"], writes=[("spf", tt)])
            allsp = [("spf", tt) for tt in range(4)]
            S.dve(lambda e: e.memset(tmpf[:], 1.0), writes=[("tmpf", tt) for tt in range(4)] + ["m8"])
            S.dve(lambda e: e.tensor_tensor_scan(out=cn[:], data0=tmpf[:], data1=spf[:], initial=0.0, op0=ALU.mult, op1=ALU.add),
                  reads=allsp + ["m8"], writes=["cn"])
            for b in range(NB):
                pt, ptk = P[6 + b % 2], ("ps", 6 + b % 2)
                S.pe(lambda e, b=b, pt=pt: e.transpose(out=pt[:, 0:16], in_=cn[:, b * 128:(b + 1) * 128], identity=self.identf[0:16, 0:16]),
                     reads=["cn", "identf"], writes=[ptk])
                S.dve(lambda e, b=b, pt=pt: e.tensor_copy(out=cnT[:, b, :], in_=pt[:, 0:16]), reads=[ptk], writes=["cnT"])
            S.dve(lambda e: e.tensor_scalar(out=tmpf[:], in0=cn[:], scalar1=-8.0, scalar2=None, op0=ALU.mult),
                  reads=["cn"] + [("tmpf", tt) for tt in range(4)], writes=["m8"])
            for i in range(3):
                S.dve(lambda e, i=i: e.tensor_copy(out=c3b[:, i, :], in_=tmpf[:]), reads=["m8"], writes=[("c3b", i)])
                if i < 2:
                    S.dve(lambda e, i=i: e.tensor_tensor(out=tmpf[:], in0=tmpf[:], in1=c3b[:, i, :], op=ALU.subtract),
                          reads=["m8", ("c3b", i)], writes=["m8"])
            S.dma("sp", lambda e: e.dma_start(out=self.scr[0, :, :].rearrange("i (h t) -> h i t", h=16), in_=c3b[:]),
                  reads=[("c3b", i) for i in range(3)], writes=["scr0"])
            S.flush(barrier=True)
            fpx.close()
        else:
            zs = [self.sb(sctx, "zs", [128, 512], F32) for _ in range(4)]
            ez = [self.sb(sctx, "ez", [128, 512], F32) for _ in range(2)]
            spb = [self.sb(sctx, "spb", [128, 512], BF16) for _ in range(4)]
            lw = [self.sb(sctx, "lw", [128, 512], F32) for _ in range(2)]
            zerob = self.sb(sctx, "zerob", [128, 128], BF16)
            S.dve(lambda e: e.memset(zerob[:], 0.0), writes=["zerob"])
            kTp = [[self.sb(sctx, "kTp", [128, T], BF16) for _ in range(2)] for _ in range(2)]
            for i_ in range(2):
                for j_ in range(2):
                    S.dve(lambda e, i_=i_, j_=j_: e.memset(kTp[i_][j_][:], 0.0), writes=[("kTpz", i_, j_)])
        self.oneb_needed = True
        ei = 0
        for c in range(8):
            sl = c % 2
            for i, off in enumerate((0, 1024, 2048)):
                self.wload(wq[sl][:, :, i * 128:(i + 1) * 128],
                           Wq[:, off + c * 128:off + (c + 1) * 128].rearrange("(k p) c -> p k c", p=128), ("wq", sl, i))
            if fox:
                S.dma("sp", lambda e, sl=sl, c=c: e.dma_start(out=c3[sl][0:3, :], in_=self.scr[0, :, 2 * c * T:(2 * c + 2) * T]),
                      reads=["scr0"], writes=[("c3", sl)])
            for i, dst in enumerate((qT[sl], kT[sl])):
                nm = ("qT", "kT")[i]
                for tt in range(4):
                    pp, ppk = P[6 + tt % 2], ("ps", 6 + tt % 2)
                    for k in range(8):
                        S.pe(lambda e, k=k, tt=tt, pp=pp, i=i, sl=sl: e.matmul(pp[:], lhsT=wq[sl][:, k, i * 128:(i + 1) * 128],
                                                                          rhs=self.xT[:, k, tt * 512:(tt + 1) * 512],
                                                                          start=(k == 0), stop=(k == 7)),
                             reads=[("wq", sl, i)] + [("xT", tt * 4 + q) for q in range(4)], writes=[ppk])
                    if i == 1:
                        S.act(lambda e, tt=tt, pp=pp, sl=sl: e.activation(out=kTp[sl][0][0:64, tt * 512:(tt + 1) * 512], in_=pp[0:64, :], func=AF.Copy),
                              reads=[ppk, ("kTpz", sl, 0)], writes=[(nm, sl, tt)])
                        S.act(lambda e, tt=tt, pp=pp, sl=sl: e.activation(out=kTp[sl][1][64:128, tt * 512:(tt + 1) * 512], in_=pp[64:128, :], func=AF.Copy),
                              reads=[ppk, ("kTpz", sl, 1)], writes=[(nm, sl, tt)])
                    else:
                        S.act(lambda e, dst=dst, tt=tt, pp=pp: e.activation(out=dst[:, tt * 512:(tt + 1) * 512], in_=pp[:], func=AF.Copy),
                              reads=[ppk], writes=[(nm, sl, tt)])
            for bq in range(4):
                pp, ppk = P[6 + bq % 2], ("ps", 6 + bq % 2)
                for q in range(4):
                    b = bq * 4 + q
                    for k in range(8):
                        S.pe(lambda e, k=k, b=b, q=q, pp=pp, sl=sl: e.matmul(pp[:, q * 128:(q + 1) * 128], lhsT=self.xT[:, k, b * 128:(b + 1) * 128],
                                                                        rhs=wq[sl][:, k, 256:384], start=(k == 0), stop=(k == 7)),
                             reads=[("wq", sl, 2), ("xT", b)], writes=[ppk])
                S.act(lambda e, bq=bq, pp=pp, sl=sl: e.activation(out=vv[sl][:, bq * 4:bq * 4 + 4, :],
                                                                 in_=pp[:].rearrange("p (a c) -> p a c", a=4), func=AF.Copy),
                      reads=[ppk], writes=[("vv", sl, bq)])
            its = []
            for qt in range(4):
                nkb = 4 * qt + 4
                if fox:
                    seq = [(hh, kb) for hh in (0, 1) for kb in range(nkb)]
                else:
                    seq = [(hh, kb) for kb in range(nkb - 1, -1, -1) for hh in (0, 1)]
                for idx, (hh, kb) in enumerate(seq):
                    first = (kb == 0) if fox else (kb == nkb - 1)
                    last = (kb == nkb - 1) if fox else (kb == 0)
                    its.append(dict(qt=qt, hh=hh, kb=kb, first=first, last=last, epi=(idx == len(seq) - 1)))

            def geom(it):
                t0 = it["qt"] * 512
                j0 = it["kb"] * 128
                off = max(0, j0 - t0)
                return t0, j0, off, j0 >= t0, 64 * it["hh"]

            if fox:
                PSB = (0, 1, 4)
                POB = ((2, 5), (3, 6))

                def f1(it, n, c=c, sl=sl):
                    t0, j0, off, diag, r0 = geom(it)
                    hh, kb, qt = it["hh"], it["kb"], it["qt"]
                    h = 2 * c + hh
                    ps, psk = P[PSB[n % 3]], ("ps", PSB[n % 3])
                    Et, Ek = E[n % 4], ("E", n % 4)
                    kTh = kTp[sl][hh]
                    S.pe(lambda e: e.matmul(ps[:, off:512], lhsT=kTh[:, j0:j0 + 128], rhs=qT[sl][:, t0 + off:t0 + 512],
                                            start=True, stop=False), reads=[("kT", sl, kb // 4), ("qT", sl, qt)], writes=[psk])
                    S.pe(lambda e: e.matmul(ps[:, off:512], lhsT=ones3p[:], rhs=c3[sl][:, hh * T + t0 + off:hh * T + t0 + 512],
                                            start=False, stop=(not diag)), reads=[("c3", sl), "ones3p"], writes=[psk])
                    if diag:
                        S.pe(lambda e: e.matmul(ps[:, off:off + 128], lhsT=self.identb[:], rhs=self.negm[:], start=False, stop=True),
                             reads=["identb", "negm"], writes=[psk])
                    S.act(lambda e: e.activation(out=Et[:, off:512], in_=ps[:, off:512], func=AF.Exp, bias=cnT[:, kb, h:h + 1], scale=0.125),
                          reads=[psk, "cnT"], writes=[Ek])

                def f2(it, n, c=c, sl=sl):
                    t0, j0, off, diag, r0 = geom(it)
                    kb, qt, hh = it["kb"], it["qt"], it["hh"]
                    Et, Ek = E[n % 4], ("E", n % 4)
                    po, pok = P[POB[hh][0]], ("ps", POB[hh][0])
                    pl, plk = P[POB[hh][1]], ("ps", POB[hh][1])
                    first, last = it["first"], it["last"]
                    S.pe(lambda e: e.matmul(po[:, off:512], lhsT=vv[sl][:, kb, :], rhs=Et[:, off:512], start=first, stop=last),
                         reads=[Ek, ("vv", sl, kb // 4)], writes=[pok])
                    S.pe(lambda e: e.matmul(pl[:, off:512], lhsT=self.onesb[:], rhs=Et[:, off:512], start=first, stop=last),
                         reads=[Ek, "onesb"], writes=[plk])
                    if last:
                        S.dve(lambda e: e.reciprocal(out=rl[r0:r0 + 64, :], in_=pl[r0:r0 + 64, :]), reads=[plk], writes=[("rl", hh)])
                        S.dve(lambda e: e.tensor_tensor(out=OT[r0:r0 + 64, c, t0:t0 + 512], in0=po[r0:r0 + 64, :], in1=rl[r0:r0 + 64, :], op=ALU.mult),
                              reads=[pok, ("rl", hh)], writes=[("OT", c, qt)])
                self.run_pipeline(its, [f1, f2], [3])
            else:
                def s1(it, n, c=c, sl=sl):
                    t0, j0, off, diag, r0 = geom(it)
                    kb, qt = it["kb"], it["qt"]
                    ps, psk = P[(0, 1, 5)[n % 3]], ("ps", (0, 1, 5)[n % 3])
                    zt, zk = zs[n % 4], ("zs", n % 4)
                    et, ek = ez[n % 2], ("ez", n % 2)
                    st_, stk = spb[n % 4], ("spb", n % 4)
                    S.pe(lambda e, kTh=kTp[sl][it["hh"]]: e.matmul(ps[:, off:512], lhsT=kTh[:, j0:j0 + 128], rhs=qT[sl][:, t0 + off:t0 + 512],
                                                                 start=True, stop=(not diag)), reads=[("kT", sl, kb // 4), ("qT", sl, qt)], writes=[psk])
                    if diag:
                        S.pe(lambda e: e.matmul(ps[:, off:off + 128], lhsT=self.identb[:], rhs=self.negs[:], start=False, stop=True),
                             reads=["identb", "negs"], writes=[psk])
                    S.dve(lambda e: e.tensor_scalar(out=zt[:, off:512], in0=ps[:, off:512], scalar1=0.125, scalar2=None, op0=ALU.mult),
                          reads=[psk], writes=[zk])
                    S.act(lambda e: e.activation(out=et[:, off:512], in_=zt[:, off:512], func=AF.Exp), reads=[zk], writes=[ek])
                    S.act(lambda e: e.activation(out=st_[:, off:512], in_=et[:, off:512], func=AF.Ln, bias=self.oneb[:, 0:1], scale=1.0),
                          reads=[ek, "oneb"], writes=[stk])

                def s2(it, n, c=c, sl=sl):
                    t0, j0, off, diag, r0 = geom(it)
                    hh = it["hh"]
                    zt, zk = zs[n % 4], ("zs", n % 4)
                    st_, stk = spb[n % 4], ("spb", n % 4)
                    pc, pck = P[4], ("ps", 4)
                    pr, prk = P[6 + hh], ("ps", 6 + hh)
                    lt, lk = lw[n % 2], ("lw", n % 2)
                    Et, Ek = E[n % 3], ("E", n % 3)
                    first, last = it["first"], it["last"]
                    if it["first"]:
                        S.pe(lambda e, hh=it["hh"]: e.matmul(P[6 + hh][:, 0:512], lhsT=zerob[:], rhs=self.onesb[:, 0:1].to_broadcast([128, 512]),
                                                            start=True, stop=False), reads=["zerob", "onesb"], writes=[("ps", 6 + it["hh"])])
                    S.pe(lambda e: e.matmul(pc[:, off:512], lhsT=self.trige[:], rhs=st_[:, off:512], start=True, stop=True),
                         reads=[stk, "trige"], writes=[pck])
                    S.dve(lambda e: e.tensor_tensor(out=lt[:, off:512], in0=zt[:, off:512], in1=pc[:, off:512], op=ALU.subtract),
                          reads=[zk, pck], writes=[lk])
                    if not first:
                        S.dve(lambda e: e.tensor_tensor(out=lt[:, off:512], in0=lt[:, off:512], in1=pr[:, off:512], op=ALU.subtract),
                              reads=[lk, prk], writes=[lk])
                    if not last:
                        S.pe(lambda e: e.matmul(pr[:, off:512], lhsT=self.onesb[:], rhs=st_[:, off:512], start=False, stop=(it["kb"] == 1)),
                             reads=[stk, "onesb"], writes=[prk])
                    S.act(lambda e: e.activation(out=Et[:, off:512], in_=lt[:, off:512], func=AF.Exp), reads=[lk], writes=[Ek])

                def s3(it, n, c=c, sl=sl):
                    t0, j0, off, diag, r0 = geom(it)
                    kb, qt = it["kb"], it["qt"]
                    Et, Ek = E[n % 3], ("E", n % 3)
                    po, pok = P[2 + it["hh"]], ("ps", 2 + it["hh"])
                    S.pe(lambda e: e.matmul(po[:, off:512], lhsT=vv[sl][:, kb, :], rhs=Et[:, off:512],
                                            start=it["first"], stop=it["last"], skip_group_check=True),
                         reads=[Ek, ("vv", sl, kb // 4)], writes=[pok])
                    if it["last"]:
                        S.act(lambda e: e.activation(out=OT[r0:r0 + 64, c, t0:t0 + 512], in_=po[r0:r0 + 64, :], func=AF.Copy),
                              reads=[pok], writes=[("OT", c, qt)])
                self.run_pipeline(its, [s1, s2, s3], [2, 2])
        self.mixer_out(sctx, layer, lambda k, b: OT[:, k, b * 128:(b + 1) * 128], lambda k, b: [("OT", k, b // 4)], 8, Wo, L)

    def colvec(self, ctx2, vec_ap, n, dst, key):
        S, P = self.S, self.P
        tmp = self.sb(ctx2, "cvt", [n, 128], F32)
        tk = ("cvt", key)
        S.dma("sp", lambda e: e.dma_start(out=tmp[:], in_=vec_ap.rearrange("(c p) -> c p", p=128)), writes=[tk])
        S.pe(lambda e: e.transpose(out=P[7][:, 0:n], in_=tmp[:], identity=self.identf[0:n, 0:n]), reads=[tk, "identf"], writes=[("ps", 7)])
        S.dve(lambda e: e.tensor_copy(out=dst, in_=P[7][:, 0:n]), reads=[("ps", 7)], writes=[key])

    def ssd_stage(self, sctx, layer):
        S, P, nc = self.S, self.P, self.nc
        pre = "l%d_ssd_" % layer
        Win = self.inp(pre + "w_in", [D, 5152])
        convw = self.inp(pre + "conv_w", [4, 3072])
        convb = self.inp(pre + "conv_b", [3072])
        dtbias = self.inp(pre + "dt_bias", [32])
        alog = self.inp(pre + "a_log", [32])
        dskip = self.inp(pre + "d_skip", [32])
        normw = self.inp(pre + "norm_w", [2048])
        Wo = self.inp(pre + "w_out", [2048, D])
        L = self.ln_setup(sctx, layer, "mix")
        self.epsb = self.sb(sctx, "epsb", [128, 1], F32)
        S.dve(lambda e: e.memset(self.epsb[:], LN_EPS), writes=["epsb"])
        if layer % 2 == 1:
            self.router_setup(sctx, layer)
        ysp = nc.dram_tensor("ysp%d" % layer, [16, 128, T], BF16, kind="Internal").ap()
        cw = self.sb(sctx, "cw", [128, 24, 4], F32)
        cb = self.sb(sctx, "cb", [128, 24], F32)
        nwp = self.sb(sctx, "nwp", [128, 16], F32)
        dskp = self.sb(sctx, "dskp", [128, 16], F32)
        nAT = self.sb(sctx, "nAT", [128, NB, 32], F32)
        dtk = self.sb(sctx, "dtk", [128, NB, 32], F32)
        with ExitStack() as c2:
            self.colvec(c2, convb, 24, cb[:], "cb")
            self.colvec(c2, normw, 16, nwp[:], "nwp")
            cwr = self.sb(c2, "cwr", [4, 3072], F32)
            S.dma("sp", lambda e: e.dma_start(out=cwr[:], in_=convw), writes=["cwr"])
            for fc in range(24):
                S.pe(lambda e, fc=fc: e.transpose(out=P[6][:, fc * 4:fc * 4 + 4], in_=cwr[:, fc * 128:(fc + 1) * 128],
                                                  identity=self.identf[0:4, 0:4]), reads=["cwr", "identf"], writes=[("ps", 6)])
            S.dve(lambda e: e.tensor_copy(out=cw[:].rearrange("p a b -> p (a b)"), in_=P[6][:, 0:96]), reads=[("ps", 6)], writes=["cw"])
            with nc.allow_non_contiguous_dma(reason="tiny"):
                pass
            d2 = dskip.rearrange("(c two) -> two c", two=2)
            S.dma("sp", lambda e: e.dma_start(out=dskp[0:64, :], in_=d2[0].partition_broadcast(64), allow_slow_non_contiguous=True), writes=["dskp0"])
            S.dma("sp", lambda e: e.dma_start(out=dskp[64:128, :], in_=d2[1].partition_broadcast(64), allow_slow_non_contiguous=True), writes=["dskp1"])
            wdt = self.sb(c2, "wdt", [128, 8, 32], BF16)
            self.wload(wdt[:], Win[:, 5120:5152].rearrange("(k p) c -> p k c", p=128), "wdt")
            dtb = self.sb(c2, "dtb", [32, 1], F32)
            al = self.sb(c2, "al", [32, 1], F32)
            S.dma("sp", lambda e: e.dma_start(out=dtb[:], in_=dtbias.rearrange("(p o) -> p o", o=1)), writes=["dtb"])
            S.dma("sp", lambda e: e.dma_start(out=al[:], in_=alog.rearrange("(p o) -> p o", o=1)), writes=["al"])
            S.act(lambda e: e.activation(out=al[:], in_=al[:], func=AF.Exp), reads=["al"], writes=["al"])
            S.dve(lambda e: e.tensor_scalar(out=al[:], in0=al[:], scalar1=-1.0, scalar2=None, op0=ALU.mult), reads=["al"], writes=["al"])
            dtT = self.sb(c2, "dtT", [32, T], F32)
            An = self.sb(c2, "An", [32, T], F32)
            tm = self.sb(c2, "tm", [32, T], F32)
            Ad = self.sb(c2, "Ad", [32, T], F32)
            c3b = self.sb(c2, "c3b", [32, 3, T], BF16)
            for tt in range(4):
                pf, pfk = P[tt % 2], ("ps", tt % 2)
                for k in range(8):
                    S.pe(lambda e, k=k, tt=tt, pf=pf: e.matmul(pf[0:32, :], lhsT=wdt[:, k, :], rhs=self.xT[:, k, tt * 512:(tt + 1) * 512],
                                                            start=(k == 0), stop=(k == 7)),
                         reads=["wdt"] + [("xT", tt * 4 + q) for q in range(4)], writes=[pfk])
                S.act(lambda e, tt=tt, pf=pf: e.activation(out=tm[:, tt * 512:(tt + 1) * 512], in_=pf[0:32, :], func=AF.Exp,
                                                        bias=dtb[:, 0:1], scale=1.0), reads=[pfk, "dtb"], writes=[("tm", tt)])
                S.act(lambda e, tt=tt: e.activation(out=dtT[:, tt * 512:(tt + 1) * 512], in_=tm[:, tt * 512:(tt + 1) * 512],
                                                    func=AF.Ln, bias=self.oneb[0:32, 0:1], scale=1.0),
                      reads=[("tm", tt), "oneb"], writes=[("dtT", tt)])
            alld = [("dtT", tt) for tt in range(4)]
            allt = [("tm", tt) for tt in range(4)]
            S.dve(lambda e: e.tensor_scalar(out=Ad[:], in0=dtT[:], scalar1=al[:, 0:1], scalar2=None, op0=ALU.mult),
                  reads=alld + ["al"], writes=["da"])
            S.dve(lambda e: e.memset(tm[:], 1.0), writes=allt + ["tm1"])
            S.dve(lambda e: e.tensor_tensor_scan(out=An[:], data0=tm[:], data1=Ad[:], initial=0.0, op0=ALU.mult, op1=ALU.add),
                  reads=["da", "tm1"], writes=["An"])
            for b in range(NB):
                pt, ptk = P[2 + b % 2], ("ps", 2 + b % 2)
                S.pe(lambda e, b=b, pt=pt: e.transpose(out=pt[:, 0:32], in_=An[:, b * 128:(b + 1) * 128], identity=self.identf[0:32, 0:32]),
                     reads=["An", "identf"], writes=[ptk])
                S.pe(lambda e, b=b, pt=pt: e.transpose(out=pt[:, 32:64], in_=dtT[:, b * 128:(b + 1) * 128], identity=self.identf[0:32, 0:32]),
                     reads=alld + ["identf"], writes=[ptk])
                S.dve(lambda e, b=b, pt=pt: e.tensor_scalar(out=nAT[:, b, :], in0=pt[:, 0:32], scalar1=-1.0, scalar2=None, op0=ALU.mult),
                      reads=[ptk], writes=["nAT"])
                S.dve(lambda e, b=b, pt=pt: e.tensor_copy(out=dtk[:, b, :], in_=pt[:, 32:64]), reads=[ptk], writes=["dtk"])
            S.dve(lambda e: e.tensor_copy(out=tm[:], in_=An[:]), reads=["An", "tm1"], writes=["m8"])
            for i in range(3):
                S.dve(lambda e, i=i: e.tensor_copy(out=c3b[:, i, :], in_=tm[:]), reads=["m8"], writes=[("c3b", i)])
                if i < 2:
                    S.dve(lambda e, i=i: e.tensor_tensor(out=tm[:], in0=tm[:], in1=c3b[:, i, :], op=ALU.subtract),
                          reads=["m8", ("c3b", i)], writes=["m8"])
            for a in range(2):
                S.dma("sp", lambda e, a=a: e.dma_start(out=self.scr[1 + a, :, :].rearrange("i (h t) -> h i t", h=16), in_=c3b[a * 16:(a + 1) * 16, :, :]),
                      reads=[("c3b", i) for i in range(3)], writes=[("scrA", a)])
            S.flush(barrier=True)
        with ExitStack() as c3x:
            xsT = self.sb(c3x, "xsT", [128, 4, T], BF16)
            zT = self.sb(c3x, "zT", [128, 4, T], BF16)
            Vg = self.sb(c3x, "Vg", [128, NB, 512], BF16)
            BT = self.sb(c3x, "BT", [128, T], BF16)
            CT = self.sb(c3x, "CT", [128, T], BF16)
            raw = [self.sb(c3x, "raw", [128, 3 + T], BF16) for _ in range(2)]
            Dg = self.sb(c3x, "Dg", [128, 6, 4, 128], BF16)
            wch = [self.sb(c3x, "wch", [128, 8, 128], BF16) for _ in range(2)]
            tmpf = [self.sb(c3x, "tmpf", [128, 512], F32) for _ in range(2)]
            c3q = [self.sb(c3x, "c3q", [128, 8, 512], BF16) for _ in range(2)]
            ones3p = self.sb(c3x, "ones3p", [128, 128], BF16)
            S.dve(lambda e: e.memset(ones3p[:], 0.0), writes=["ones3p"])
            S.dve(lambda e: e.memset(ones3p[0:3, :], 1.0), reads=["ones3p"], writes=["ones3p"])
            for i_ in range(2):
                S.dve(lambda e, i_=i_: e.memset(c3q[i_][:], 0.0), writes=[("c3q", i_)])
            Ab8 = self.sb(c3x, "Ab8", [128, 8, 512], F32)
            CBs = [self.sb(c3x, "CBs", [128, 512], BF16) for _ in range(3)]
            Ef = [self.sb(c3x, "Ef", [128, 512], BF16) for _ in range(3)]
            Mt = [self.sb(c3x, "Mt", [128, 512], BF16) for _ in range(3)]
            yz = self.sb(c3x, "yz", [128, 4, 512], F32)
            sq = [self.sb(c3x, "sq", [128, 512], BF16) for _ in range(2)]
            rs = self.sb(c3x, "rs", [128, 512], F32)
            ynt = [self.sb(c3x, "ynt", [128, 512], BF16) for _ in range(2)]
            for i in range(2):
                S.dve(lambda e, i=i: e.memset(raw[i][:, 0:3], 0.0), writes=[("rawpad", i)])
            wi = 0
            ri = 0
            ti = 0
            for g in range(4):
                a, hloc = (8 * g) // 16, (8 * g) % 16
                chunks = [("xs", i, 2048 + g * 512 + i * 128, g * 4 + i) for i in range(4)]
                chunks += [("B", 0, 4096 + g * 128, 16 + g), ("C", 0, 4608 + g * 128, 20 + g)]
                chunks += [("z", i, g * 512 + i * 128, None) for i in range(4)]
                ci = 0
                for kind, idx, col0, fc in chunks:
                    ws = wi % 2
                    wi += 1
                    self.wload(wch[ws][:], Win[:, col0:col0 + 128].rearrange("(k p) c -> p k c", p=128), ("wch", ws))
                    if fc is not None:
                        for k in range(4):
                            S.dve(lambda e, ci=ci, k=k, fc=fc: e.tensor_scalar(out=Dg[:, ci, k, :], in0=self.identb[:], scalar1=cw[:, fc, k:k + 1],
                                                                              scalar2=None, op0=ALU.mult),
                                  reads=["identb", "cw"], writes=[("Dg", ci)])
                        rw = ri % 2
                        ri += 1
                    for tt in range(4):
                        pp, ppk = P[tt % 2], ("ps", tt % 2)
                        for k in range(8):
                            S.pe(lambda e, k=k, tt=tt, pp=pp, ws=ws: e.matmul(pp[:], lhsT=wch[ws][:, k, :], rhs=self.xT[:, k, tt * 512:(tt + 1) * 512],
                                                                         start=(k == 0), stop=(k == 7)),
                                 reads=[("wch", ws)] + [("xT", tt * 4 + q) for q in range(4)], writes=[ppk])
                        if fc is None:
                            S.act(lambda e, tt=tt, pp=pp, idx=idx: e.activation(out=zT[:, idx, tt * 512:(tt + 1) * 512], in_=pp[:], func=AF.Silu),
                                  reads=[ppk], writes=[("zT", idx, tt)])
                        else:
                            S.act(lambda e, tt=tt, pp=pp, rw=rw: e.activation(out=raw[rw][:, 3 + tt * 512:3 + (tt + 1) * 512], in_=pp[:], func=AF.Copy),
                                  reads=[ppk, ("rawpad", rw)], writes=[("raw", rw, tt)])
                    if fc is not None:
                        for tt in range(4):
                            pc, pck = P[2 + tt % 2], ("ps", 2 + tt % 2)
                            rr = [("raw", rw, tt), ("rawpad", rw)] + ([("raw", rw, tt - 1)] if tt > 0 else [])
                            for k in range(4):
                                S.pe(lambda e, k=k, tt=tt, pc=pc, ci=ci, rw=rw: e.matmul(pc[:], lhsT=Dg[:, ci, k, :],
                                                                                   rhs=raw[rw][:, tt * 512 + k:tt * 512 + k + 512],
                                                                                   start=(k == 0), stop=(k == 3)),
                                     reads=rr + [("Dg", ci)], writes=[pck])
                            if kind == "xs":
                                tf, tfk = tmpf[ti % 2], ("tmpf", ti % 2)
                                ti += 1
                                S.act(lambda e, pc=pc, tf=tf, fc=fc: e.activation(out=tf[:], in_=pc[:], func=AF.Silu, bias=cb[:, fc:fc + 1], scale=1.0),
                                      reads=[pck, "cb"], writes=[tfk])
                                S.dve(lambda e, tf=tf, idx=idx, tt=tt: e.tensor_copy(out=xsT[:, idx, tt * 512:(tt + 1) * 512], in_=tf[:]),
                                      reads=[tfk], writes=[("xsT", idx, tt)])
                                pt, ptk = P[4 + tt % 2], ("ps", 4 + tt % 2)
                                for q in range(4):
                                    S.pe(lambda e, q=q, pt=pt, tf=tf: e.transpose(out=pt[:, q * 128:(q + 1) * 128], in_=tf[:, q * 128:(q + 1) * 128],
                                                                                identity=self.identf[:]), reads=[tfk, "identf"], writes=[ptk])
                                h0 = 8 * g + 2 * idx
                                S.dve(lambda e, pt=pt, tt=tt, idx=idx, h0=h0: e.tensor_tensor(
                                    out=Vg[:, tt * 4:tt * 4 + 4, idx * 128:(idx + 1) * 128].rearrange("p b (h d) -> p b h d", h=2),
                                    in0=pt[:].rearrange("p (b h d) -> p b h d", b=4, h=2),
                                    in1=dtk[:, tt * 4:tt * 4 + 4, h0:h0 + 2].unsqueeze(3).to_broadcast([128, 4, 2, 64]), op=ALU.mult),
                                    reads=[ptk, "dtk"], writes=[("Vg", tt)])
                            else:
                                dstT = BT if kind == "B" else CT
                                S.act(lambda e, pc=pc, dstT=dstT, tt=tt, fc=fc: e.activation(out=dstT[:, tt * 512:(tt + 1) * 512], in_=pc[:], func=AF.Silu,
                                                                                          bias=cb[:, fc:fc + 1], scale=1.0),
                                      reads=[pck, "cb"], writes=[(kind + "T", tt)])
                        ci += 1
                its = []
                for qt in range(4):
                    nkb = 4 * qt + 4
                    for half in range(2):
                        for kb in range(nkb):
                            for hq in range(4):
                                its.append(dict(qt=qt, kb=kb, hl=4 * half + hq, hq=hq, half=half, first=(kb == 0), last=(kb == nkb - 1),
                                                cbn=len(its) // 4, epi=(kb == nkb - 1 and hq == 3)))
                PSB = (0, 1, 7)

                def epi_half(qt, half, g=g):
                    t0 = qt * 512
                    for hq in range(4):
                        i = 2 * half + hq // 2
                        r0 = 64 * (hq % 2)
                        fcg = g * 4 + i
                        po, pok = P[3 + hq], ("ps", 3 + hq)
                        S.dve(lambda e, i=i, po=po, fcg=fcg, t0=t0, r0=r0: e.scalar_tensor_tensor(
                            out=yz[r0:r0 + 64, i, :], in0=xsT[r0:r0 + 64, i, t0:t0 + 512], scalar=dskp[r0:r0 + 64, fcg:fcg + 1], in1=po[r0:r0 + 64, :],
                            op0=ALU.mult, op1=ALU.add), reads=[("xsT", i, qt), "dskp0", "dskp1", pok], writes=[("yz", i)])
                        S.dve(lambda e, i=i, t0=t0, r0=r0: e.tensor_tensor(out=yz[r0:r0 + 64, i, :], in0=yz[r0:r0 + 64, i, :],
                                                                        in1=zT[r0:r0 + 64, i, t0:t0 + 512], op=ALU.mult),
                              reads=[("yz", i), ("zT", i, qt)], writes=[("yz", i)])

                def epilogue(qt, g=g):
                    t0 = qt * 512
                    pss, pssk = P[7], ("ps", 7)
                    for i in range(4):
                        fcg = g * 4 + i
                        sqt, sqk = sq[i % 2], ("sq", i % 2)
                        S.act(lambda e, i=i, sqt=sqt: e.activation(out=sqt[:], in_=yz[:, i, :], func=AF.Square), reads=[("yz", i)], writes=[sqk])
                        S.pe(lambda e, i=i, sqt=sqt: e.matmul(pss[:], lhsT=self.onesb[:], rhs=sqt[:], start=(i == 0), stop=(i == 3)),
                             reads=[sqk, "onesb"], writes=[pssk])
                    S.act(lambda e: e.activation(out=rs[:], in_=pss[:], func=AF.Sqrt, bias=self.epsb[:, 0:1], scale=1.0 / 512.0),
                          reads=[pssk, "epsb"], writes=["rs"])
                    S.dve(lambda e: e.reciprocal(out=rs[:], in_=rs[:]), reads=["rs"], writes=["rs"])
                    for i in range(4):
                        fcg = g * 4 + i
                        yt, ytk = ynt[i % 2], ("ynt", i % 2)
                        S.dve(lambda e, i=i, yt=yt, fcg=fcg: e.scalar_tensor_tensor(out=yt[:], in0=yz[:, i, :], scalar=nwp[:, fcg:fcg + 1], in1=rs[:],
                                                                                  op0=ALU.mult, op1=ALU.mult),
                              reads=[("yz", i), "nwp", "rs"], writes=[ytk])
                        S.dma("sp", lambda e, yt=yt, fcg=fcg, t0=t0: e.dma_start(out=ysp[fcg, :, t0:t0 + 512], in_=yt[:]),
                              reads=[ytk], writes=[("ysp", fcg, qt)], semkey=("yspst", i % 2))

                def d1(it, n, g=g):
                    qt, kb, hl = it["qt"], it["kb"], it["hl"]
                    t0, j0 = qt * 512, kb * 128
                    off = max(0, j0 - t0)
                    diag = j0 >= t0
                    h = 8 * g + hl
                    cbt, cbk = CBs[it["cbn"] % 3], ("CBs", it["cbn"] % 3)
                    if it["hq"] == 0:
                        pcb, pcbk = P[2], ("ps", 2)
                        S.pe(lambda e: e.matmul(pcb[:, off:512], lhsT=BT[:, j0:j0 + 128], rhs=CT[:, t0 + off:t0 + 512], start=True, stop=True),
                             reads=[("BT", kb // 4), ("CT", qt)], writes=[pcbk])
                        S.dve(lambda e: e.tensor_copy(out=cbt[:, off:512], in_=pcb[:, off:512]), reads=[pcbk], writes=[cbk])
                    ps, psk = P[PSB[n % 3]], ("ps", PSB[n % 3])
                    et, ek = Ef[n % 3], ("Ef", n % 3)
                    mt, mk = Mt[n % 3], ("Mt", n % 3)
                    c3t, c3k = c3q[qt % 2], ("c3q", qt % 2)
                    if kb == 0 and hl == 0:
                        a_, hloc_ = (8 * g) // 16, (8 * g) % 16
                        S.dma("sp", lambda e, c3t=c3t, a_=a_, hloc_=hloc_: e.dma_start(
                            out=c3t[0:3, :, :], in_=self.scr[1 + a_, :, :].rearrange("i (h t) -> i h t", h=16)[:, hloc_:hloc_ + 8, t0:t0 + 512]),
                            reads=[("scrA", a_)], writes=[c3k])
                    if qt > 0 and kb == 0 and hl == 0:
                        for h2 in range(8):
                            pa, pak = P[PSB[h2 % 3]], ("ps", PSB[h2 % 3])
                            S.pe(lambda e, h2=h2, pa=pa: e.matmul(pa[:, 0:512], lhsT=ones3p[:], rhs=c3t[:, h2, :],
                                                                 start=True, stop=True), reads=[c3k, "onesb"], writes=[pak])
                            if False:
                                pass
                            else:
                                S.dve(lambda e, h2=h2, pa=pa: e.tensor_copy(out=Ab8[:, h2, :], in_=pa[:, 0:512]), reads=[pak], writes=[("Ab8", h2)])
                    if diag:
                        S.pe(lambda e: e.matmul(ps[:, off:512], lhsT=ones3p[:], rhs=c3t[:, hl, off:512],
                                                start=True, stop=False), reads=[c3k, "ones3p"], writes=[psk])
                        S.pe(lambda e: e.matmul(ps[:, off:off + 128], lhsT=self.identb[:], rhs=self.negm[:], start=False, stop=True),
                             reads=["identb", "negm"], writes=[psk])
                        S.act(lambda e: e.activation(out=et[:, off:512], in_=ps[:, off:512], func=AF.Exp, bias=nAT[:, kb, h:h + 1], scale=1.0),
                              reads=[psk, "nAT"], writes=[ek])
                    else:
                        S.act(lambda e: e.activation(out=et[:, 0:512], in_=Ab8[:, hl, :], func=AF.Exp, bias=nAT[:, kb, h:h + 1], scale=1.0),
                              reads=[("Ab8", hl), "nAT"], writes=[ek])
                    S.dve(lambda e: e.tensor_tensor(out=mt[:, off:512], in0=et[:, off:512], in1=cbt[:, off:512], op=ALU.mult),
                          reads=[ek, cbk], writes=[mk])

                def d2(it, n, g=g):
                    qt, kb, hl = it["qt"], it["kb"], it["hl"]
                    t0, j0 = qt * 512, kb * 128
                    off = max(0, j0 - t0)
                    hq, half = it["hq"], it["half"]
                    po, pok = P[3 + hq], ("ps", 3 + hq)
                    mt, mk = Mt[n % 3], ("Mt", n % 3)
                    pc0 = (hl // 2) * 128
                    S.pe(lambda e: e.matmul(po[:, off:512], lhsT=Vg[:, kb, pc0:pc0 + 128], rhs=mt[:, off:512],
                                            start=it["first"], stop=it["last"]), reads=[mk, ("Vg", kb // 4)], writes=[pok])
                    if it["epi"]:
                        epi_half(qt, half)
                        if half == 1:
                            epilogue(qt)
                self.run_pipeline(its, [d1, d2], [2])
            S.flush(barrier=True)
        ynb = [self.sb(sctx, "ynb", [128, 16, 128], BF16) for _ in range(2)]

        def pre_block(b):
            S.dma("sp", lambda e: e.dma_start(out=ynb[b % 2][:], in_=ysp[:, :, b * 128:(b + 1) * 128].rearrange("c p t -> p c t")),
                  writes=[("ynb", b % 2)])
        self.mixer_out(sctx, layer, lambda k, b: ynb[b % 2][:, k, :], lambda k, b: [("ynb", b % 2)], 16, Wo, L, pre_block=pre_block)


_CACHE = {}


def run(inputs, stages):
    key = tuple(stages)
    if key not in _CACHE:
        bld = Builder(stages)
        nc = bld.build()
        _CACHE[key] = (bld, nc)
    bld, nc = _CACHE[key]
    x = np.asarray(inputs["x"], dtype=np.float32)
    in_maps = []
    shared = {n: np.ascontiguousarray(np.asarray(inputs[n], dtype=np.float32)) for n in bld.used_inputs if n != "x"}
    for c in range(8):
        m = dict(shared)
        m["x"] = np.ascontiguousarray(x[c])
        in_maps.append(m)
    res = run_bass_kernel_spmd(nc, in_maps, core_ids=list(range(8)))
    return np.stack([np.asarray(r["y"]) for r in res.results], axis=0).astype(np.float32)


ALL_INPUT_NAMES = (
    "x",
    "l0_ssd_w_in", "l0_ssd_conv_w", "l0_ssd_conv_b", "l0_ssd_dt_bias", "l0_ssd_a_log", "l0_ssd_d_skip", "l0_ssd_norm_w", "l0_ssd_w_out",
    "l0_ln_mix_g", "l0_ln_mix_b", "l0_ffn_w_gate", "l0_ffn_w_up", "l0_ffn_w_down", "l0_ln_ffn_g", "l0_ln_ffn_b",
    "l1_sb_w_qkv", "l1_sb_w_out", "l1_ln_mix_g", "l1_ln_mix_b",
    "l1_moe_w_router", "l1_moe_w_gate", "l1_moe_w_up", "l1_moe_w_down", "l1_ln_ffn_g", "l1_ln_ffn_b",
    "l2_fox_w_qkvf", "l2_fox_b_f", "l2_fox_w_out", "l2_ln_mix_g", "l2_ln_mix_b",
    "l2_ffn_w_gate", "l2_ffn_w_up", "l2_ffn_w_down", "l2_ln_ffn_g", "l2_ln_ffn_b",
    "l3_ssd_w_in", "l3_ssd_conv_w", "l3_ssd_conv_b", "l3_ssd_dt_bias", "l3_ssd_a_log", "l3_ssd_d_skip", "l3_ssd_norm_w", "l3_ssd_w_out",
    "l3_ln_mix_g", "l3_ln_mix_b",
    "l3_moe_w_router", "l3_moe_w_gate", "l3_moe_w_up", "l3_moe_w_down", "l3_ln_ffn_g", "l3_ln_ffn_b",
)


def kernel(**inputs):
    missing = [n for n in ALL_INPUT_NAMES if n not in inputs]
    assert not missing, missing
    return run(inputs, list(range(8)))
```
